# Optimizing a Trainium2 kernel written in Bass

```python
import math
import jax, jax.numpy as jnp
from jax import lax
import numpy as np

D_MODEL = 1024
BATCH = 8
SEQ = 4096
DEPTH = 1

RET_HEADS = 8
RET_DK = 64
RET_DV = 64
RET_WIDTH = RET_HEADS * RET_DV
CHUNK = 128
ROPE_BASE = 10000.0
DIFF_HEADS = 4
DIFF_DK = 64
DIFF_DV = 2 * DIFF_DK
DIFF_WIDTH = DIFF_HEADS * DIFF_DV
Q_BLOCK = 128
REL_BUCKETS = 32
REL_MAX_DIST = 128
PEER_HEADS = 8
PEER_NKEYS = 128
PEER_EXPERTS = PEER_NKEYS * PEER_NKEYS
PEER_DQ = 256
PEER_TOPK = 16
TOKEN_BLOCK = 128
EPS = 1e-6

MIX_WIDTH = RET_WIDTH + DIFF_WIDTH
IN_SIZES = [RET_HEADS * RET_DK, RET_HEADS * RET_DK, RET_WIDTH, RET_WIDTH,
            DIFF_HEADS * 2 * DIFF_DK, DIFF_HEADS * 2 * DIFF_DK, DIFF_WIDTH]
IN_WIDTH = sum(IN_SIZES)
IN_OFFSETS = [int(o) for o in np.cumsum(IN_SIZES)[:-1]]

kernel_name = "hymba_retention_diffattn_peer"


def rmsnorm(x, g):
    xf = x.astype(jnp.float32)
    y = xf * lax.rsqrt(jnp.mean(xf * xf, axis=-1, keepdims=True) + EPS)
    return (y * g.astype(jnp.float32)).astype(x.dtype)


def rotary(x, pos):
    half = x.shape[-1] // 2
    freqs = ROPE_BASE ** (-jnp.arange(half, dtype=jnp.float32) / half)
    ang = pos.astype(jnp.float32)[:, None] * freqs[None, :]
    cos, sin = jnp.cos(ang).astype(x.dtype), jnp.sin(ang).astype(x.dtype)
    x1, x2 = x[..., :half], x[..., half:]
    return jnp.concatenate([x1 * cos - x2 * sin, x2 * cos + x1 * sin], axis=-1)


def retention(q, k, v):
    B, H, S, dk = q.shape
    dv = v.shape[-1]
    C = S // CHUNK
    log_gamma = jnp.log(1.0 - 2.0 ** (-5.0 - jnp.arange(H, dtype=jnp.float32)))
    idx = jnp.arange(CHUNK, dtype=jnp.float32)
    dist = idx[:, None] - idx[None, :]
    decay = jnp.where(dist >= 0, jnp.exp(log_gamma[:, None, None] * jnp.maximum(dist, 0.0)), 0.0)
    qc = q.astype(jnp.float32).reshape(B, H, C, CHUNK, dk)
    kc = k.astype(jnp.float32).reshape(B, H, C, CHUNK, dk)
    vc = v.astype(jnp.float32).reshape(B, H, C, CHUNK, dv)
    scores = jnp.einsum('bhcld,bhcmd->bhclm', qc, kc) * decay[None, :, None]
    inner = jnp.einsum('bhclm,bhcme->bhcle', scores, vc)
    k_decay = jnp.exp(log_gamma[:, None] * (CHUNK - 1 - idx)[None, :])
    kv = jnp.einsum('bhcld,bhcle->cbhde', kc * k_decay[None, :, None, :, None], vc)
    chunk_decay = jnp.exp(log_gamma * CHUNK)[None, :, None, None]

    def step(R, kv_c):
        return R * chunk_decay + kv_c, R

    _, R_prev = lax.scan(step, jnp.zeros((B, H, dk, dv), jnp.float32), kv)
    q_decay = jnp.exp(log_gamma[:, None] * (idx + 1.0)[None, :])
    cross = jnp.einsum('bhcld,cbhde->bhcle', qc, R_prev) * q_decay[None, :, None, :, None]
    return (inner + cross).reshape(B, H, S, dv)


def t5_bucket(n):
    n = jnp.maximum(n, 0)
    max_exact = REL_BUCKETS // 2
    large = max_exact + (jnp.log(jnp.maximum(n, 1).astype(jnp.float32) / max_exact)
                         / math.log(REL_MAX_DIST / max_exact)
                         * (REL_BUCKETS - max_exact)).astype(jnp.int32)
    large = jnp.minimum(large, REL_BUCKETS - 1)
    return jnp.where(n < max_exact, n, large)


def diff_attention(q1, q2, k1, k2, v, lam, rel_bias):
    B, H, S, _ = q1.shape
    dv = v.shape[-1]
    nb = S // Q_BLOCK
    kpos = jnp.arange(S, dtype=jnp.int32)
    scale = DIFF_DK ** -0.5
    vf = v.astype(jnp.float32)

    def block(i):
        start = i * Q_BLOCK
        qb1 = lax.dynamic_slice_in_dim(q1, start, Q_BLOCK, axis=2)
        qb2 = lax.dynamic_slice_in_dim(q2, start, Q_BLOCK, axis=2)
        qpos = start + jnp.arange(Q_BLOCK, dtype=jnp.int32)
        rel = qpos[:, None] - kpos[None, :]
        bias = jnp.take(rel_bias.astype(jnp.float32), t5_bucket(rel), axis=0)
        bias = jnp.transpose(bias, (2, 0, 1))[None]
        mask = (rel >= 0)[None, None]

        def probs(qb, k):
            s = jnp.einsum('bhqd,bhkd->bhqk', qb, k).astype(jnp.float32) * scale + bias
            return jax.nn.softmax(jnp.where(mask, s, -jnp.inf), axis=-1)

        p = probs(qb1, k1) - lam * probs(qb2, k2)
        return jnp.einsum('bhqk,bhkd->bhqd', p, vf)

    out = lax.map(block, jnp.arange(nb))
    return jnp.transpose(out, (1, 2, 0, 3, 4)).reshape(B, H, S, dv)


def peer(h, w_pq, sub_keys, w_down, w_up):
    B, S, D = h.shape
    hb = h.reshape((B * S) // TOKEN_BLOCK, TOKEN_BLOCK, D)

    def block(hx):
        q = (hx @ w_pq).reshape(TOKEN_BLOCK, PEER_HEADS, 2, PEER_DQ // 2)
        s = jnp.einsum('nhpd,hpkd->nhpk', q, sub_keys).astype(jnp.float32)
        sv, si = lax.top_k(s, PEER_TOPK)
        cand = (sv[:, :, 0, :, None] + sv[:, :, 1, None, :]).reshape(TOKEN_BLOCK, PEER_HEADS, PEER_TOPK * PEER_TOPK)
        cidx = (si[:, :, 0, :, None] * PEER_NKEYS + si[:, :, 1, None, :]).reshape(TOKEN_BLOCK, PEER_HEADS, PEER_TOPK * PEER_TOPK)
        top, pos = lax.top_k(cand, PEER_TOPK)
        experts = jnp.take_along_axis(cidx, pos, axis=-1)
        g = jax.nn.softmax(top, axis=-1)
        u = w_down[experts]
        a = jnp.einsum('nd,nhkd->nhk', hx, u).astype(jnp.float32)
        act = (jax.nn.gelu(a, approximate=False) * g).astype(hx.dtype)
        vsel = w_up[experts]
        return jnp.einsum('nhk,nhkd->nd', act, vsel)

    return lax.map(block, hb).reshape(B, S, D)


def setup_inputs(seed: int = 0) -> dict:
    key = jax.random.key(seed)
    ks = jax.random.split(key, 20)
    f32 = jnp.float32
    nrm = lambda k, shape, s: jax.random.normal(k, shape, f32) * s
    return {
        "x": nrm(ks[0], (BATCH, SEQ, D_MODEL), 1.0),
        "norm_mix": 1.0 + nrm(ks[1], (DEPTH, D_MODEL), 0.01),
        "w_in": nrm(ks[2], (DEPTH, D_MODEL, IN_WIDTH), D_MODEL ** -0.5),
        "ret_gn": 1.0 + nrm(ks[3], (DEPTH, RET_WIDTH), 0.01),
        "diff_lambda_q1": nrm(ks[4], (DEPTH, DIFF_DK), 0.1),
        "diff_lambda_k1": nrm(ks[5], (DEPTH, DIFF_DK), 0.1),
        "diff_lambda_q2": nrm(ks[6], (DEPTH, DIFF_DK), 0.1),
        "diff_lambda_k2": nrm(ks[7], (DEPTH, DIFF_DK), 0.1),
        "diff_subln": 1.0 + nrm(ks[8], (DEPTH, DIFF_DV), 0.01),
        "rel_bias": nrm(ks[9], (REL_BUCKETS, DIFF_HEADS), 0.5),
        "w_out": nrm(ks[10], (DEPTH, MIX_WIDTH, D_MODEL), MIX_WIDTH ** -0.5),
        "norm_ffn": 1.0 + nrm(ks[11], (DEPTH, D_MODEL), 0.01),
        "peer_query": nrm(ks[12], (DEPTH, D_MODEL, PEER_HEADS * PEER_DQ), D_MODEL ** -0.5),
        "peer_keys": nrm(ks[13], (DEPTH, PEER_HEADS, 2, PEER_NKEYS, PEER_DQ // 2), (PEER_DQ // 2) ** -0.5),
        "peer_down": nrm(ks[14], (DEPTH, PEER_EXPERTS, D_MODEL), D_MODEL ** -0.5),
        "peer_up": nrm(ks[15], (DEPTH, PEER_EXPERTS, D_MODEL), PEER_HEADS ** -0.5),
        "norm_final": 1.0 + nrm(ks[16], (D_MODEL,), 0.01),
    }


def reference(x, norm_mix, w_in, ret_gn, diff_lambda_q1, diff_lambda_k1, diff_lambda_q2,
              diff_lambda_k2, diff_subln, rel_bias, w_out, norm_ffn, peer_query, peer_keys,
              peer_down, peer_up, norm_final):
    B, S, _ = x.shape
    pos = jnp.arange(S, dtype=jnp.int32)

    def heads(t, H):
        return t.reshape(B, S, H, -1).transpose(0, 2, 1, 3)

    for l in range(DEPTH):
        lambda_init = 0.8 - 0.6 * math.exp(-0.3 * l)
        h = rmsnorm(x, norm_mix[l])
        proj = h @ w_in[l]
        rq, rk, rv, rg, dq, dk, dvv = jnp.split(proj, IN_OFFSETS, axis=-1)

        rq = rotary(heads(rq, RET_HEADS), pos)
        rk = rotary(heads(rk, RET_HEADS), pos) * (RET_DK ** -0.5)
        ro = retention(rq, rk, heads(rv, RET_HEADS))
        mu = jnp.mean(ro, axis=-1, keepdims=True)
        var = jnp.mean(jnp.square(ro - mu), axis=-1, keepdims=True)
        ro = ((ro - mu) * lax.rsqrt(var + EPS)).transpose(0, 2, 1, 3).reshape(B, S, RET_WIDTH)
        ro = jax.nn.silu(rg.astype(jnp.float32)) * (ro * ret_gn[l].astype(jnp.float32))

        dq = dq.reshape(B, S, DIFF_HEADS, 2, DIFF_DK)
        dk = dk.reshape(B, S, DIFF_HEADS, 2, DIFF_DK)
        q1 = dq[..., 0, :].transpose(0, 2, 1, 3)
        q2 = dq[..., 1, :].transpose(0, 2, 1, 3)
        k1 = dk[..., 0, :].transpose(0, 2, 1, 3)
        k2 = dk[..., 1, :].transpose(0, 2, 1, 3)
        lam = (jnp.exp(jnp.sum(diff_lambda_q1[l].astype(jnp.float32) * diff_lambda_k1[l].astype(jnp.float32)))
               - jnp.exp(jnp.sum(diff_lambda_q2[l].astype(jnp.float32) * diff_lambda_k2[l].astype(jnp.float32)))
               + lambda_init)
        do = diff_attention(q1, q2, k1, k2, heads(dvv, DIFF_HEADS), lam, rel_bias)
        do = do * lax.rsqrt(jnp.mean(do * do, axis=-1, keepdims=True) + EPS)
        do = do * diff_subln[l].astype(jnp.float32) * (1.0 - lambda_init)
        do = do.transpose(0, 2, 1, 3).reshape(B, S, DIFF_WIDTH)

        mix = jnp.concatenate([ro, do], axis=-1).astype(x.dtype)
        x = x + mix @ w_out[l]

        h2 = rmsnorm(x, norm_ffn[l])
        x = x + peer(h2, peer_query[l], peer_keys[l], peer_down[l], peer_up[l]).astype(x.dtype)

    return rmsnorm(x, norm_final)
```

```python
import math
import os
from contextlib import ExitStack

import numpy as np
import concourse.bass as bass
import concourse.mybir as mybir
from concourse.bass_utils import run_bass_kernel_spmd

F32 = mybir.dt.float32
BF16 = mybir.dt.bfloat16
AF = mybir.ActivationFunctionType
ALU = mybir.AluOpType
AX = mybir.AxisListType

S = 4096
D = 1024
NC8 = 8
EPS = 1e-6
LAMBDA_INIT = 0.8 - 0.6 * math.exp(-0.3 * 0)
NEG = -30000.0


class TB:
    __slots__ = ("name", "w", "r")

    def __init__(self, name):
        self.name = name
        self.w = {}
        self.r = {}


class DSem:
    __slots__ = ("sem", "total")

    def __init__(self, sem):
        self.sem = sem
        self.total = 0


class Eng:
    def __init__(self, eng, sem, name, is_pe=False):
        self.eng = eng
        self.sem = sem
        self.name = name
        self.is_pe = is_pe
        self.cnt = 0
        self.seen = {}
        self.pend_r = []
        self.pend_w = []

    def _wait(self, evs):
        need = {}
        for so, v in evs:
            if isinstance(so, DSem):
                key, val = so.sem, so.total
            else:
                key, val = so.sem, v
            if need.get(key, 0) < val:
                need[key] = val
        for key, val in need.items():
            if self.seen.get(key, 0) < val:
                self.eng.wait_ge(key, val)
                self.seen[key] = val

    def _deps(self, reads, writes, own_dsem=None):
        evs = []
        for b in reads:
            for so, v in b.w.items():
                if so is self and self.is_pe:
                    continue
                evs.append((so, v))
        for b in writes:
            for so, v in b.w.items():
                if so is own_dsem or (so is self and self.is_pe):
                    continue
                evs.append((so, v))
            for so, v in b.r.items():
                if so is self and self.is_pe:
                    continue
                evs.append((so, v))
        return evs

    def op(self, fn, reads=(), writes=(), inc=True):
        self._wait(self._deps(reads, writes))
        ins = fn(self.eng)
        self.pend_r.extend(reads)
        self.pend_w.extend(writes)
        if inc:
            self.cnt += 1
            ins.then_inc(self.sem, 1)
            for b in self.pend_r:
                b.r[self] = self.cnt
            for b in self.pend_w:
                b.w = {self: self.cnt}
                b.r = {}
            self.pend_r = []
            self.pend_w = []
        return ins

    def dma(self, out, in_, reads, writes, dsem):
        assert not self.pend_r and not self.pend_w
        self._wait(self._deps(reads, writes, own_dsem=dsem))
        ins = self.eng.dma_start(out=out, in_=in_)
        dsem.total += 16
        ins.then_inc(dsem.sem, 16)
        for b in reads:
            b.r[dsem] = dsem.total
        for b in writes:
            b.w = {dsem: dsem.total}
            b.r = {}
        return ins


class FW:
    def __init__(self, nc, es):
        self.nc = nc
        self.es = es
        mk = lambda n: es.enter_context(nc.semaphore(n))
        self.pe = Eng(nc.tensor, mk("s_pe"), "pe", is_pe=True)
        self.act = Eng(nc.scalar, mk("s_act"), "act")
        self.dve = Eng(nc.vector, mk("s_dve"), "dve")
        self.pool = Eng(nc.gpsimd, mk("s_pool"), "pool")
        self.sp = Eng(nc.sync, mk("s_sp"), "sp")
        self.engs = [self.pe, self.act, self.dve, self.pool, self.sp]
        self.dsems = []
        self.nsem = 0
        self.banks = []
        self.bank_i = 0

    def dsem(self, name=None):
        self.nsem += 1
        d = DSem(self.es.enter_context(self.nc.semaphore(name or ("d%d" % self.nsem))))
        self.dsems.append(d)
        return d

    def barrier(self):
        for e in self.engs:
            evs = [(o, o.cnt) for o in self.engs if o is not e and o.cnt > 0]
            evs += [(d, d.total) for d in self.dsems if d.total > 0]
            e._wait(evs)

    def sb(self, stack, name, shape, dtype):
        t = stack.enter_context(self.nc.sbuf_tensor("sb_" + name, list(shape), dtype))
        return t, TB(name)

    def init_psum(self, stack):
        for i in range(8):
            t = stack.enter_context(self.nc.psum_tensor("bank%d" % i, [128, 512], F32))
            self.banks.append((t, TB("bank%d" % i)))

    def bank(self):
        b = self.banks[self.bank_i % 8]
        self.bank_i += 1
        return b


class T:
    def __init__(self, fw, stack, name, shape, dtype, dma=False, parts=0):
        self.t, self.b = fw.sb(stack, name, shape, dtype)
        self.d = fw.dsem("ds_" + name) if dma else None
        self.bs = [TB("%s.%d" % (name, i)) for i in range(parts)]

    def __getitem__(self, k):
        return self.t[k]


class Ring:
    def __init__(self, tiles):
        self.tiles = tiles
        self.i = 0

    def next(self):
        t = self.tiles[self.i % len(self.tiles)]
        self.i += 1
        return t


def _t5_bucket_np(n):
    n = np.maximum(n, 0)
    max_exact = 16
    nf = np.maximum(n, 1).astype(np.float32) / np.float32(max_exact)
    large = max_exact + (np.log(nf).astype(np.float32) / np.float32(math.log(128 / max_exact)) * np.float32(32 - max_exact)).astype(np.int32)
    large = np.minimum(large, 31)
    return np.where(n < max_exact, n, large)


def _const_tables():
    f32 = np.float32
    pos = np.arange(S, dtype=f32)
    freqs = (np.float32(10000.0) ** (-np.arange(32, dtype=f32) / np.float32(32))).astype(f32)
    ang = (pos[:, None] * freqs[None, :]).astype(f32)
    cos, sin = np.cos(ang).astype(f32), np.sin(ang).astype(f32)
    d = np.arange(128) % 64
    j = d % 32
    sign = np.where(d < 32, -1.0, 1.0).astype(f32)
    cosT2 = np.ascontiguousarray(cos[:, j].T)
    sinT2 = np.ascontiguousarray((sin[:, j] * sign[None, :]).T)
    gam = 1.0 - 2.0 ** (-5.0 - np.arange(8, dtype=np.float64))
    idx = np.arange(128, dtype=np.float64)
    dist = idx[None, :] - idx[:, None]
    decT = np.where(dist >= 0, gam[:, None, None] ** np.maximum(dist, 0.0)[None], 0.0) * 0.125
    decT = np.ascontiguousarray(decT.transpose(1, 0, 2).reshape(128, 1024)).astype(f32)
    kd = gam[None, :] ** (127.0 - idx)[:, None] * 0.125
    kdec = np.ascontiguousarray(np.repeat(kd, 64, axis=1)).astype(f32)
    qd = gam[:, None] ** (idx + 1.0)[None, :]
    qdec = np.zeros((128, 4, 512), f32)
    for p in range(128):
        for pair in range(4):
            h = 2 * pair + p // 64
            qdec[p, pair, :] = np.tile(qd[h], 4)
    cdec = np.ascontiguousarray(np.repeat((gam ** 128.0)[None, :], 64, axis=1).repeat(64, axis=0)).astype(f32)
    ident = np.eye(128, dtype=f32)
    iota = np.ascontiguousarray(np.broadcast_to(np.arange(128, dtype=f32)[None, :], (128, 128)))
    iota16r = np.ascontiguousarray(np.broadcast_to((np.arange(2048) % 16).astype(f32)[None, :], (128, 2048)))
    return dict(cosT2=cosT2, sinT2=sinT2, decT=decT, kdec=kdec, qdec=qdec, cdec=cdec, ident=ident, iota=iota, iota16r=iota16r)


def _host_prep(inp):
    f32 = np.float32
    w_in = np.asarray(inp["w_in"][0], f32)

    def swap_cols(w):
        return np.ascontiguousarray(w.reshape(1024, 8, 2, 32)[:, :, ::-1, :]).reshape(1024, 512)

    w_in_ext = np.ascontiguousarray(np.concatenate([w_in, swap_cols(w_in[:, 0:512]), swap_cols(w_in[:, 512:1024])], axis=1))
    rel_bias = np.asarray(inp["rel_bias"], f32)
    k = np.arange(128)[:, None]
    q = np.arange(128)[None, :]
    b0 = _t5_bucket_np(q - k)
    b1 = _t5_bucket_np(128 + q - k)
    biasT = np.zeros((128, 4, 2, 128), f32)
    for h in range(4):
        biasT[:, h, 0, :] = np.where(q >= k, rel_bias[b0, h], f32(NEG))
        biasT[:, h, 1, :] = rel_bias[b1, h]
    shared = dict(
        w_in_ext=w_in_ext,
        w_out=np.ascontiguousarray(inp["w_out"][0], dtype=f32),
        w_pq=np.ascontiguousarray(inp["peer_query"][0], dtype=f32),
        keysT=np.ascontiguousarray(np.asarray(inp["peer_keys"][0], f32).reshape(16, 128, 128).transpose(2, 0, 1)),
        w_downT=np.ascontiguousarray(np.asarray(inp["peer_down"][0], f32).T),
        w_up=np.ascontiguousarray(inp["peer_up"][0], dtype=f32),
        g_mix=np.ascontiguousarray(np.asarray(inp["norm_mix"][0], f32).reshape(8, 128).T),
        g_ffn=np.ascontiguousarray(np.asarray(inp["norm_ffn"][0], f32).reshape(8, 128).T),
        g_fin=np.ascontiguousarray(np.asarray(inp["norm_final"], f32).reshape(8, 128).T),
        retgn_b=np.ascontiguousarray(np.broadcast_to(np.asarray(inp["ret_gn"][0], f32)[None, :], (128, 512))),
        subln_b=np.ascontiguousarray(np.broadcast_to(np.asarray(inp["diff_subln"][0], f32)[None, :], (128, 128))),
        lamvec=np.ascontiguousarray(np.broadcast_to(np.stack([np.asarray(inp[n][0], f32) for n in
                                    ("diff_lambda_q1", "diff_lambda_k1", "diff_lambda_q2", "diff_lambda_k2")])[None], (128, 4, 64))),
        cbias=np.ascontiguousarray(np.broadcast_to(rel_bias[31][None, :], (128, 4))),
        biasT=biasT,
    )
    shared.update(_const_tables())
    return shared


IN_SPECS = [
    ("xT", [1024, S]), ("w_in_ext", [1024, 4608]), ("w_out", [1024, 1024]), ("w_pq", [1024, 2048]),
    ("keysT", [128, 16, 128]), ("w_downT", [1024, 16384]), ("w_up", [16384, 1024]),
    ("g_mix", [128, 8]), ("g_ffn", [128, 8]), ("g_fin", [128, 8]), ("retgn_b", [128, 512]), ("subln_b", [128, 128]),
    ("lamvec", [128, 4, 64]), ("cbias", [128, 4]), ("biasT", [128, 4, 2, 128]),
    ("cosT2", [128, S]), ("sinT2", [128, S]), ("decT", [128, 1024]), ("kdec", [128, 512]), ("qdec", [128, 4, 512]),
    ("cdec", [64, 512]), ("ident", [128, 128]), ("iota", [128, 128]), ("iota16r", [128, 2048]),
]


def build_program(dbg=False, phases="ABCDE"):
    nc = bass.Bass("TRN2", target_bir_lowering=False)
    I = {}
    for name, shape in IN_SPECS:
        I[name] = nc.dram_tensor(name, list(shape), F32, kind="ExternalInput").ap()
    outT = nc.dram_tensor("outT", [1024, S], F32, kind="ExternalOutput").ap()
    SK = "ExternalOutput" if dbg else "Internal"
    scr = lambda name, shape, dtype: nc.dram_tensor(name, list(shape), dtype, kind=SK).ap()
    QT_ret = scr("QT_ret", [512, S], BF16)
    QdT_ret = scr("QdT_ret", [512, S], BF16)
    KT_ret = scr("KT_ret", [512, S], BF16)
    QT_dif = scr("QT_dif", [512, S], BF16)
    KT_dif = scr("KT_dif", [512, S], BF16)
    V_ret = scr("V_ret", [S, 512], BF16)
    SG = scr("SG", [S, 512], F32)
    V_dif = scr("V_dif", [S, 512], BF16)
    mixT = scr("mixT", [1024, S], BF16)
    x1T = scr("x1T", [1024, S], F32)
    h2T_d = scr("h2T_d", [1024, S], BF16)
    LSTd = scr("LSTd", [128, 3, S], BF16)
    wdT_bf = nc.dram_tensor("wdT_bf", [32, 128, 8, 512], BF16, kind="Internal").ap()
    wup_bf = nc.dram_tensor("wup_bf", [32, 128, 4, 1024], BF16, kind="Internal").ap()
    wout_bf = nc.dram_tensor("wout_bf", [1024, 1024], BF16, kind="Internal").ap()
    wpq_bf = nc.dram_tensor("wpq_bf", [1024, 2048], BF16, kind="Internal").ap()
    keys_bf = nc.dram_tensor("keys_bf", [128, 16, 128], BF16, kind="Internal").ap()

    with ExitStack() as es:
        fw = FW(nc, es)
        fw.init_psum(es)
        pe, act, dve, pool, sp = fw.pe, fw.act, fw.dve, fw.pool, fw.sp
        out_dsems = []

        cst = ExitStack()
        es.enter_context(cst)
        ones_f = T(fw, cst, "ones_f", [128, 128], F32)
        epst = T(fw, cst, "epst", [128, 1], F32)
        ident_f = T(fw, cst, "ident_f", [128, 128], F32, dma=True)
        ident_b = T(fw, cst, "ident_b", [128, 128], BF16)
        sqr = Ring([T(fw, cst, "sq%d" % i, [128, 512], F32) for i in range(2)])
        sd = T(fw, cst, "sd", [128, 512], F32)
        dve.op(lambda e: e.memset(ones_f[:], 1.0), [], [ones_f.b])
        neghalf = T(fw, cst, "neghalf", [128, 16], F32)
        dve.op(lambda e: e.memset(neghalf[:], -0.5), [], [neghalf.b])
        dve.op(lambda e: e.memset(epst[:], EPS), [], [epst.b])
        sp.dma(ident_f[:], I["ident"][:, :], [], [ident_f.b], ident_f.d)
        dve.op(lambda e: e.tensor_copy(out=ident_b[:], in_=ident_f[:]), [ident_f.b], [ident_b.b])

        wcast = fw.dsem("wcast")
        wd_b = TB("wdT_bf")
        wu_b = TB("wup_bf")

        wdv = I["w_downT"].rearrange("(c p) e -> p c e", p=128)
        wuv = I["w_up"].rearrange("(i j) d -> j i d", j=128)
        wcast_todo = []
        for ig in range(32):
            wcast_todo.append(lambda ig=ig: pool.dma(wdT_bf[ig], wdv[:, :, ig * 512:(ig + 1) * 512], [], [wd_b], wcast))
            wcast_todo.append(lambda ig=ig: pool.dma(wup_bf[ig], wuv[:, ig * 4:(ig + 1) * 4, :], [], [wu_b], wcast))

        wsm_b = TB("wsmall_bf")
        for r in range(2):
            wcast_todo.append(lambda r=r: pool.dma(wout_bf[r * 512:(r + 1) * 512, :], I["w_out"][r * 512:(r + 1) * 512, :], [], [wsm_b], wcast))
        for r in range(4):
            wcast_todo.append(lambda r=r: pool.dma(wpq_bf[r * 256:(r + 1) * 256, :], I["w_pq"][r * 256:(r + 1) * 256, :], [], [wsm_b], wcast))
        wcast_todo.append(lambda: pool.dma(keys_bf[:, :, :], I["keysT"][:, :, :], [], [wsm_b], wcast))

        def issue_wcasts(n=None):
            k = len(wcast_todo) if n is None else min(n, len(wcast_todo))
            for _ in range(k):
                wcast_todo.pop(0)()

        def rms_a(xt, N, bank=None):
            bk, bk_b = bank if bank is not None else fw.bank()
            for c in range(8):
                sq = sqr.next()
                act.op(lambda e: e.activation(out=sq[:, 0:N], in_=xt[:, c, 0:N], func=AF.Square), [xt.b], [sq.b])
                pe.op(lambda e: e.matmul(bk[:, 0:N], lhsT=ones_f[:], rhs=sq[:, 0:N], start=(c == 0), stop=(c == 7)),
                      [ones_f.b, sq.b], [bk_b])
            act.op(lambda e: e.activation(out=sd[:, 0:N], in_=bk[:, 0:N], func=AF.Sqrt, bias=epst[:, 0:1], scale=1.0 / D),
                   [epst.b], [sd.b, bk_b])

        def rms_b(xt, g, out, N):
            dve.op(lambda e: e.reciprocal(out=sd[:, 0:N], in_=sd[:, 0:N]), [sd.b], [sd.b])
            for c in range(8):
                dve.op(lambda e: e.scalar_tensor_tensor(out=out[:, c, 0:N], in0=xt[:, c, 0:N], scalar=g[:, c:c + 1], in1=sd[:, 0:N],
                                                        op0=ALU.mult, op1=ALU.mult), [xt.b, g.b, sd.b], [out.b])

        def rmsnorm(xt, g, out, N, act_sq=True):
            rms_a(xt, N)
            rms_b(xt, g, out, N)

        if "A" in phases:
            with ExitStack() as ph:
                w_in = T(fw, ph, "w_in", [128, 8, 4608], BF16, dma=True)
                for c in range(8):
                    pool.dma(w_in[:, c, :], I["w_in_ext"][c * 128:(c + 1) * 128, :], [], [w_in.b], w_in.d)
                g_mix = T(fw, ph, "g_mix", [128, 8], F32, dma=True)
                sp.dma(g_mix[:], I["g_mix"][:, :], [], [g_mix.b], g_mix.d)
                qdec = T(fw, ph, "qdec", [128, 4, 512], F32, dma=True)
                sp.dma(qdec[:], I["qdec"][:, :, :], [], [qdec.b], qdec.d)
                xr = Ring([T(fw, ph, "xa%d" % i, [128, 8, 512], F32, dma=True) for i in range(2)])
                cr = Ring([T(fw, ph, "cs%d" % i, [128, 2, 512], F32, dma=True) for i in range(2)])
                hTr = Ring([T(fw, ph, "hT%d" % i, [128, 8, 512], BF16) for i in range(2)])
                t1r = Ring([T(fw, ph, "t1_%d" % i, [128, 512], F32) for i in range(3)])
                t2r = Ring([T(fw, ph, "t2_%d" % i, [128, 512], F32) for i in range(3)])
                sbf = Ring([T(fw, ph, "sbf%d" % i, [128, 512], BF16, dma=True) for i in range(8)])
                sf3 = Ring([T(fw, ph, "sf3_%d" % i, [128, 512], F32, dma=True) for i in range(3)])
                xT_v = I["xT"].rearrange("(c p) n -> p c n", p=128)

                def store(eng_q, dst, st):
                    eng_q.dma(dst, st[:], [st.b], [], st.d)

                loaded = {}

                def load_x(t_):
                    ts_ = slice(t_ * 512, (t_ + 1) * 512)
                    xt = xr.next()
                    sp.dma(xt[:], xT_v[:, :, ts_], [], [xt.b], xt.d)
                    cs_ = cr.next()
                    sp.dma(cs_[:, 0, :], I["cosT2"][:, ts_], [], [cs_.b], cs_.d)
                    sp.dma(cs_[:, 1, :], I["sinT2"][:, ts_], [], [cs_.b], cs_.d)
                    loaded[t_] = (xt, cs_)

                def prep(t_):
                    xt, cs_ = loaded.pop(t_)
                    hT_ = hTr.next()
                    rmsnorm(xt, g_mix, hT_, 512)
                    return hT_, cs_

                load_x(0)
                nxt_prep = prep(0)
                for t in range(8):
                    ts = slice(t * 512, (t + 1) * 512)
                    hT, cs = nxt_prep
                    if t + 1 < 8:
                        load_x(t + 1)

                    def fm_group(col0):
                        bk, bk_b = fw.bank()
                        for c in range(8):
                            pe.op(lambda e: e.matmul(bk[:, :], lhsT=w_in[:, c, col0:col0 + 128], rhs=hT[:, c, :], start=(c == 0), stop=(c == 7)),
                                  [w_in.b, hT.b], [bk_b], inc=(c == 7))
                        return bk, bk_b

                    for kind, c0, c0s, dst in (("q", 0, 3584, QT_ret), ("k", 512, 4096, KT_ret)):
                        for j in range(4):
                            bp, bp_b = fm_group(c0 + j * 128)
                            bs, bs_b = fm_group(c0s + j * 128)
                            t1 = t1r.next()
                            t2 = t2r.next()
                            dve.op(lambda e: e.tensor_tensor(out=t1[:], in0=bp[:, :], in1=cs[:, 0, :], op=ALU.mult), [cs.b], [t1.b, bp_b])
                            dve.op(lambda e: e.tensor_tensor(out=t2[:], in0=bs[:, :], in1=cs[:, 1, :], op=ALU.mult), [cs.b], [t2.b, bs_b])
                            st = sbf.next()
                            if kind == "q":
                                dve.op(lambda e: e.tensor_tensor(out=t1[:], in0=t1[:], in1=t2[:], op=ALU.add), [t1.b, t2.b], [t1.b])
                                act.op(lambda e: e.activation(out=st[:], in_=t1[:], func=AF.Copy), [t1.b], [st.b])
                                store(sp, dst[j * 128:(j + 1) * 128, ts], st)
                                st2 = sbf.next()
                                pool.op(lambda e: e.tensor_tensor(out=st2[:], in0=t1[:], in1=qdec[:, j, :], op=ALU.mult), [t1.b, qdec.b], [st2.b])
                                store(sp, QdT_ret[j * 128:(j + 1) * 128, ts], st2)
                            else:
                                dve.op(lambda e: e.tensor_tensor(out=st[:], in0=t1[:], in1=t2[:], op=ALU.add), [t1.b, t2.b], [st.b])
                                store(sp, dst[j * 128:(j + 1) * 128, ts], st)
                    for c0, dst, scl in ((2048, QT_dif, 0.125), (2560, KT_dif, 1.0)):
                        for j in range(4):
                            bk, bk_b = fm_group(c0 + j * 128)
                            st = sbf.next()
                            act.op(lambda e: e.activation(out=st[:], in_=bk[:, :], func=AF.Copy, scale=scl), [], [st.b, bk_b])
                            store(sp, dst[j * 128:(j + 1) * 128, ts], st)
                    if t + 1 < 8:
                        nxt_prep = prep(t + 1)
                    for sub in range(4):
                        rows = slice(t * 512 + sub * 128, t * 512 + (sub + 1) * 128)
                        for gi, col0 in enumerate((1024, 1536, 3072)):
                            bk, bk_b = fw.bank()
                            for c in range(8):
                                pe.op(lambda e: e.matmul(bk[:, :], lhsT=hT[:, c, sub * 128:(sub + 1) * 128], rhs=w_in[:, c, col0:col0 + 512],
                                                         start=(c == 0), stop=(c == 7)), [w_in.b, hT.b], [bk_b], inc=(c == 7))
                            if gi == 0:
                                st = sbf.next()
                                act.op(lambda e: e.activation(out=st[:], in_=bk[:, :], func=AF.Copy), [], [st.b, bk_b])
                                store(sp, V_ret[rows, :], st)
                            elif gi == 1:
                                st = sf3.next()
                                act.op(lambda e: e.activation(out=st[:], in_=bk[:, :], func=AF.Silu), [], [st.b, bk_b])
                                store(sp, SG[rows, :], st)
                            else:
                                st = sbf.next()
                                dve.op(lambda e: e.tensor_copy(out=st[:], in_=bk[:, :]), [], [st.b, bk_b])
                                store(sp, V_dif[rows, :], st)
                fw.barrier()
        if "C" not in phases:
            issue_wcasts()

        if "B" in phases and "C" in phases:
            PHASE_C(locals(), with_B=True)
        else:
            PHASE_B(locals()) if "B" in phases else None
            PHASE_C(locals()) if "C" in phases else None
        PHASE_DE(locals(), phases) if ("D" in phases or "E" in phases) else None

        fw.barrier()
    return nc


class NS:
    def __init__(self, d):
        self.__dict__.update(d)


class RetentionB:
    def __init__(self, L, ph, bank_ring):
        self.L = L
        fw, I = L.fw, L.I
        self.fw = fw
        sp = fw.sp
        self.banks = bank_ring
        self.decT = T(fw, ph, "decT", [128, 1024], F32, dma=True)
        sp.dma(self.decT[:], I["decT"][:, :], [], [self.decT.b], self.decT.d)
        self.kdec = T(fw, ph, "kdec", [128, 512], F32, dma=True)
        sp.dma(self.kdec[:], I["kdec"][:, :], [], [self.kdec.b], self.kdec.d)
        self.cdec = T(fw, ph, "cdec", [64, 512], F32, dma=True)
        sp.dma(self.cdec[:], I["cdec"][:, :], [], [self.cdec.b], self.cdec.d)
        self.retgn = T(fw, ph, "retgn", [128, 512], F32, dma=True)
        sp.dma(self.retgn[:], I["retgn_b"][:, :], [], [self.retgn.b], self.retgn.d)
        self.Qr = Ring([T(fw, ph, "Qg%d" % i, [64, 8, 512], BF16, dma=True) for i in range(2)])
        self.Qdr = Ring([T(fw, ph, "Qdg%d" % i, [64, 8, 512], BF16, dma=True) for i in range(2)])
        self.Kr = Ring([T(fw, ph, "Kg%d" % i, [64, 8, 512], BF16, dma=True) for i in range(2)])
        self.Vr = Ring([T(fw, ph, "Vg%d" % i, [128, 4, 512], BF16, dma=True) for i in range(2)])
        self.SGr = Ring([T(fw, ph, "SGg%d" % i, [128, 4, 512], F32, dma=True) for i in range(2)])
        self.sTm_r = Ring([T(fw, ph, "sTm%d" % i, [128, 1024], BF16) for i in range(2)])
        self.Kp_r = Ring([T(fw, ph, "Kp%d" % i, [128, 512], BF16) for i in range(2)])
        self.R32 = T(fw, ph, "R32", [64, 512], F32)
        self.Rb_r = Ring([T(fw, ph, "Rb%d" % i, [64, 512], BF16) for i in range(2)])
        self.ro = T(fw, ph, "ro", [128, 512], F32)
        self.sq = T(fw, ph, "rsq", [128, 512], F32)
        self.y = T(fw, ph, "ry", [128, 512], F32)
        self.st8 = T(fw, ph, "st8", [128, 32], F32)
        self.mtok = T(fw, ph, "mtok", [128, 512], BF16)
        self.mst_r = Ring([T(fw, ph, "mst%d" % i, [128, 4, 512], BF16, dma=True) for i in range(2)])
        fw.dve.op(lambda e: e.memset(self.R32[:], 0.0), [], [self.R32.b])
        self.Rb_prev = None
        self.groups = {}
        self.st = {}
        self.mst = None

    def load_group(self, tg):
        L, sp = self.L, self.fw.sp
        ts = slice(tg * 512, (tg + 1) * 512)
        Qg, Qdg, Kg, Vg, SGg = self.Qr.next(), self.Qdr.next(), self.Kr.next(), self.Vr.next(), self.SGr.next()
        sp.dma(Qg[:], L.QT_ret.rearrange("(h d) n -> d h n", d=64)[:, :, ts], [], [Qg.b], Qg.d)
        sp.dma(Qdg[:], L.QdT_ret.rearrange("(h d) n -> d h n", d=64)[:, :, ts], [], [Qdg.b], Qdg.d)
        sp.dma(Kg[:], L.KT_ret.rearrange("(h d) n -> d h n", d=64)[:, :, ts], [], [Kg.b], Kg.d)
        sp.dma(Vg[:], L.V_ret[ts, :].rearrange("(c p) f -> p c f", p=128), [], [Vg.b], Vg.d)
        sp.dma(SGg[:], L.SG[ts, :].rearrange("(c p) f -> p c f", p=128), [], [SGg.b], SGg.d)
        self.groups[tg] = (Qg, Qdg, Kg, Vg, SGg)

    def s1(self, c):
        fw = self.fw
        pe, dve = fw.pe, fw.dve
        tg, cc = divmod(c, 4)
        if cc == 0:
            if tg == 0:
                self.load_group(0)
            if tg + 1 < 8:
                self.load_group(tg + 1)
        Qg, Qdg, Kg, Vg, SGg = self.groups[tg]
        lr = slice(cc * 128, (cc + 1) * 128)
        ident_b = self.L.ident_b
        bA = [self.banks.next(), self.banks.next()]
        for h in range(8):
            bk, bk_b = bA[h // 4]
            pe.op(lambda e: e.matmul(bk[:, (h % 4) * 128:(h % 4 + 1) * 128], lhsT=Kg[:, h, lr], rhs=Qg[:, h, lr], start=True, stop=True),
                  [Kg.b, Qg.b], [bk_b], inc=(h % 4 == 3))
        sTm = self.sTm_r.next()
        for half in range(2):
            bk, bk_b = bA[half]
            dve.op(lambda e: e.tensor_tensor(out=sTm[:, half * 512:(half + 1) * 512], in0=bk[:, :], in1=self.decT[:, half * 512:(half + 1) * 512],
                                             op=ALU.mult), [self.decT.b], [sTm.b, bk_b])
        bT, bT_b = self.banks.next()
        for h in range(8):
            pe.op(lambda e: e.matmul(bT[:, h * 64:(h + 1) * 64], lhsT=Kg[:, h, lr], rhs=ident_b[0:64, 0:64], start=True, stop=True),
                  [Kg.b, ident_b.b], [bT_b], inc=(h == 7))
        Kp = self.Kp_r.next()
        dve.op(lambda e: e.tensor_tensor(out=Kp[:], in0=bT[:, :], in1=self.kdec[:], op=ALU.mult), [self.kdec.b], [Kp.b, bT_b])
        self.st[c] = (sTm, Kp)

    def s2(self, c):
        fw = self.fw
        pe, act, dve, pool = fw.pe, fw.act, fw.dve, fw.pool
        tg, cc = divmod(c, 4)
        Qg, Qdg, Kg, Vg, SGg = self.groups[tg]
        lr = slice(cc * 128, (cc + 1) * 128)
        sTm, Kp = self.st.pop(c)
        R32, ro, sq, y, st8, mtok, epst = self.R32, self.ro, self.sq, self.y, self.st8, self.mtok, self.L.epst
        Rb_prev = self.Rb_prev
        bO, bO_b = self.banks.next()
        for h in range(8):
            hs = slice(h * 64, (h + 1) * 64)
            pe.op(lambda e: e.matmul(bO[:, hs], lhsT=sTm[:, h * 128:(h + 1) * 128], rhs=Vg[:, cc, hs], start=True, stop=(c == 0)),
                  [sTm.b, Vg.b], [bO_b], inc=(c == 0 and h == 7))
            if c > 0:
                pe.op(lambda e: e.matmul(bO[:, hs], lhsT=Qdg[:, h, lr], rhs=Rb_prev[:, hs], start=False, stop=True),
                      [Qdg.b, Rb_prev.b], [bO_b], inc=(h == 7))
        if c < 31:
            bKV, bKV_b = self.banks.next()
            for h in range(8):
                hs = slice(h * 64, (h + 1) * 64)
                pe.op(lambda e: e.matmul(bKV[0:64, hs], lhsT=Kp[:, hs], rhs=Vg[:, cc, hs], start=True, stop=True),
                      [Kp.b, Vg.b], [bKV_b], inc=(h == 7))
            dve.op(lambda e: e.tensor_tensor(out=R32[:], in0=R32[:], in1=self.cdec[:], op=ALU.mult), [R32.b, self.cdec.b], [R32.b])
            dve.op(lambda e: e.tensor_tensor(out=R32[:], in0=R32[:], in1=bKV[0:64, :], op=ALU.add), [R32.b], [R32.b, bKV_b])
            Rb = self.Rb_r.next()
            pool.op(lambda e: e.tensor_copy(out=Rb[:], in_=R32[:]), [R32.b], [Rb.b])
            self.Rb_prev = Rb
        hview = lambda ap: ap.rearrange("p (h e) -> p h e", e=64)
        dve.op(lambda e: e.tensor_copy(out=ro[:], in_=bO[:, :]), [], [ro.b, bO_b])
        dve.op(lambda e: e.tensor_tensor(out=sq[:], in0=ro[:], in1=ro[:], op=ALU.mult), [ro.b], [sq.b])
        dve.op(lambda e: e.tensor_reduce(out=st8[:, 0:8], in_=hview(ro[:]), axis=AX.X, op=ALU.add), [ro.b], [st8.b])
        dve.op(lambda e: e.tensor_reduce(out=st8[:, 8:16], in_=hview(sq[:]), axis=AX.X, op=ALU.add), [sq.b, st8.b], [st8.b])
        dve.op(lambda e: e.tensor_scalar(out=st8[:, 16:24], in0=st8[:, 0:8], scalar1=1.0 / 64, scalar2=None, op0=ALU.mult), [st8.b], [st8.b])
        dve.op(lambda e: e.tensor_tensor(out=st8[:, 24:32], in0=st8[:, 16:24], in1=st8[:, 16:24], op=ALU.mult), [st8.b], [st8.b])
        dve.op(lambda e: e.scalar_tensor_tensor(out=st8[:, 8:16], in0=st8[:, 8:16], scalar=1.0 / 64, in1=st8[:, 24:32],
                                                op0=ALU.mult, op1=ALU.subtract), [st8.b], [st8.b])
        dve.op(lambda e: e.tensor_scalar(out=st8[:, 8:16], in0=st8[:, 8:16], scalar1=EPS, scalar2=None, op0=ALU.add), [st8.b], [st8.b])
        neghalf = self.L.neghalf
        pool.op(lambda e: e.tensor_tensor(out=st8[:, 8:16], in0=st8[:, 8:16], in1=neghalf[:, 0:8], op=ALU.pow), [st8.b, neghalf.b], [st8.b])
        mean_b = st8[:, 16:24].unsqueeze(2).to_broadcast([128, 8, 64])
        rstd_b = st8[:, 8:16].unsqueeze(2).to_broadcast([128, 8, 64])
        dve.op(lambda e: e.tensor_tensor(out=hview(y[:]), in0=hview(ro[:]), in1=mean_b, op=ALU.subtract), [ro.b, st8.b], [y.b])
        dve.op(lambda e: e.tensor_tensor(out=hview(y[:]), in0=hview(y[:]), in1=rstd_b, op=ALU.mult), [y.b, st8.b], [y.b])
        dve.op(lambda e: e.tensor_tensor(out=y[:], in0=y[:], in1=SGg[:, cc, :], op=ALU.mult), [y.b, SGg.b], [y.b])
        dve.op(lambda e: e.tensor_tensor(out=mtok[:], in0=y[:], in1=self.retgn[:], op=ALU.mult), [y.b, self.retgn.b], [mtok.b])

    def s3(self, c):
        fw = self.fw
        pe, dve, sp = fw.pe, fw.dve, fw.sp
        tg, cc = divmod(c, 4)
        lr = slice(cc * 128, (cc + 1) * 128)
        ident_b, mtok = self.L.ident_b, self.mtok
        if cc == 0:
            self.mst = self.mst_r.next()
        mst = self.mst
        bX, bX_b = self.banks.next()
        for fc in range(4):
            pe.op(lambda e: e.matmul(bX[:, fc * 128:(fc + 1) * 128], lhsT=mtok[:, fc * 128:(fc + 1) * 128], rhs=ident_b[:], start=True, stop=True),
                  [mtok.b, ident_b.b], [bX_b], inc=(fc == 3))
        dve.op(lambda e: e.tensor_copy(out=mst[:, :, lr], in_=bX[:, :].rearrange("p (f l) -> p f l", l=128)), [], [mst.b, bX_b])
        if cc == 3:
            ts = slice(tg * 512, (tg + 1) * 512)
            sp.dma(self.L.mixT[0:512, ts].rearrange("(f p) n -> p f n", p=128), mst[:], [mst.b], [], mst.d)


def PHASE_B(L):
    L = NS(L)
    fw = L.fw

    class _AllBanks:
        def next(self):
            return fw.bank()

    with ExitStack() as ph:
        B = RetentionB(L, ph, _AllBanks())
        for c in range(32):
            B.s1(c)
            B.s2(c)
            B.s3(c)
        fw.barrier()


def PHASE_C(L, with_B=False):
    L = NS(L)
    fw, I = L.fw, L.I
    pe, act, dve, pool, sp = fw.pe, fw.act, fw.dve, fw.pool, fw.sp
    ident_b, ident_f, epst = L.ident_b, L.ident_f, L.epst
    with ExitStack() as ph:
        biasT = T(fw, ph, "biasT", [128, 4, 2, 128], F32, dma=True)
        sp.dma(biasT[:], I["biasT"][:, :, :, :], [], [biasT.b], biasT.d)
        cb = T(fw, ph, "cb", [128, 4], F32, dma=True)
        sp.dma(cb[:], I["cbias"][:, :], [], [cb.b], cb.d)
        subln = T(fw, ph, "subln", [128, 128], F32, dma=True)
        sp.dma(subln[:], I["subln_b"][:, :], [], [subln.b], subln.d)
        lamv = T(fw, ph, "lamv", [128, 4, 64], F32, dma=True)
        sp.dma(lamv[:], I["lamvec"][:, :, :], [], [lamv.b], lamv.d)
        bhl = T(fw, ph, "bhl", [128, 2, 4, 2, 128], BF16)
        Vp = T(fw, ph, "Vp", [128, 32, 4, 129], BF16)
        lt = T(fw, ph, "lt", [128, 8], F32)
        zero1 = T(fw, ph, "zero1", [128, 1], F32)
        dve.op(lambda e: e.memset(zero1[:], 0.0), [], [zero1.b])
        prod = T(fw, ph, "lprod", [128, 2, 64], F32)
        dve.op(lambda e: e.tensor_tensor(out=prod[:, 0, :], in0=lamv[:, 0, :], in1=lamv[:, 1, :], op=ALU.mult), [lamv.b], [prod.b])
        dve.op(lambda e: e.tensor_tensor(out=prod[:, 1, :], in0=lamv[:, 2, :], in1=lamv[:, 3, :], op=ALU.mult), [lamv.b, prod.b], [prod.b])
        dve.op(lambda e: e.tensor_reduce(out=lt[:, 0:2], in_=prod[:], axis=AX.X, op=ALU.add), [prod.b], [lt.b])
        act.op(lambda e: e.activation(out=lt[:, 2:4], in_=lt[:, 0:2], func=AF.Exp), [lt.b], [lt.b])
        dve.op(lambda e: e.tensor_tensor(out=lt[:, 4:5], in0=lt[:, 3:4], in1=lt[:, 2:3], op=ALU.subtract), [lt.b], [lt.b])
        dve.op(lambda e: e.tensor_scalar(out=lt[:, 4:5], in0=lt[:, 4:5], scalar1=-LAMBDA_INIT, scalar2=None, op0=ALU.add), [lt.b], [lt.b])
        dve.op(lambda e: e.tensor_scalar(out=subln[:], in0=subln[:], scalar1=1.0 - LAMBDA_INIT, scalar2=None, op0=ALU.mult), [subln.b], [subln.b])
        QTr = Ring([T(fw, ph, "QTh%d" % i, [128, S], BF16, dma=True) for i in range(2)])
        KTr = Ring([T(fw, ph, "KTh%d" % i, [128, S], BF16, dma=True) for i in range(2)])
        heads = {}

        def load_head(h):
            QTh, KTh = QTr.next(), KTr.next()
            sp.dma(QTh[:], L.QT_dif[h * 128:(h + 1) * 128, :], [], [QTh.b], QTh.d)
            sp.dma(KTh[:], L.KT_dif[h * 128:(h + 1) * 128, :], [], [KTh.b], KTh.d)
            heads[h] = (QTh, KTh)

        load_head(0)
        with ExitStack() as tmp:
            btmp = T(fw, tmp, "btmp", [128, 4, 2, 128], F32)
            dve.op(lambda e: e.tensor_copy(out=bhl[:, 0], in_=biasT[:]), [biasT.b], [bhl.b])
            dve.op(lambda e: e.tensor_copy(out=btmp[:], in_=bhl[:, 0]), [bhl.b], [btmp.b])
            dve.op(lambda e: e.tensor_tensor(out=btmp[:], in0=biasT[:], in1=btmp[:], op=ALU.subtract), [biasT.b, btmp.b], [btmp.b])
            dve.op(lambda e: e.tensor_copy(out=bhl[:, 1], in_=btmp[:]), [btmp.b, bhl.b], [bhl.b])
            Vall = T(fw, tmp, "Vall", [128, 32, 512], BF16, dma=True)
            for g in range(4):
                sp.dma(Vall[:, g * 8:(g + 1) * 8, :], L.V_dif[g * 1024:(g + 1) * 1024, :].rearrange("(c p) f -> p c f", p=128), [], [Vall.b], Vall.d)
            pool.op(lambda e: e.memset(Vp[:, :, :, 128:129], 1.0), [], [Vp.b])
            for g in range(4):
                src = Vall[:, g * 8:(g + 1) * 8, :].rearrange("p c (h e) -> p c h e", e=128)
                if g % 2 == 0:
                    dve.op(lambda e: e.tensor_copy(out=Vp[:, g * 8:(g + 1) * 8, :, 0:128], in_=src), [Vall.b], [Vp.b])
                else:
                    act.op(lambda e: e.activation(out=Vp[:, g * 8:(g + 1) * 8, :, 0:128], in_=src, func=AF.Copy), [Vall.b], [Vp.b])
            fw.barrier()
        Pr = Ring([T(fw, ph, "P%d" % i, [128, 512], BF16) for i in range(3)])
        o1 = T(fw, ph, "o1", [128, 4, 128], F32)
        oo = T(fw, ph, "oo", [128, 4, 128], F32)
        osq = T(fw, ph, "osq", [128, 4, 128], F32)
        zz = T(fw, ph, "zz", [128, 16], F32)
        mtk_r = Ring([T(fw, ph, "mtk%d" % i, [128, 4, 128], BF16) for i in range(2)])
        oraw = T(fw, ph, "oraw", [128, 4, 132], F32, parts=4)
        epi_late = []
        mst_r = Ring([T(fw, ph, "cmst%d" % i, [128, 512], BF16, dma=True) for i in range(2)])
        Ob = [fw.banks[i] for i in range(4)]
        Sb = Ring([fw.banks[i] for i in (4, 5, 6)])
        bX, bX_b = fw.banks[7]
        steps = [(h, s, m, kb) for h in range(4) for s in range(8) for m in range(2) for kb in range(4 * s + 4)]

        def emit_qk(step):
            h, s, m, kb = step
            if h not in heads:
                load_head(h)
            if s == 5 and m == 0 and kb == 0 and h + 1 < 4 and (h + 1) not in heads:
                load_head(h + 1)
            QTh, KTh = heads[h]
            pr = slice(m * 64, (m + 1) * 64)
            qb_lo = max(4 * s, kb)
            off = (qb_lo - 4 * s) * 128
            near = []
            if kb >= 4 * s:
                near.append((kb, 0))
            if 4 * s <= kb + 1 <= 4 * s + 3:
                near.append((kb + 1, 1))
            sbk, sbk_b = Sb.next()
            pe.op(lambda e: e.matmul(sbk[:, off:512], lhsT=KTh[pr, kb * 128:(kb + 1) * 128], rhs=QTh[pr, qb_lo * 128:(4 * s + 4) * 128],
                                     start=True, stop=(len(near) == 0)), [KTh.b, QTh.b], [sbk_b], inc=(len(near) == 0))
            for ni, (qb, kind) in enumerate(near):
                o_ = (qb - 4 * s) * 128
                for hl in range(2):
                    last = (ni == len(near) - 1) and hl == 1
                    pe.op(lambda e: e.matmul(sbk[:, o_:o_ + 128], lhsT=ident_b[:], rhs=bhl[:, hl, h, kind, :], start=False, stop=last),
                          [ident_b.b, bhl.b], [sbk_b], inc=last)
            far_lo = max(4 * s, kb + 2)
            P = Pr.next()
            if far_lo <= 4 * s + 3:
                fo = (far_lo - 4 * s) * 128
                act.op(lambda e: e.activation(out=P[:, fo:512], in_=sbk[:, fo:512], func=AF.Exp, bias=cb[:, h:h + 1], scale=1.0),
                       [cb.b], [P.b, sbk_b])
            if near:
                n0 = (near[0][0] - 4 * s) * 128
                n1 = (near[-1][0] - 4 * s + 1) * 128
                act.op(lambda e: e.activation(out=P[:, n0:n1], in_=sbk[:, n0:n1], func=AF.Exp, bias=zero1[:, 0:1], scale=1.0),
                       [zero1.b], [P.b, sbk_b])
            return P

        def emit_pv(step, P):
            h, s, m, kb = step
            qb_lo = max(4 * s, kb)
            for qb in range(qb_lo, 4 * s + 4):
                j = qb - 4 * s
                ob, ob_b = Ob[j]
                pe.op(lambda e: e.matmul(ob[:, 0:129], lhsT=P[:, j * 128:(j + 1) * 128], rhs=Vp[:, kb, h, :], start=(kb == 0), stop=(kb == qb)),
                      [P.b, Vp.b], [ob_b], inc=True)

        def emit_epilogue(h, s, m):
            for j in range(4):
                ob, ob_b = Ob[j]
                act.op(lambda e: e.activation(out=oraw[:, j, 0:129], in_=ob[:, 0:129], func=AF.Copy), [], [oraw.bs[j], ob_b])
            for j in range(4):
                rb = oraw.bs[j]
                if m == 0:
                    dve.op(lambda e: e.reciprocal(out=zz[:, j:j + 1], in_=oraw[:, j, 128:129]), [rb], [zz.b])
                    dve.op(lambda e: e.tensor_scalar(out=o1[:, j, :], in0=oraw[:, j, 0:128], scalar1=zz[:, j:j + 1], scalar2=None, op0=ALU.mult),
                           [rb, zz.b], [o1.b])
                else:
                    dve.op(lambda e: e.reciprocal(out=zz[:, j:j + 1], in_=oraw[:, j, 128:129]), [rb], [zz.b])
                    dve.op(lambda e: e.tensor_tensor(out=zz[:, 4 + j:5 + j], in0=zz[:, j:j + 1], in1=L_lt(lt), op=ALU.mult), [zz.b, lt.b], [zz.b])
                    dve.op(lambda e: e.scalar_tensor_tensor(out=oo[:, j, :], in0=oraw[:, j, 0:128], scalar=zz[:, 4 + j:5 + j], in1=o1[:, j, :],
                                                            op0=ALU.mult, op1=ALU.add), [rb, zz.b, o1.b], [oo.b])
            if m == 0:
                return
            dve.op(lambda e: e.tensor_tensor(out=osq[:], in0=oo[:], in1=oo[:], op=ALU.mult), [oo.b], [osq.b])
            mtk = mtk_r.next()
            dve.op(lambda e: e.tensor_reduce(out=zz[:, 8:12], in_=osq[:], axis=AX.X, op=ALU.add), [osq.b, zz.b], [zz.b])
            dve.op(lambda e: e.tensor_scalar(out=zz[:, 12:16], in0=zz[:, 8:12], scalar1=1.0 / 128, scalar2=EPS, op0=ALU.mult, op1=ALU.add), [zz.b], [zz.b])
            pool.op(lambda e: e.tensor_tensor(out=zz[:, 12:16], in0=zz[:, 12:16], in1=L.neghalf[:, 0:4], op=ALU.pow), [zz.b, L.neghalf.b], [zz.b])
            for j in range(4):
                dve.op(lambda e: e.scalar_tensor_tensor(out=mtk[:, j, :], in0=oo[:, j, :], scalar=zz[:, 12 + j:13 + j], in1=subln[:],
                                                        op0=ALU.mult, op1=ALU.mult), [oo.b, zz.b, subln.b], [mtk.b])

            def late(mtk=mtk, h=h, s=s):
                for j in range(4):
                    pe.op(lambda e: e.matmul(bX[:, j * 128:(j + 1) * 128], lhsT=mtk[:, j, :], rhs=ident_b[:], start=True, stop=True),
                          [mtk.b, ident_b.b], [bX_b], inc=(j == 3))
                mst = mst_r.next()
                dve.op(lambda e: e.tensor_copy(out=mst[:], in_=bX[:, :]), [], [mst.b, bX_b])
                sp.dma(L.mixT[(4 + h) * 128:(5 + h) * 128, s * 512:(s + 1) * 512], mst[:], [mst.b], [], mst.d)

            epi_late.append([20, late])

        Bsched = {}
        if with_B:
            RB = RetentionB(L, ph, Ring([fw.banks[i] for i in (4, 5, 6, 7)]))
            per = len(steps) // 32
            for c in range(32):
                Bsched.setdefault(c * per, []).append(lambda c=c: RB.s1(c))
                Bsched.setdefault(c * per + per // 6, []).append(lambda c=c: RB.s2(c))
                Bsched.setdefault(c * per + (5 * per) // 6, []).append(lambda c=c: RB.s3(c))
        LOOK = 2
        pendP = [emit_qk(steps[i]) for i in range(LOOK)]
        for k, step in enumerate(steps):
            if k % 16 == 8:
                L.issue_wcasts(1)
            for fn in Bsched.get(k, ()):
                fn()
            for item in list(epi_late):
                item[0] -= 1
                if item[0] <= 0:
                    epi_late.remove(item)
                    item[1]()
            if k + LOOK < len(steps):
                pendP.append(emit_qk(steps[k + LOOK]))
            curP = pendP.pop(0)
            emit_pv(step, curP)
            h, s, m, kb = step
            if kb == 4 * s + 3:
                emit_epilogue(h, s, m)
        for item in epi_late:
            item[1]()
        L.issue_wcasts()
        fw.barrier()


def L_lt(lt):
    return lt[:, 4:5]


def PHASE_DE(L, phases):
    L = NS(L)
    fw, I = L.fw, L.I
    pe, act, dve, pool, sp = fw.pe, fw.act, fw.dve, fw.pool, fw.sp
    ident_b, ident_f, epst, rmsnorm = L.ident_b, L.ident_f, L.epst, L.rmsnorm
    U32 = mybir.dt.uint32
    NT = 256
    with ExitStack() as outer:
        if "D" in phases:
            with ExitStack() as ph:
                wout = T(fw, ph, "wout", [128, 8, 1024], BF16, dma=True)
                wpq = T(fw, ph, "wpq", [128, 8, 2048], BF16, dma=True)
                keys = T(fw, ph, "keys", [128, 16, 128], BF16, dma=True)
                if "C" in phases:
                    sp.dma(wout[:], L.wout_bf.rearrange("(c p) f -> p c f", p=128), [], [wout.b], wout.d)
                    sp.dma(wpq[:], L.wpq_bf.rearrange("(c p) f -> p c f", p=128), [], [wpq.b], wpq.d)
                    sp.dma(keys[:], L.keys_bf[:, :, :], [], [keys.b], keys.d)
                else:
                    for c in range(8):
                        pool.dma(wout[:, c, :], I["w_out"][c * 128:(c + 1) * 128, :], [], [wout.b], wout.d)
                        pool.dma(wpq[:, c, :], I["w_pq"][c * 128:(c + 1) * 128, :], [], [wpq.b], wpq.d)
                    pool.dma(keys[:], I["keysT"][:, :, :], [], [keys.b], keys.d)
                g_ffn = T(fw, ph, "g_ffn", [128, 8], F32, dma=True)
                sp.dma(g_ffn[:], I["g_ffn"][:, :], [], [g_ffn.b], g_ffn.d)
                io16 = T(fw, ph, "io16", [128, 8, 16, 16], F32, dma=True)
                sp.dma(io16[:], I["iota16r"].rearrange("p (h k a) -> p h k a", h=8, k=16), [], [io16.b], io16.d)
                xr = Ring([T(fw, ph, "xd%d" % i, [128, 8, NT], F32, dma=True) for i in range(2)])
                mr = Ring([T(fw, ph, "md%d" % i, [128, 8, NT], BF16, dma=True) for i in range(2)])
                x1r = Ring([T(fw, ph, "x1d%d" % i, [128, 8, NT], F32, dma=True) for i in range(2)])
                h2r = Ring([T(fw, ph, "h2d%d" % i, [128, 8, NT], BF16, dma=True) for i in range(2)])
                qTr = Ring([T(fw, ph, "qT%d" % i, [128, 16, NT], BF16) for i in range(2)])
                scr_ = Ring([T(fw, ph, "sc%d" % i, [128, 16, 128], F32) for i in range(2)])
                sc2 = T(fw, ph, "sc2", [128, 16, 128], F32, parts=16)
                mx = T(fw, ph, "mx", [128, 16, 16], F32, parts=16)
                idx = T(fw, ph, "idx", [128, 16, 16], U32, parts=16)
                idxf = T(fw, ph, "idxf", [128, 16, 16], F32)
                cand = T(fw, ph, "cand", [128, 8, 112], F32)
                cand2 = T(fw, ph, "cand2", [128, 8, 112], F32, parts=8)
                m2 = T(fw, ph, "m2", [128, 8, 16], F32, parts=8)
                pos = T(fw, ph, "pos", [128, 8, 16], U32, parts=8)
                pa = T(fw, ph, "pa", [128, 4, 8, 16], U32)
                paf = T(fw, ph, "paf", [128, 4, 8, 16], F32)
                abf = T(fw, ph, "abf", [128, 4, 8, 16], F32)
                oh = T(fw, ph, "oh", [128, 8, 16, 16], F32)
                IJG = T(fw, ph, "IJG", [128, 3, 128], F32, parts=3)
                gz = T(fw, ph, "gz", [128, 16], F32)
                lstg = Ring([T(fw, ph, "lstg%d" % i, [128, 3, 128], BF16, dma=True) for i in range(2)])
                xT_v = I["xT"].rearrange("(c p) n -> p c n", p=128)
                mT_v = L.mixT.rearrange("(c p) n -> p c n", p=128)
                x1_v = L.x1T.rearrange("(c p) n -> p c n", p=128)
                h2_v = L.h2T_d.rearrange("(c p) n -> p c n", p=128)

                pst = {}

                def load_p(t):
                    ts = slice(t * NT, (t + 1) * NT)
                    xt, mt = xr.next(), mr.next()
                    sp.dma(xt[:], xT_v[:, :, ts], [], [xt.b], xt.d)
                    sp.dma(mt[:], mT_v[:, :, ts], [], [mt.b], mt.d)
                    pst[t] = dict(xt=xt, mt=mt, x1=x1r.next(), h2=h2r.next(), qT=qTr.next())

                def p1(t):
                    d = pst[t]
                    ts = slice(t * NT, (t + 1) * NT)
                    xt, mt, x1 = d["xt"], d["mt"], d["x1"]
                    for fc in range(8):
                        bk, bk_b = fw.bank()
                        for mc in range(8):
                            pe.op(lambda e: e.matmul(bk[:, 0:NT], lhsT=wout[:, mc, fc * 128:(fc + 1) * 128], rhs=mt[:, mc, :], start=(mc == 0), stop=(mc == 7)),
                                  [wout.b, mt.b], [bk_b], inc=(mc == 7))
                        dve.op(lambda e: e.tensor_tensor(out=x1[:, fc, :], in0=bk[:, 0:NT], in1=xt[:, fc, :], op=ALU.add), [xt.b], [x1.b, bk_b])
                    sp.dma(x1_v[:, :, ts], x1[:], [x1.b], [], x1.d)
                    L.rms_a(x1, NT)

                def p2(t):
                    d = pst.pop(t)
                    ts = slice(t * NT, (t + 1) * NT)
                    x1, h2, qT = d["x1"], d["h2"], d["qT"]
                    L.rms_b(x1, g_ffn, h2, NT)
                    sp.dma(h2_v[:, :, ts], h2[:], [h2.b], [], h2.d)
                    for hp in range(16):
                        bk, bk_b = fw.bank()
                        for c in range(8):
                            pe.op(lambda e: e.matmul(bk[:, 0:NT], lhsT=wpq[:, c, hp * 128:(hp + 1) * 128], rhs=h2[:, c, :], start=(c == 0), stop=(c == 7)),
                                  [wpq.b, h2.b], [bk_b], inc=(c == 7))
                        act.op(lambda e: e.activation(out=qT[:, hp, :], in_=bk[:, 0:NT], func=AF.Copy), [], [qT.b, bk_b])
                    return qT

                def stage_s(qT, sub):
                    nsl = slice(sub * 128, (sub + 1) * 128)
                    sc = scr_.next()
                    for q4 in range(4):
                        bk, bk_b = fw.bank()
                        for r in range(4):
                            hp = q4 * 4 + r
                            pe.op(lambda e: e.matmul(bk[:, r * 128:(r + 1) * 128], lhsT=qT[:, hp, nsl], rhs=keys[:, hp, :], start=True, stop=True),
                                  [qT.b, keys.b], [bk_b], inc=(r == 3))
                        act.op(lambda e: e.activation(out=sc[:, q4 * 4:(q4 + 1) * 4, :], in_=bk[:, :].rearrange("p (r k) -> p r k", k=128), func=AF.Copy),
                               [], [sc.b, bk_b])
                    return sc

                def k_a(sc):
                    for g in range(16):
                        dve.op(lambda e: e.max(out=mx[:, g, 0:8], in_=sc[:, g, :]), [sc.b], [mx.bs[g]])
                    for g in range(16):
                        dve.op(lambda e: e.max_index(out=idx[:, g, 0:8], in_max=mx[:, g, 0:8], in_values=sc[:, g, :]), [sc.b, mx.bs[g]], [idx.bs[g]])
                    for g in range(16):
                        dve.op(lambda e: e.match_replace(out=sc2[:, g, :], in_to_replace=mx[:, g, 0:8], in_values=sc[:, g, :], imm_value=-1e30),
                               [sc.b, mx.bs[g]], [sc2.bs[g]])
                    for g in range(16):
                        dve.op(lambda e: e.max(out=mx[:, g, 8:16], in_=sc2[:, g, :]), [sc2.bs[g]], [mx.bs[g]])
                    for g in range(16):
                        dve.op(lambda e: e.max_index(out=idx[:, g, 8:16], in_max=mx[:, g, 8:16], in_values=sc2[:, g, :]), [sc2.bs[g], mx.bs[g]], [idx.bs[g]])
                    dve.op(lambda e: e.tensor_copy(out=idxf[:], in_=idx[:]), idx.bs, [idxf.b])

                def k_b(t, sub):
                    ncol = slice(t * NT + sub * 128, t * NT + (sub + 1) * 128)
                    mxv = mx[:].rearrange("p (h two) k -> p h two k", two=2)
                    idv = idxf[:].rearrange("p (h two) k -> p h two k", two=2)
                    c1 = cand[:, :, 0:64].rearrange("p h (a b) -> p h a b", b=16)
                    dve.op(lambda e: e.tensor_tensor(out=c1, in0=mxv[:, :, 0, 0:4].unsqueeze(3).to_broadcast([128, 8, 4, 16]),
                                                     in1=mxv[:, :, 1, :].unsqueeze(2).to_broadcast([128, 8, 4, 16]), op=ALU.add), mx.bs, [cand.b])
                    c2 = cand[:, :, 64:112].rearrange("p h (a b) -> p h a b", b=4)
                    dve.op(lambda e: e.tensor_tensor(out=c2, in0=mxv[:, :, 0, 4:16].unsqueeze(3).to_broadcast([128, 8, 12, 4]),
                                                     in1=mxv[:, :, 1, 0:4].unsqueeze(2).to_broadcast([128, 8, 12, 4]), op=ALU.add), mx.bs, [cand.b])
                    for h in range(8):
                        dve.op(lambda e: e.max(out=m2[:, h, 0:8], in_=cand[:, h, :]), [cand.b], [m2.bs[h]])
                    for h in range(8):
                        dve.op(lambda e: e.max_index(out=pos[:, h, 0:8], in_max=m2[:, h, 0:8], in_values=cand[:, h, :]), [cand.b, m2.bs[h]], [pos.bs[h]])
                    for h in range(8):
                        dve.op(lambda e: e.match_replace(out=cand2[:, h, :], in_to_replace=m2[:, h, 0:8], in_values=cand[:, h, :], imm_value=-1e30),
                               [cand.b, m2.bs[h]], [cand2.bs[h]])
                    for h in range(8):
                        dve.op(lambda e: e.max(out=m2[:, h, 8:16], in_=cand2[:, h, :]), [cand2.bs[h]], [m2.bs[h]])
                    for h in range(8):
                        dve.op(lambda e: e.max_index(out=pos[:, h, 8:16], in_max=m2[:, h, 8:16], in_values=cand2[:, h, :]), [cand2.bs[h], m2.bs[h]], [pos.bs[h]])
                    dve.op(lambda e: e.tensor_single_scalar(out=pa[:, 0], in_=pos[:], scalar=4, op=ALU.logical_shift_right), pos.bs, [pa.b])
                    dve.op(lambda e: e.tensor_single_scalar(out=pa[:, 1], in_=pos[:], scalar=15, op=ALU.bitwise_and), pos.bs + [pa.b], [pa.b])
                    dve.op(lambda e: e.tensor_single_scalar(out=pa[:, 2], in_=pos[:], scalar=2, op=ALU.logical_shift_right), pos.bs + [pa.b], [pa.b])
                    dve.op(lambda e: e.tensor_single_scalar(out=pa[:, 3], in_=pos[:], scalar=3, op=ALU.bitwise_and), pos.bs + [pa.b], [pa.b])
                    dve.op(lambda e: e.tensor_copy(out=paf[:], in_=pa[:]), [pa.b], [paf.b])
                    dve.op(lambda e: e.tensor_single_scalar(out=abf[:, 2], in_=paf[:, 0], scalar=4.0, op=ALU.is_ge), [paf.b], [abf.b])
                    dve.op(lambda e: e.scalar_tensor_tensor(out=abf[:, 0], in0=paf[:, 2], scalar=-12.0, in1=paf[:, 0], op0=ALU.add, op1=ALU.subtract),
                           [paf.b, abf.b], [abf.b])
                    dve.op(lambda e: e.tensor_tensor(out=abf[:, 1], in0=paf[:, 3], in1=paf[:, 1], op=ALU.subtract), [paf.b, abf.b], [abf.b])
                    dve.op(lambda e: e.tensor_tensor(out=abf[:, 0:2], in0=abf[:, 0:2], in1=abf[:, 2:3].to_broadcast([128, 2, 8, 16]), op=ALU.mult),
                           [abf.b], [abf.b])
                    dve.op(lambda e: e.tensor_tensor(out=abf[:, 0:2], in0=abf[:, 0:2], in1=paf[:, 0:2], op=ALU.add), [abf.b, paf.b], [abf.b])
                    for w in range(2):
                        dve.op(lambda e: e.tensor_tensor(out=oh[:], in0=io16[:], in1=abf[:, w].unsqueeze(3).to_broadcast([128, 8, 16, 16]), op=ALU.is_equal),
                               [io16.b, abf.b], [oh.b])
                        dve.op(lambda e: e.tensor_tensor(out=oh[:], in0=oh[:], in1=idv[:, :, w, :].unsqueeze(2).to_broadcast([128, 8, 16, 16]), op=ALU.mult),
                               [oh.b, idxf.b], [oh.b])
                        dve.op(lambda e: e.tensor_reduce(out=IJG[:, w, :].rearrange("p (h k) -> p h k", k=16), in_=oh[:], axis=AX.X, op=ALU.add),
                               [oh.b], [IJG.bs[w]])
                    g3 = IJG[:, 2, :].rearrange("p (h k) -> p h k", k=16)
                    gb = IJG.bs[2]
                    dve.op(lambda e: e.tensor_tensor(out=g3, in0=m2[:], in1=m2[:, :, 0:1].to_broadcast([128, 8, 16]), op=ALU.subtract), m2.bs, [gb])
                    act.op(lambda e: e.activation(out=IJG[:, 2, :], in_=IJG[:, 2, :], func=AF.Exp), [gb], [gb])
                    dve.op(lambda e: e.tensor_reduce(out=gz[:, 0:8], in_=g3, axis=AX.X, op=ALU.add), [gb], [gz.b])
                    dve.op(lambda e: e.reciprocal(out=gz[:, 8:16], in_=gz[:, 0:8]), [gz.b], [gz.b])
                    dve.op(lambda e: e.tensor_tensor(out=g3, in0=g3, in1=gz[:, 8:16].unsqueeze(2).to_broadcast([128, 8, 16]), op=ALU.mult), [gb, gz.b], [gb])
                    bk, bk_b = fw.bank()
                    for a in range(3):
                        pe.op(lambda e: e.matmul(bk[:, a * 128:(a + 1) * 128], lhsT=IJG[:, a, :], rhs=ident_f[:], start=True, stop=True),
                              [IJG.bs[a], ident_f.b], [bk_b], inc=(a == 2))
                    lg = lstg.next()
                    act.op(lambda e: e.activation(out=lg[:], in_=bk[:, 0:384].rearrange("p (a n) -> p a n", n=128), func=AF.Copy),
                           [], [lg.b, bk_b])
                    sp.dma(L.LSTd[:, :, ncol], lg[:], [lg.b], [], lg.d)

                NTL = S // NT
                load_p(0)
                p1(0)
                qn = p2(0)
                for t in range(NTL):
                    qc = qn
                    if t + 1 < NTL:
                        load_p(t + 1)
                    s0 = stage_s(qc, 0)
                    s1 = stage_s(qc, 1)
                    k_a(s0)
                    if t + 1 < NTL:
                        p1(t + 1)
                    k_b(t, 0)
                    k_a(s1)
                    if t + 1 < NTL:
                        qn = p2(t + 1)
                    k_b(t, 1)
                fw.barrier()
        if "E" in phases:
            with ExitStack() as ph:
                g_fin = T(fw, ph, "g_fin", [128, 8], F32, dma=True)
                sp.dma(g_fin[:], I["g_fin"][:, :], [], [g_fin.b], g_fin.d)
                iof = T(fw, ph, "iof", [128, 128], F32, dma=True)
                sp.dma(iof[:], I["iota"][:, :], [], [iof.b], iof.d)
                NG = 8
                NTILE = S // NT
                io3 = T(fw, ph, "io3", [128, NG, 128], BF16)
                for r in range(NG):
                    dve.op(lambda e: e.tensor_copy(out=io3[:, r, :], in_=iof[:]), [iof.b], [io3.b])
                H = [T(fw, ph, "GTh%d" % i, [128, NT, 64], BF16) for i in range(2)]
                Ar = Ring([T(fw, ph, "A%d" % i, [128, NG, 64], BF16) for i in range(3)])
                Br = Ring([T(fw, ph, "B%d" % i, [128, NG, 128], BF16) for i in range(3)])
                lstr = Ring([T(fw, ph, "lst%d" % i, [128, 3, NT], BF16, dma=True) for i in range(3)])
                wdr = Ring([T(fw, ph, "wd%d" % i, [128, 8, 512], BF16, dma=True) for i in range(2)])
                wur = Ring([T(fw, ph, "wu%d" % i, [128, 4, 1024], BF16, dma=True) for i in range(3)])
                h2r = Ring([T(fw, ph, "h2e%d" % i, [128, 8, NT], BF16, dma=True) for i in range(2)])
                x1r = Ring([T(fw, ph, "x1e%d" % i, [128, 8, NT], F32, dma=True) for i in range(2)])
                ost = T(fw, ph, "ost", [128, 8, NT], F32, dma=True)
                LAG = 3
                gar = Ring([T(fw, ph, "ga%d" % i, [128, NT], F32) for i in range(3)])
                awr = Ring([T(fw, ph, "aw%d" % i, [128, NT], BF16) for i in range(LAG + 3)])
                x1_v = L.x1T.rearrange("(c p) n -> p c n", p=128)
                h2_v = L.h2T_d.rearrange("(c p) n -> p c n", p=128)
                oT_v = L.outT.rearrange("(c p) n -> p c n", p=128)
                Ob = [fw.banks[i] for i in range(4)]
                Ab = Ring([fw.banks[i] for i in (4, 5)])
                Gb = Ring([fw.banks[i] for i in (6, 7)])

                lists = {}

                def load_lists(t_):
                    lt_ = lstr.next()
                    sp.dma(lt_[:], L.LSTd[:, :, t_ * NT:(t_ + 1) * NT], [], [lt_.b], lt_.d)
                    lists[t_] = lt_

                units = [(0, 0, g) for g in range(NT // NG)]
                for t_ in range(NTILE):
                    units += [(t_, 1, g) for g in range(NT // NG)]
                    if t_ + 1 < NTILE:
                        units += [(t_ + 1, 0, g) for g in range(NT // NG)]
                ust = {"next1": 0, "next2": 0, "ops": {}}

                def g_stage1(u):
                    t_, half, grp = units[u]
                    LT = lists[t_]
                    n0 = grp * NG
                    A, B = Ar.next(), Br.next()
                    dve.op(lambda e: e.tensor_tensor(out=A[:], in0=io3[:, :, half * 64:(half + 1) * 64],
                                                     in1=LT[:, 0, n0:n0 + NG].unsqueeze(2).to_broadcast([128, NG, 64]), op=ALU.is_equal),
                           [io3.b, LT.b], [A.b])
                    pool.op(lambda e: e.tensor_tensor(out=A[:], in0=A[:], in1=LT[:, 2, n0:n0 + NG].unsqueeze(2).to_broadcast([128, NG, 64]), op=ALU.mult),
                            [A.b, LT.b], [A.b])
                    dve.op(lambda e: e.tensor_tensor(out=B[:], in0=io3[:], in1=LT[:, 1, n0:n0 + NG].unsqueeze(2).to_broadcast([128, NG, 128]), op=ALU.is_equal),
                           [io3.b, LT.b], [B.b])
                    ust["ops"][u] = (A, B)

                def g_stage2(u):
                    t_, half, grp = units[u]
                    A, B = ust["ops"].pop(u)
                    bk, bk_b = Gb.next()
                    for r in range(NG):
                        pe.op(lambda e: e.matmul(bk[:, r * 64:(r + 1) * 64], lhsT=B[:, r, :], rhs=A[:, r, :], start=True, stop=True),
                              [A.b, B.b], [bk_b], inc=(r == NG - 1))
                    nl = grp * NG
                    G = H[half]
                    act.op(lambda e: e.activation(out=G[:, nl:nl + NG, :], in_=bk[:, :].rearrange("j (n i) -> j n i", i=64), func=AF.Copy),
                           [], [G.b, bk_b])

                def g_step():
                    if ust["next2"] >= len(units):
                        return
                    while ust["next1"] <= min(ust["next2"] + 1, len(units) - 1) and units[ust["next1"]][0] in lists:
                        g_stage1(ust["next1"])
                        ust["next1"] += 1
                    g_stage2(ust["next2"])
                    ust["next2"] += 1

                load_lists(0)
                for _ in range(NT // NG):
                    g_step()

                tiles = {}

                def load_tile(t_):
                    h2_, x1_ = h2r.next(), x1r.next()
                    ts_ = slice(t_ * NT, (t_ + 1) * NT)
                    sp.dma(h2_[:], h2_v[:, :, ts_], [], [h2_.b], h2_.d)
                    sp.dma(x1_[:], x1_v[:, :, ts_], [], [x1_.b], x1_.d)
                    tiles[t_] = (h2_, x1_)

                wts = {}

                def load_w(gi):
                    ig = gi % 32
                    wd_, wu_ = wdr.next(), wur.next()
                    sp.dma(wd_[:], L.wdT_bf[ig], [], [wd_.b], wd_.d)
                    sp.dma(wu_[:], L.wup_bf[ig], [], [wu_.b], wu_.d)
                    wts[gi] = (wd_, wu_)

                deferred = [None]
                load_tile(0)
                load_w(0)
                for t in range(NTILE):
                    ts = slice(t * NT, (t + 1) * NT)
                    h2, x1 = tiles.pop(t)
                    if t + 1 < NTILE:
                        load_lists(t + 1)
                        if deferred[0] is None:
                            load_tile(t + 1)
                    for j in range(4):
                        ob, ob_b = Ob[j]
                        dve.op(lambda e: e.memset(ob[:, :], 0.0), [], [ob_b])
                    pend = []

                    def emit_up(item):
                        p_aw, p_wu, p_ii = item
                        for dc in range(8):
                            ob, ob_b = Ob[dc // 2]
                            pe.op(lambda e: e.matmul(ob[:, (dc % 2) * NT:(dc % 2 + 1) * NT], lhsT=p_wu[:, p_ii, dc * 128:(dc + 1) * 128], rhs=p_aw[:],
                                                     start=False, stop=False, skip_group_check=True), [p_wu.b, p_aw.b], [ob_b], inc=(dc == 7))

                    for i in range(128):
                        if i == 6 and deferred[0] is not None:
                            deferred[0]()
                            deferred[0] = None
                            if t + 1 < NTILE:
                                load_tile(t + 1)
                        ii = i % 4
                        gi = t * 32 + i // 4
                        if ii == 0 and gi + 1 < NTILE * 32:
                            load_w(gi + 1)
                        wd, wu = wts[gi]
                        if i % 2 == 0:
                            g_step()
                        ab, ab_b = Ab.next()
                        for c in range(8):
                            pe.op(lambda e: e.matmul(ab[:, 0:NT], lhsT=wd[:, c, ii * 128:(ii + 1) * 128], rhs=h2[:, c, :], start=(c == 0), stop=(c == 7)),
                                  [wd.b, h2.b], [ab_b], inc=(c == 7))
                        ga, aw = gar.next(), awr.next()
                        act.op(lambda e: e.activation(out=ga[:], in_=ab[:, 0:NT], func=AF.Gelu), [], [ga.b, ab_b])
                        G = H[i // 64]
                        dve.op(lambda e: e.tensor_tensor(out=aw[:], in0=ga[:], in1=G[:, :, i % 64], op=ALU.mult), [ga.b, G.b], [aw.b])
                        pend.append((aw, wu, ii))
                        if len(pend) > LAG:
                            emit_up(pend.pop(0))
                        if ii == 3:
                            wts.pop(gi - 1, None)
                    while pend:
                        emit_up(pend.pop(0))
                    for dc in range(8):
                        ob, ob_b = Ob[dc // 2]
                        dve.op(lambda e: e.tensor_tensor(out=x1[:, dc, :], in0=ob[:, (dc % 2) * NT:(dc % 2 + 1) * NT], in1=x1[:, dc, :], op=ALU.add),
                               [x1.b], [x1.b, ob_b])

                    def fin(x1=x1, ts=ts):
                        L.rms_a(x1, NT, bank=Ab.next())
                        L.rms_b(x1, g_fin, ost, NT)
                        sp.dma(oT_v[:, :, ts], ost[:], [ost.b], [], ost.d)

                    deferred[0] = fin
                deferred[0]()
                fw.barrier()


def kernel(**inputs):
    inp = {k: np.asarray(v) for k, v in inputs.items()}
    shared = _host_prep(inp)
    nc = build_program()
    x = np.asarray(inp["x"], np.float32)
    in_maps = []
    for c in range(8):
        m = dict(shared)
        m["xT"] = np.ascontiguousarray(x[c].T)
        in_maps.append(m)
    res = run_bass_kernel_spmd(nc, in_maps, core_ids=list(range(8)))
    out = np.stack([np.ascontiguousarray(np.asarray(res.results[c]["outT"]).T) for c in range(8)], axis=0)
    return out.astype(np.float32)
```

```python
import math
import os
from contextlib import ExitStack

import numpy as np
import concourse.bass as bass
import concourse.mybir as mybir
from concourse.bass_utils import run_bass_kernel_spmd

F32 = mybir.dt.float32
BF16 = mybir.dt.bfloat16
AF = mybir.ActivationFunctionType
ALU = mybir.AluOpType
AX = mybir.AxisListType

S = 4096
D = 1024
NC8 = 8
EPS = 1e-6
LAMBDA_INIT = 0.8 - 0.6 * math.exp(-0.3 * 0)
NEG = -30000.0


class TB:
    __slots__ = ("name", "w", "r")

    def __init__(self, name):
        self.name = name
        self.w = {}
        self.r = {}


class DSem:
    __slots__ = ("sem", "total")

    def __init__(self, sem):
        self.sem = sem
        self.total = 0


class Eng:
    def __init__(self, eng, sem, name, is_pe=False):
        self.eng = eng
        self.sem = sem
        self.name = name
        self.is_pe = is_pe
        self.cnt = 0
        self.seen = {}
        self.pend_r = []
        self.pend_w = []

    def _wait(self, evs):
        need = {}
        for so, v in evs:
            if isinstance(so, DSem):
                key, val = so.sem, so.total
            else:
                key, val = so.sem, v
            if need.get(key, 0) < val:
                need[key] = val
        for key, val in need.items():
            if self.seen.get(key, 0) < val:
                self.eng.wait_ge(key, val)
                self.seen[key] = val

    def _deps(self, reads, writes, own_dsem=None):
        evs = []
        for b in reads:
            for so, v in b.w.items():
                if so is self and self.is_pe:
                    continue
                evs.append((so, v))
        for b in writes:
            for so, v in b.w.items():
                if so is own_dsem or (so is self and self.is_pe):
                    continue
                evs.append((so, v))
            for so, v in b.r.items():
                if so is self and self.is_pe:
                    continue
                evs.append((so, v))
        return evs

    def op(self, fn, reads=(), writes=(), inc=True):
        self._wait(self._deps(reads, writes))
        ins = fn(self.eng)
        self.pend_r.extend(reads)
        self.pend_w.extend(writes)
        if inc:
            self.cnt += 1
            ins.then_inc(self.sem, 1)
            for b in self.pend_r:
                b.r[self] = self.cnt
            for b in self.pend_w:
                b.w = {self: self.cnt}
                b.r = {}
            self.pend_r = []
            self.pend_w = []
        return ins

    def dma(self, out, in_, reads, writes, dsem):
        assert not self.pend_r and not self.pend_w
        self._wait(self._deps(reads, writes, own_dsem=dsem))
        ins = self.eng.dma_start(out=out, in_=in_)
        dsem.total += 16
        ins.then_inc(dsem.sem, 16)
        for b in reads:
            b.r[dsem] = dsem.total
        for b in writes:
            b.w = {dsem: dsem.total}
            b.r = {}
        return ins


class FW:
    def __init__(self, nc, es):
        self.nc = nc
        self.es = es
        mk = lambda n: es.enter_context(nc.semaphore(n))
        self.pe = Eng(nc.tensor, mk("s_pe"), "pe", is_pe=True)
        self.act = Eng(nc.scalar, mk("s_act"), "act")
        self.dve = Eng(nc.vector, mk("s_dve"), "dve")
        self.pool = Eng(nc.gpsimd, mk("s_pool"), "pool")
        self.sp = Eng(nc.sync, mk("s_sp"), "sp")
        self.engs = [self.pe, self.act, self.dve, self.pool, self.sp]
        self.dsems = []
        self.nsem = 0
        self.banks = []
        self.bank_i = 0

    def dsem(self, name=None):
        self.nsem += 1
        d = DSem(self.es.enter_context(self.nc.semaphore(name or ("d%d" % self.nsem))))
        self.dsems.append(d)
        return d

    def barrier(self):
        for e in self.engs:
            evs = [(o, o.cnt) for o in self.engs if o is not e and o.cnt > 0]
            evs += [(d, d.total) for d in self.dsems if d.total > 0]
            e._wait(evs)

    def sb(self, stack, name, shape, dtype):
        t = stack.enter_context(self.nc.sbuf_tensor("sb_" + name, list(shape), dtype))
        return t, TB(name)

    def init_psum(self, stack):
        for i in range(8):
            t = stack.enter_context(self.nc.psum_tensor("bank%d" % i, [128, 512], F32))
            self.banks.append((t, TB("bank%d" % i)))

    def bank(self):
        b = self.banks[self.bank_i % 8]
        self.bank_i += 1
        return b


class T:
    def __init__(self, fw, stack, name, shape, dtype, dma=False, parts=0):
        self.t, self.b = fw.sb(stack, name, shape, dtype)
        self.d = fw.dsem("ds_" + name) if dma else None
        self.bs = [TB("%s.%d" % (name, i)) for i in range(parts)]

    def __getitem__(self, k):
        return self.t[k]


class Ring:
    def __init__(self, tiles):
        self.tiles = tiles
        self.i = 0

    def next(self):
        t = self.tiles[self.i % len(self.tiles)]
        self.i += 1
        return t


def _t5_bucket_np(n):
    n = np.maximum(n, 0)
    max_exact = 16
    nf = np.maximum(n, 1).astype(np.float32) / np.float32(max_exact)
    large = max_exact + (np.log(nf).astype(np.float32) / np.float32(math.log(128 / max_exact)) * np.float32(32 - max_exact)).astype(np.int32)
    large = np.minimum(large, 31)
    return np.where(n < max_exact, n, large)


def _const_tables():
    f32 = np.float32
    pos = np.arange(S, dtype=f32)
    freqs = (np.float32(10000.0) ** (-np.arange(32, dtype=f32) / np.float32(32))).astype(f32)
    ang = (pos[:, None] * freqs[None, :]).astype(f32)
    cos, sin = np.cos(ang).astype(f32), np.sin(ang).astype(f32)
    d = np.arange(128) % 64
    j = d % 32
    sign = np.where(d < 32, -1.0, 1.0).astype(f32)
    cosT2 = np.ascontiguousarray(cos[:, j].T)
    sinT2 = np.ascontiguousarray((sin[:, j] * sign[None, :]).T)
    gam = 1.0 - 2.0 ** (-5.0 - np.arange(8, dtype=np.float64))
    idx = np.arange(128, dtype=np.float64)
    dist = idx[None, :] - idx[:, None]
    decT = np.where(dist >= 0, gam[:, None, None] ** np.maximum(dist, 0.0)[None], 0.0) * 0.125
    decT = np.ascontiguousarray(decT.transpose(1, 0, 2).reshape(128, 1024)).astype(f32)
    kd = gam[None, :] ** (127.0 - idx)[:, None] * 0.125
    kdec = np.ascontiguousarray(np.repeat(kd, 64, axis=1)).astype(f32)
    qd = gam[:, None] ** (idx + 1.0)[None, :]
    qdec = np.zeros((128, 4, 512), f32)
    for p in range(128):
        for pair in range(4):
            h = 2 * pair + p // 64
            qdec[p, pair, :] = np.tile(qd[h], 4)
    cdec = np.ascontiguousarray(np.repeat((gam ** 128.0)[None, :], 64, axis=1).repeat(64, axis=0)).astype(f32)
    ident = np.eye(128, dtype=f32)
    iota = np.ascontiguousarray(np.broadcast_to(np.arange(128, dtype=f32)[None, :], (128, 128)))
    iota16r = np.ascontiguousarray(np.broadcast_to((np.arange(2048) % 16).astype(f32)[None, :], (128, 2048)))
    return dict(cosT2=cosT2, sinT2=sinT2, decT=decT, kdec=kdec, qdec=qdec, cdec=cdec, ident=ident, iota=iota, iota16r=iota16r)


def _host_prep(inp):
    f32 = np.float32
    w_in = np.asarray(inp["w_in"][0], f32)

    def swap_cols(w):
        return np.ascontiguousarray(w.reshape(1024, 8, 2, 32)[:, :, ::-1, :]).reshape(1024, 512)

    w_in_ext = np.ascontiguousarray(np.concatenate([w_in, swap_cols(w_in[:, 0:512]), swap_cols(w_in[:, 512:1024])], axis=1))
    rel_bias = np.asarray(inp["rel_bias"], f32)
    k = np.arange(128)[:, None]
    q = np.arange(128)[None, :]
    b0 = _t5_bucket_np(q - k)
    b1 = _t5_bucket_np(128 + q - k)
    biasT = np.zeros((128, 4, 2, 128), f32)
    for h in range(4):
        biasT[:, h, 0, :] = np.where(q >= k, rel_bias[b0, h], f32(NEG))
        biasT[:, h, 1, :] = rel_bias[b1, h]
    shared = dict(
        w_in_ext=w_in_ext,
        w_out=np.ascontiguousarray(inp["w_out"][0], dtype=f32),
        w_pq=np.ascontiguousarray(inp["peer_query"][0], dtype=f32),
        keysT=np.ascontiguousarray(np.asarray(inp["peer_keys"][0], f32).reshape(16, 128, 128).transpose(2, 0, 1)),
        w_downT=np.ascontiguousarray(np.asarray(inp["peer_down"][0], f32).T),
        w_up=np.ascontiguousarray(inp["peer_up"][0], dtype=f32),
        g_mix=np.ascontiguousarray(np.asarray(inp["norm_mix"][0], f32).reshape(8, 128).T),
        g_ffn=np.ascontiguousarray(np.asarray(inp["norm_ffn"][0], f32).reshape(8, 128).T),
        g_fin=np.ascontiguousarray(np.asarray(inp["norm_final"], f32).reshape(8, 128).T),
        retgn_b=np.ascontiguousarray(np.broadcast_to(np.asarray(inp["ret_gn"][0], f32)[None, :], (128, 512))),
        subln_b=np.ascontiguousarray(np.broadcast_to(np.asarray(inp["diff_subln"][0], f32)[None, :], (128, 128))),
        lamvec=np.ascontiguousarray(np.broadcast_to(np.stack([np.asarray(inp[n][0], f32) for n in
                                    ("diff_lambda_q1", "diff_lambda_k1", "diff_lambda_q2", "diff_lambda_k2")])[None], (128, 4, 64))),
        cbias=np.ascontiguousarray(np.broadcast_to(rel_bias[31][None, :], (128, 4))),
        biasT=biasT,
    )
    shared.update(_const_tables())
    return shared


IN_SPECS = [
    ("xT", [1024, S]), ("w_in_ext", [1024, 4608]), ("w_out", [1024, 1024]), ("w_pq", [1024, 2048]),
    ("keysT", [128, 16, 128]), ("w_downT", [1024, 16384]), ("w_up", [16384, 1024]),
    ("g_mix", [128, 8]), ("g_ffn", [128, 8]), ("g_fin", [128, 8]), ("retgn_b", [128, 512]), ("subln_b", [128, 128]),
    ("lamvec", [128, 4, 64]), ("cbias", [128, 4]), ("biasT", [128, 4, 2, 128]),
    ("cosT2", [128, S]), ("sinT2", [128, S]), ("decT", [128, 1024]), ("kdec", [128, 512]), ("qdec", [128, 4, 512]),
    ("cdec", [64, 512]), ("ident", [128, 128]), ("iota", [128, 128]), ("iota16r", [128, 2048]),
]


def build_program(dbg=False, phases="ABCDE"):
    nc = bass.Bass("TRN2", target_bir_lowering=False)
    I = {}
    for name, shape in IN_SPECS:
        I[name] = nc.dram_tensor(name, list(shape), F32, kind="ExternalInput").ap()
    outT = nc.dram_tensor("outT", [1024, S], F32, kind="ExternalOutput").ap()
    SK = "ExternalOutput" if dbg else "Internal"
    scr = lambda name, shape, dtype: nc.dram_tensor(name, list(shape), dtype, kind=SK).ap()
    QT_ret = scr("QT_ret", [512, S], BF16)
    QdT_ret = scr("QdT_ret", [512, S], BF16)
    KT_ret = scr("KT_ret", [512, S], BF16)
    QT_dif = scr("QT_dif", [512, S], BF16)
    KT_dif = scr("KT_dif", [512, S], BF16)
    V_ret = scr("V_ret", [S, 512], BF16)
    SG = scr("SG", [S, 512], F32)
    V_dif = scr("V_dif", [S, 512], BF16)
    mixT = scr("mixT", [1024, S], BF16)
    x1T = scr("x1T", [1024, S], F32)
    h2T_d = scr("h2T_d", [1024, S], BF16)
    LSTd = scr("LSTd", [128, 3, S], BF16)
    wdT_bf = nc.dram_tensor("wdT_bf", [32, 128, 8, 512], BF16, kind="Internal").ap()
    wup_bf = nc.dram_tensor("wup_bf", [32, 128, 4, 1024], BF16, kind="Internal").ap()
    wout_bf = nc.dram_tensor("wout_bf", [1024, 1024], BF16, kind="Internal").ap()
    wpq_bf = nc.dram_tensor("wpq_bf", [1024, 2048], BF16, kind="Internal").ap()
    keys_bf = nc.dram_tensor("keys_bf", [128, 16, 128], BF16, kind="Internal").ap()

    with ExitStack() as es:
        fw = FW(nc, es)
        fw.init_psum(es)
        pe, act, dve, pool, sp = fw.pe, fw.act, fw.dve, fw.pool, fw.sp
        out_dsems = []

        cst = ExitStack()
        es.enter_context(cst)
        ones_f = T(fw, cst, "ones_f", [128, 128], F32)
        epst = T(fw, cst, "epst", [128, 1], F32)
        ident_f = T(fw, cst, "ident_f", [128, 128], F32, dma=True)
        ident_b = T(fw, cst, "ident_b", [128, 128], BF16)
        sqr = Ring([T(fw, cst, "sq%d" % i, [128, 512], F32) for i in range(2)])
        sd = T(fw, cst, "sd", [128, 512], F32)
        dve.op(lambda e: e.memset(ones_f[:], 1.0), [], [ones_f.b])
        neghalf = T(fw, cst, "neghalf", [128, 16], F32)
        dve.op(lambda e: e.memset(neghalf[:], -0.5), [], [neghalf.b])
        dve.op(lambda e: e.memset(epst[:], EPS), [], [epst.b])
        sp.dma(ident_f[:], I["ident"][:, :], [], [ident_f.b], ident_f.d)
        dve.op(lambda e: e.tensor_copy(out=ident_b[:], in_=ident_f[:]), [ident_f.b], [ident_b.b])

        wcast = fw.dsem("wcast")
        wd_b = TB("wdT_bf")
        wu_b = TB("wup_bf")

        wdv = I["w_downT"].rearrange("(c p) e -> p c e", p=128)
        wuv = I["w_up"].rearrange("(i j) d -> j i d", j=128)
        wcast_todo = []
        for ig in range(32):
            wcast_todo.append(lambda ig=ig: pool.dma(wdT_bf[ig], wdv[:, :, ig * 512:(ig + 1) * 512], [], [wd_b], wcast))
            wcast_todo.append(lambda ig=ig: pool.dma(wup_bf[ig], wuv[:, ig * 4:(ig + 1) * 4, :], [], [wu_b], wcast))

        wsm_b = TB("wsmall_bf")
        for r in range(2):
            wcast_todo.append(lambda r=r: pool.dma(wout_bf[r * 512:(r + 1) * 512, :], I["w_out"][r * 512:(r + 1) * 512, :], [], [wsm_b], wcast))
        for r in range(4):
            wcast_todo.append(lambda r=r: pool.dma(wpq_bf[r * 256:(r + 1) * 256, :], I["w_pq"][r * 256:(r + 1) * 256, :], [], [wsm_b], wcast))
        wcast_todo.append(lambda: pool.dma(keys_bf[:, :, :], I["keysT"][:, :, :], [], [wsm_b], wcast))

        def issue_wcasts(n=None):
            k = len(wcast_todo) if n is None else min(n, len(wcast_todo))
            for _ in range(k):
                wcast_todo.pop(0)()

        def rms_a(xt, N, bank=None):
            bk, bk_b = bank if bank is not None else fw.bank()
            for c in range(8):
                sq = sqr.next()
                act.op(lambda e: e.activation(out=sq[:, 0:N], in_=xt[:, c, 0:N], func=AF.Square), [xt.b], [sq.b])
                pe.op(lambda e: e.matmul(bk[:, 0:N], lhsT=ones_f[:], rhs=sq[:, 0:N], start=(c == 0), stop=(c == 7)),
                      [ones_f.b, sq.b], [bk_b])
            act.op(lambda e: e.activation(out=sd[:, 0:N], in_=bk[:, 0:N], func=AF.Sqrt, bias=epst[:, 0:1], scale=1.0 / D),
                   [epst.b], [sd.b, bk_b])

        def rms_b(xt, g, out, N):
            dve.op(lambda e: e.reciprocal(out=sd[:, 0:N], in_=sd[:, 0:N]), [sd.b], [sd.b])
            for c in range(8):
                dve.op(lambda e: e.scalar_tensor_tensor(out=out[:, c, 0:N], in0=xt[:, c, 0:N], scalar=g[:, c:c + 1], in1=sd[:, 0:N],
                                                        op0=ALU.mult, op1=ALU.mult), [xt.b, g.b, sd.b], [out.b])

        def rmsnorm(xt, g, out, N, act_sq=True):
            rms_a(xt, N)
            rms_b(xt, g, out, N)

        if "A" in phases:
            with ExitStack() as ph:
                w_in = T(fw, ph, "w_in", [128, 8, 4608], BF16, dma=True)
                for c in range(8):
                    pool.dma(w_in[:, c, :], I["w_in_ext"][c * 128:(c + 1) * 128, :], [], [w_in.b], w_in.d)
                g_mix = T(fw, ph, "g_mix", [128, 8], F32, dma=True)
                sp.dma(g_mix[:], I["g_mix"][:, :], [], [g_mix.b], g_mix.d)
                qdec = T(fw, ph, "qdec", [128, 4, 512], F32, dma=True)
                sp.dma(qdec[:], I["qdec"][:, :, :], [], [qdec.b], qdec.d)
                xr = Ring([T(fw, ph, "xa%d" % i, [128, 8, 512], F32, dma=True) for i in range(2)])
                cr = Ring([T(fw, ph, "cs%d" % i, [128, 2, 512], F32, dma=True) for i in range(2)])
                hTr = Ring([T(fw, ph, "hT%d" % i, [128, 8, 512], BF16) for i in range(2)])
                t1r = Ring([T(fw, ph, "t1_%d" % i, [128, 512], F32) for i in range(3)])
                t2r = Ring([T(fw, ph, "t2_%d" % i, [128, 512], F32) for i in range(3)])
                sbf = Ring([T(fw, ph, "sbf%d" % i, [128, 512], BF16, dma=True) for i in range(8)])
                sf3 = Ring([T(fw, ph, "sf3_%d" % i, [128, 512], F32, dma=True) for i in range(3)])
                xT_v = I["xT"].rearrange("(c p) n -> p c n", p=128)

                def store(eng_q, dst, st):
                    eng_q.dma(dst, st[:], [st.b], [], st.d)

                loaded = {}

                def load_x(t_):
                    ts_ = slice(t_ * 512, (t_ + 1) * 512)
                    xt = xr.next()
                    sp.dma(xt[:], xT_v[:, :, ts_], [], [xt.b], xt.d)
                    cs_ = cr.next()
                    sp.dma(cs_[:, 0, :], I["cosT2"][:, ts_], [], [cs_.b], cs_.d)
                    sp.dma(cs_[:, 1, :], I["sinT2"][:, ts_], [], [cs_.b], cs_.d)
                    loaded[t_] = (xt, cs_)

                def prep(t_):
                    xt, cs_ = loaded.pop(t_)
                    hT_ = hTr.next()
                    rmsnorm(xt, g_mix, hT_, 512)
                    return hT_, cs_

                load_x(0)
                nxt_prep = prep(0)
                for t in range(8):
                    ts = slice(t * 512, (t + 1) * 512)
                    hT, cs = nxt_prep
                    if t + 1 < 8:
                        load_x(t + 1)

                    def fm_group(col0):
                        bk, bk_b = fw.bank()
                        for c in range(8):
                            pe.op(lambda e: e.matmul(bk[:, :], lhsT=w_in[:, c, col0:col0 + 128], rhs=hT[:, c, :], start=(c == 0), stop=(c == 7)),
                                  [w_in.b, hT.b], [bk_b], inc=(c == 7))
                        return bk, bk_b

                    for kind, c0, c0s, dst in (("q", 0, 3584, QT_ret), ("k", 512, 4096, KT_ret)):
                        for j in range(4):
                            bp, bp_b = fm_group(c0 + j * 128)
                            bs, bs_b = fm_group(c0s + j * 128)
                            t1 = t1r.next()
                            t2 = t2r.next()
                            dve.op(lambda e: e.tensor_tensor(out=t1[:], in0=bp[:, :], in1=cs[:, 0, :], op=ALU.mult), [cs.b], [t1.b, bp_b])
                            dve.op(lambda e: e.tensor_tensor(out=t2[:], in0=bs[:, :], in1=cs[:, 1, :], op=ALU.mult), [cs.b], [t2.b, bs_b])
                            st = sbf.next()
                            if kind == "q":
                                dve.op(lambda e: e.tensor_tensor(out=t1[:], in0=t1[:], in1=t2[:], op=ALU.add), [t1.b, t2.b], [t1.b])
                                act.op(lambda e: e.activation(out=st[:], in_=t1[:], func=AF.Copy), [t1.b], [st.b])
                                store(sp, dst[j * 128:(j + 1) * 128, ts], st)
                                st2 = sbf.next()
                                pool.op(lambda e: e.tensor_tensor(out=st2[:], in0=t1[:], in1=qdec[:, j, :], op=ALU.mult), [t1.b, qdec.b], [st2.b])
                                store(sp, QdT_ret[j * 128:(j + 1) * 128, ts], st2)
                            else:
                                dve.op(lambda e: e.tensor_tensor(out=st[:], in0=t1[:], in1=t2[:], op=ALU.add), [t1.b, t2.b], [st.b])
                                store(sp, dst[j * 128:(j + 1) * 128, ts], st)
                    for c0, dst, scl in ((2048, QT_dif, 0.125), (2560, KT_dif, 1.0)):
                        for j in range(4):
                            bk, bk_b = fm_group(c0 + j * 128)
                            st = sbf.next()
                            act.op(lambda e: e.activation(out=st[:], in_=bk[:, :], func=AF.Copy, scale=scl), [], [st.b, bk_b])
                            store(sp, dst[j * 128:(j + 1) * 128, ts], st)
                    if t + 1 < 8:
                        nxt_prep = prep(t + 1)
                    for sub in range(4):
                        rows = slice(t * 512 + sub * 128, t * 512 + (sub + 1) * 128)
                        for gi, col0 in enumerate((1024, 1536, 3072)):
                            bk, bk_b = fw.bank()
                            for c in range(8):
                                pe.op(lambda e: e.matmul(bk[:, :], lhsT=hT[:, c, sub * 128:(sub + 1) * 128], rhs=w_in[:, c, col0:col0 + 512],
                                                         start=(c == 0), stop=(c == 7)), [w_in.b, hT.b], [bk_b], inc=(c == 7))
                            if gi == 0:
                                st = sbf.next()
                                act.op(lambda e: e.activation(out=st[:], in_=bk[:, :], func=AF.Copy), [], [st.b, bk_b])
                                store(sp, V_ret[rows, :], st)
                            elif gi == 1:
                                st = sf3.next()
                                act.op(lambda e: e.activation(out=st[:], in_=bk[:, :], func=AF.Silu), [], [st.b, bk_b])
                                store(sp, SG[rows, :], st)
                            else:
                                st = sbf.next()
                                dve.op(lambda e: e.tensor_copy(out=st[:], in_=bk[:, :]), [], [st.b, bk_b])
                                store(sp, V_dif[rows, :], st)
                fw.barrier()
        if "C" not in phases:
            issue_wcasts()

        if "B" in phases and "C" in phases:
            PHASE_C(locals(), with_B=True)
        else:
            PHASE_B(locals()) if "B" in phases else None
            PHASE_C(locals()) if "C" in phases else None
        PHASE_DE(locals(), phases) if ("D" in phases or "E" in phases) else None

        fw.barrier()
    return nc


class NS:
    def __init__(self, d):
        self.__dict__.update(d)


class RetentionB:
    def __init__(self, L, ph, bank_ring):
        self.L = L
        fw, I = L.fw, L.I
        self.fw = fw
        sp = fw.sp
        self.banks = bank_ring
        self.decT = T(fw, ph, "decT", [128, 1024], F32, dma=True)
        sp.dma(self.decT[:], I["decT"][:, :], [], [self.decT.b], self.decT.d)
        self.kdec = T(fw, ph, "kdec", [128, 512], F32, dma=True)
        sp.dma(self.kdec[:], I["kdec"][:, :], [], [self.kdec.b], self.kdec.d)
        self.cdec = T(fw, ph, "cdec", [64, 512], F32, dma=True)
        sp.dma(self.cdec[:], I["cdec"][:, :], [], [self.cdec.b], self.cdec.d)
        self.retgn = T(fw, ph, "retgn", [128, 512], F32, dma=True)
        sp.dma(self.retgn[:], I["retgn_b"][:, :], [], [self.retgn.b], self.retgn.d)
        self.Qr = Ring([T(fw, ph, "Qg%d" % i, [64, 8, 512], BF16, dma=True) for i in range(2)])
        self.Qdr = Ring([T(fw, ph, "Qdg%d" % i, [64, 8, 512], BF16, dma=True) for i in range(2)])
        self.Kr = Ring([T(fw, ph, "Kg%d" % i, [64, 8, 512], BF16, dma=True) for i in range(2)])
        self.Vr = Ring([T(fw, ph, "Vg%d" % i, [128, 4, 512], BF16, dma=True) for i in range(2)])
        self.SGr = Ring([T(fw, ph, "SGg%d" % i, [128, 4, 512], F32, dma=True) for i in range(2)])
        self.sTm_r = Ring([T(fw, ph, "sTm%d" % i, [128, 1024], BF16) for i in range(2)])
        self.Kp_r = Ring([T(fw, ph, "Kp%d" % i, [128, 512], BF16) for i in range(2)])
        self.R32 = T(fw, ph, "R32", [64, 512], F32)
        self.Rb_r = Ring([T(fw, ph, "Rb%d" % i, [64, 512], BF16) for i in range(2)])
        self.ro = T(fw, ph, "ro", [128, 512], F32)
        self.sq = T(fw, ph, "rsq", [128, 512], F32)
        self.y = T(fw, ph, "ry", [128, 512], F32)
        self.st8 = T(fw, ph, "st8", [128, 32], F32)
        self.mtok = T(fw, ph, "mtok", [128, 512], BF16)
        self.mst_r = Ring([T(fw, ph, "mst%d" % i, [128, 4, 512], BF16, dma=True) for i in range(2)])
        fw.dve.op(lambda e: e.memset(self.R32[:], 0.0), [], [self.R32.b])
        self.Rb_prev = None
        self.groups = {}
        self.st = {}
        self.mst = None

    def load_group(self, tg):
        L, sp = self.L, self.fw.sp
        ts = slice(tg * 512, (tg + 1) * 512)
        Qg, Qdg, Kg, Vg, SGg = self.Qr.next(), self.Qdr.next(), self.Kr.next(), self.Vr.next(), self.SGr.next()
        sp.dma(Qg[:], L.QT_ret.rearrange("(h d) n -> d h n", d=64)[:, :, ts], [], [Qg.b], Qg.d)
        sp.dma(Qdg[:], L.QdT_ret.rearrange("(h d) n -> d h n", d=64)[:, :, ts], [], [Qdg.b], Qdg.d)
        sp.dma(Kg[:], L.KT_ret.rearrange("(h d) n -> d h n", d=64)[:, :, ts], [], [Kg.b], Kg.d)
        sp.dma(Vg[:], L.V_ret[ts, :].rearrange("(c p) f -> p c f", p=128), [], [Vg.b], Vg.d)
        sp.dma(SGg[:], L.SG[ts, :].rearrange("(c p) f -> p c f", p=128), [], [SGg.b], SGg.d)
        self.groups[tg] = (Qg, Qdg, Kg, Vg, SGg)

    def s1(self, c):
        fw = self.fw
        pe, dve = fw.pe, fw.dve
        tg, cc = divmod(c, 4)
        if cc == 0:
            if tg == 0:
                self.load_group(0)
            if tg + 1 < 8:
                self.load_group(tg + 1)
        Qg, Qdg, Kg, Vg, SGg = self.groups[tg]
        lr = slice(cc * 128, (cc + 1) * 128)
        ident_b = self.L.ident_b
        bA = [self.banks.next(), self.banks.next()]
        for h in range(8):
            bk, bk_b = bA[h // 4]
            pe.op(lambda e: e.matmul(bk[:, (h % 4) * 128:(h % 4 + 1) * 128], lhsT=Kg[:, h, lr], rhs=Qg[:, h, lr], start=True, stop=True),
                  [Kg.b, Qg.b], [bk_b], inc=(h % 4 == 3))
        sTm = self.sTm_r.next()
        for half in range(2):
            bk, bk_b = bA[half]
            dve.op(lambda e: e.tensor_tensor(out=sTm[:, half * 512:(half + 1) * 512], in0=bk[:, :], in1=self.decT[:, half * 512:(half + 1) * 512],
                                             op=ALU.mult), [self.decT.b], [sTm.b, bk_b])
        bT, bT_b = self.banks.next()
        for h in range(8):
            pe.op(lambda e: e.matmul(bT[:, h * 64:(h + 1) * 64], lhsT=Kg[:, h, lr], rhs=ident_b[0:64, 0:64], start=True, stop=True),
                  [Kg.b, ident_b.b], [bT_b], inc=(h == 7))
        Kp = self.Kp_r.next()
        dve.op(lambda e: e.tensor_tensor(out=Kp[:], in0=bT[:, :], in1=self.kdec[:], op=ALU.mult), [self.kdec.b], [Kp.b, bT_b])
        self.st[c] = (sTm, Kp)

    def s2(self, c):
        fw = self.fw
        pe, act, dve, pool = fw.pe, fw.act, fw.dve, fw.pool
        tg, cc = divmod(c, 4)
        Qg, Qdg, Kg, Vg, SGg = self.groups[tg]
        lr = slice(cc * 128, (cc + 1) * 128)
        sTm, Kp = self.st.pop(c)
        R32, ro, sq, y, st8, mtok, epst = self.R32, self.ro, self.sq, self.y, self.st8, self.mtok, self.L.epst
        Rb_prev = self.Rb_prev
        bO, bO_b = self.banks.next()
        for h in range(8):
            hs = slice(h * 64, (h + 1) * 64)
            pe.op(lambda e: e.matmul(bO[:, hs], lhsT=sTm[:, h * 128:(h + 1) * 128], rhs=Vg[:, cc, hs], start=True, stop=(c == 0)),
                  [sTm.b, Vg.b], [bO_b], inc=(c == 0 and h == 7))
            if c > 0:
                pe.op(lambda e: e.matmul(bO[:, hs], lhsT=Qdg[:, h, lr], rhs=Rb_prev[:, hs], start=False, stop=True),
                      [Qdg.b, Rb_prev.b], [bO_b], inc=(h == 7))
        if c < 31:
            bKV, bKV_b = self.banks.next()
            for h in range(8):
                hs = slice(h * 64, (h + 1) * 64)
                pe.op(lambda e: e.matmul(bKV[0:64, hs], lhsT=Kp[:, hs], rhs=Vg[:, cc, hs], start=True, stop=True),
                      [Kp.b, Vg.b], [bKV_b], inc=(h == 7))
            dve.op(lambda e: e.tensor_tensor(out=R32[:], in0=R32[:], in1=self.cdec[:], op=ALU.mult), [R32.b, self.cdec.b], [R32.b])
            dve.op(lambda e: e.tensor_tensor(out=R32[:], in0=R32[:], in1=bKV[0:64, :], op=ALU.add), [R32.b], [R32.b, bKV_b])
            Rb = self.Rb_r.next()
            pool.op(lambda e: e.tensor_copy(out=Rb[:], in_=R32[:]), [R32.b], [Rb.b])
            self.Rb_prev = Rb
        hview = lambda ap: ap.rearrange("p (h e) -> p h e", e=64)
        dve.op(lambda e: e.tensor_copy(out=ro[:], in_=bO[:, :]), [], [ro.b, bO_b])
        dve.op(lambda e: e.tensor_tensor(out=sq[:], in0=ro[:], in1=ro[:], op=ALU.mult), [ro.b], [sq.b])
        dve.op(lambda e: e.tensor_reduce(out=st8[:, 0:8], in_=hview(ro[:]), axis=AX.X, op=ALU.add), [ro.b], [st8.b])
        dve.op(lambda e: e.tensor_reduce(out=st8[:, 8:16], in_=hview(sq[:]), axis=AX.X, op=ALU.add), [sq.b, st8.b], [st8.b])
        dve.op(lambda e: e.tensor_scalar(out=st8[:, 16:24], in0=st8[:, 0:8], scalar1=1.0 / 64, scalar2=None, op0=ALU.mult), [st8.b], [st8.b])
        dve.op(lambda e: e.tensor_tensor(out=st8[:, 24:32], in0=st8[:, 16:24], in1=st8[:, 16:24], op=ALU.mult), [st8.b], [st8.b])
        dve.op(lambda e: e.scalar_tensor_tensor(out=st8[:, 8:16], in0=st8[:, 8:16], scalar=1.0 / 64, in1=st8[:, 24:32],
                                                op0=ALU.mult, op1=ALU.subtract), [st8.b], [st8.b])
        dve.op(lambda e: e.tensor_scalar(out=st8[:, 8:16], in0=st8[:, 8:16], scalar1=EPS, scalar2=None, op0=ALU.add), [st8.b], [st8.b])
        neghalf = self.L.neghalf
        pool.op(lambda e: e.tensor_tensor(out=st8[:, 8:16], in0=st8[:, 8:16], in1=neghalf[:, 0:8], op=ALU.pow), [st8.b, neghalf.b], [st8.b])
        mean_b = st8[:, 16:24].unsqueeze(2).to_broadcast([128, 8, 64])
        rstd_b = st8[:, 8:16].unsqueeze(2).to_broadcast([128, 8, 64])
        dve.op(lambda e: e.tensor_tensor(out=hview(y[:]), in0=hview(ro[:]), in1=mean_b, op=ALU.subtract), [ro.b, st8.b], [y.b])
        dve.op(lambda e: e.tensor_tensor(out=hview(y[:]), in0=hview(y[:]), in1=rstd_b, op=ALU.mult), [y.b, st8.b], [y.b])
        dve.op(lambda e: e.tensor_tensor(out=y[:], in0=y[:], in1=SGg[:, cc, :], op=ALU.mult), [y.b, SGg.b], [y.b])
        dve.op(lambda e: e.tensor_tensor(out=mtok[:], in0=y[:], in1=self.retgn[:], op=ALU.mult), [y.b, self.retgn.b], [mtok.b])

    def s3(self, c):
        fw = self.fw
        pe, dve, sp = fw.pe, fw.dve, fw.sp
        tg, cc = divmod(c, 4)
        lr = slice(cc * 128, (cc + 1) * 128)
        ident_b, mtok = self.L.ident_b, self.mtok
        if cc == 0:
            self.mst = self.mst_r.next()
        mst = self.mst
        bX, bX_b = self.banks.next()
        for fc in range(4):
            pe.op(lambda e: e.matmul(bX[:, fc * 128:(fc + 1) * 128], lhsT=mtok[:, fc * 128:(fc + 1) * 128], rhs=ident_b[:], start=True, stop=True),
                  [mtok.b, ident_b.b], [bX_b], inc=(fc == 3))
        dve.op(lambda e: e.tensor_copy(out=mst[:, :, lr], in_=bX[:, :].rearrange("p (f l) -> p f l", l=128)), [], [mst.b, bX_b])
        if cc == 3:
            ts = slice(tg * 512, (tg + 1) * 512)
            sp.dma(self.L.mixT[0:512, ts].rearrange("(f p) n -> p f n", p=128), mst[:], [mst.b], [], mst.d)


def PHASE_B(L):
    L = NS(L)
    fw = L.fw

    class _AllBanks:
        def next(self):
            return fw.bank()

    with ExitStack() as ph:
        B = RetentionB(L, ph, _AllBanks())
        for c in range(32):
            B.s1(c)
            B.s2(c)
            B.s3(c)
        fw.barrier()


def PHASE_C(L, with_B=False):
    L = NS(L)
    fw, I = L.fw, L.I
    pe, act, dve, pool, sp = fw.pe, fw.act, fw.dve, fw.pool, fw.sp
    ident_b, ident_f, epst = L.ident_b, L.ident_f, L.epst
    with ExitStack() as ph:
        biasT = T(fw, ph, "biasT", [128, 4, 2, 128], F32, dma=True)
        sp.dma(biasT[:], I["biasT"][:, :, :, :], [], [biasT.b], biasT.d)
        cb = T(fw, ph, "cb", [128, 4], F32, dma=True)
        sp.dma(cb[:], I["cbias"][:, :], [], [cb.b], cb.d)
        subln = T(fw, ph, "subln", [128, 128], F32, dma=True)
        sp.dma(subln[:], I["subln_b"][:, :], [], [subln.b], subln.d)
        lamv = T(fw, ph, "lamv", [128, 4, 64], F32, dma=True)
        sp.dma(lamv[:], I["lamvec"][:, :, :], [], [lamv.b], lamv.d)
        bhl = T(fw, ph, "bhl", [128, 2, 4, 2, 128], BF16)
        Vp = T(fw, ph, "Vp", [128, 32, 4, 129], BF16)
        lt = T(fw, ph, "lt", [128, 8], F32)
        zero1 = T(fw, ph, "zero1", [128, 1], F32)
        dve.op(lambda e: e.memset(zero1[:], 0.0), [], [zero1.b])
        prod = T(fw, ph, "lprod", [128, 2, 64], F32)
        dve.op(lambda e: e.tensor_tensor(out=prod[:, 0, :], in0=lamv[:, 0, :], in1=lamv[:, 1, :], op=ALU.mult), [lamv.b], [prod.b])
        dve.op(lambda e: e.tensor_tensor(out=prod[:, 1, :], in0=lamv[:, 2, :], in1=lamv[:, 3, :], op=ALU.mult), [lamv.b, prod.b], [prod.b])
        dve.op(lambda e: e.tensor_reduce(out=lt[:, 0:2], in_=prod[:], axis=AX.X, op=ALU.add), [prod.b], [lt.b])
        act.op(lambda e: e.activation(out=lt[:, 2:4], in_=lt[:, 0:2], func=AF.Exp), [lt.b], [lt.b])
        dve.op(lambda e: e.tensor_tensor(out=lt[:, 4:5], in0=lt[:, 3:4], in1=lt[:, 2:3], op=ALU.subtract), [lt.b], [lt.b])
        dve.op(lambda e: e.tensor_scalar(out=lt[:, 4:5], in0=lt[:, 4:5], scalar1=-LAMBDA_INIT, scalar2=None, op0=ALU.add), [lt.b], [lt.b])
        dve.op(lambda e: e.tensor_scalar(out=subln[:], in0=subln[:], scalar1=1.0 - LAMBDA_INIT, scalar2=None, op0=ALU.mult), [subln.b], [subln.b])
        QTr = Ring([T(fw, ph, "QTh%d" % i, [128, S], BF16, dma=True) for i in range(2)])
        KTr = Ring([T(fw, ph, "KTh%d" % i, [128, S], BF16, dma=True) for i in range(2)])
        heads = {}

        def load_head(h):
            QTh, KTh = QTr.next(), KTr.next()
            sp.dma(QTh[:], L.QT_dif[h * 128:(h + 1) * 128, :], [], [QTh.b], QTh.d)
            sp.dma(KTh[:], L.KT_dif[h * 128:(h + 1) * 128, :], [], [KTh.b], KTh.d)
            heads[h] = (QTh, KTh)

        load_head(0)
        with ExitStack() as tmp:
            btmp = T(fw, tmp, "btmp", [128, 4, 2, 128], F32)
            dve.op(lambda e: e.tensor_copy(out=bhl[:, 0], in_=biasT[:]), [biasT.b], [bhl.b])
            dve.op(lambda e: e.tensor_copy(out=btmp[:], in_=bhl[:, 0]), [bhl.b], [btmp.b])
            dve.op(lambda e: e.tensor_tensor(out=btmp[:], in0=biasT[:], in1=btmp[:], op=ALU.subtract), [biasT.b, btmp.b], [btmp.b])
            dve.op(lambda e: e.tensor_copy(out=bhl[:, 1], in_=btmp[:]), [btmp.b, bhl.b], [bhl.b])
            Vall = T(fw, tmp, "Vall", [128, 32, 512], BF16, dma=True)
            for g in range(4):
                sp.dma(Vall[:, g * 8:(g + 1) * 8, :], L.V_dif[g * 1024:(g + 1) * 1024, :].rearrange("(c p) f -> p c f", p=128), [], [Vall.b], Vall.d)
            pool.op(lambda e: e.memset(Vp[:, :, :, 128:129], 1.0), [], [Vp.b])
            for g in range(4):
                src = Vall[:, g * 8:(g + 1) * 8, :].rearrange("p c (h e) -> p c h e", e=128)
                if g % 2 == 0:
                    dve.op(lambda e: e.tensor_copy(out=Vp[:, g * 8:(g + 1) * 8, :, 0:128], in_=src), [Vall.b], [Vp.b])
                else:
                    act.op(lambda e: e.activation(out=Vp[:, g * 8:(g + 1) * 8, :, 0:128], in_=src, func=AF.Copy), [Vall.b], [Vp.b])
            fw.barrier()
        Pr = Ring([T(fw, ph, "P%d" % i, [128, 512], BF16) for i in range(3)])
        o1 = T(fw, ph, "o1", [128, 4, 128], F32)
        oo = T(fw, ph, "oo", [128, 4, 128], F32)
        osq = T(fw, ph, "osq", [128, 4, 128], F32)
        zz = T(fw, ph, "zz", [128, 16], F32)
        mtk_r = Ring([T(fw, ph, "mtk%d" % i, [128, 4, 128], BF16) for i in range(2)])
        oraw = T(fw, ph, "oraw", [128, 4, 132], F32, parts=4)
        epi_late = []
        mst_r = Ring([T(fw, ph, "cmst%d" % i, [128, 512], BF16, dma=True) for i in range(2)])
        Ob = [fw.banks[i] for i in range(4)]
        Sb = Ring([fw.banks[i] for i in (4, 5, 6)])
        bX, bX_b = fw.banks[7]
        steps = [(h, s, m, kb) for h in range(4) for s in range(8) for m in range(2) for kb in range(4 * s + 4)]

        def emit_qk(step):
            h, s, m, kb = step
            if h not in heads:
                load_head(h)
            if s == 5 and m == 0 and kb == 0 and h + 1 < 4 and (h + 1) not in heads:
                load_head(h + 1)
            QTh, KTh = heads[h]
            pr = slice(m * 64, (m + 1) * 64)
            qb_lo = max(4 * s, kb)
            off = (qb_lo - 4 * s) * 128
            near = []
            if kb >= 4 * s:
                near.append((kb, 0))
            if 4 * s <= kb + 1 <= 4 * s + 3:
                near.append((kb + 1, 1))
            sbk, sbk_b = Sb.next()
            pe.op(lambda e: e.matmul(sbk[:, off:512], lhsT=KTh[pr, kb * 128:(kb + 1) * 128], rhs=QTh[pr, qb_lo * 128:(4 * s + 4) * 128],
                                     start=True, stop=(len(near) == 0)), [KTh.b, QTh.b], [sbk_b], inc=(len(near) == 0))
            for ni, (qb, kind) in enumerate(near):
                o_ = (qb - 4 * s) * 128
                for hl in range(2):
                    last = (ni == len(near) - 1) and hl == 1
                    pe.op(lambda e: e.matmul(sbk[:, o_:o_ + 128], lhsT=ident_b[:], rhs=bhl[:, hl, h, kind, :], start=False, stop=last),
                          [ident_b.b, bhl.b], [sbk_b], inc=last)
            far_lo = max(4 * s, kb + 2)
            P = Pr.next()
            if far_lo <= 4 * s + 3:
                fo = (far_lo - 4 * s) * 128
                act.op(lambda e: e.activation(out=P[:, fo:512], in_=sbk[:, fo:512], func=AF.Exp, bias=cb[:, h:h + 1], scale=1.0),
                       [cb.b], [P.b, sbk_b])
            if near:
                n0 = (near[0][0] - 4 * s) * 128
                n1 = (near[-1][0] - 4 * s + 1) * 128
                act.op(lambda e: e.activation(out=P[:, n0:n1], in_=sbk[:, n0:n1], func=AF.Exp, bias=zero1[:, 0:1], scale=1.0),
                       [zero1.b], [P.b, sbk_b])
            return P

        def emit_pv(step, P):
            h, s, m, kb = step
            qb_lo = max(4 * s, kb)
            for qb in range(qb_lo, 4 * s + 4):
                j = qb - 4 * s
                ob, ob_b = Ob[j]
                pe.op(lambda e: e.matmul(ob[:, 0:129], lhsT=P[:, j * 128:(j + 1) * 128], rhs=Vp[:, kb, h, :], start=(kb == 0), stop=(kb == qb)),
                      [P.b, Vp.b], [ob_b], inc=True)

        def emit_epilogue(h, s, m):
            for j in range(4):
                ob, ob_b = Ob[j]
                act.op(lambda e: e.activation(out=oraw[:, j, 0:129], in_=ob[:, 0:129], func=AF.Copy), [], [oraw.bs[j], ob_b])
            for j in range(4):
                rb = oraw.bs[j]
                if m == 0:
                    dve.op(lambda e: e.reciprocal(out=zz[:, j:j + 1], in_=oraw[:, j, 128:129]), [rb], [zz.b])
                    dve.op(lambda e: e.tensor_scalar(out=o1[:, j, :], in0=oraw[:, j, 0:128], scalar1=zz[:, j:j + 1], scalar2=None, op0=ALU.mult),
                           [rb, zz.b], [o1.b])
                else:
                    dve.op(lambda e: e.reciprocal(out=zz[:, j:j + 1], in_=oraw[:, j, 128:129]), [rb], [zz.b])
                    dve.op(lambda e: e.tensor_tensor(out=zz[:, 4 + j:5 + j], in0=zz[:, j:j + 1], in1=L_lt(lt), op=ALU.mult), [zz.b, lt.b], [zz.b])
                    dve.op(lambda e: e.scalar_tensor_tensor(out=oo[:, j, :], in0=oraw[:, j, 0:128], scalar=zz[:, 4 + j:5 + j], in1=o1[:, j, :],
                                                            op0=ALU.mult, op1=ALU.add), [rb, zz.b, o1.b], [oo.b])
            if m == 0:
                return
            dve.op(lambda e: e.tensor_tensor(out=osq[:], in0=oo[:], in1=oo[:], op=ALU.mult), [oo.b], [osq.b])
            mtk = mtk_r.next()
            dve.op(lambda e: e.tensor_reduce(out=zz[:, 8:12], in_=osq[:], axis=AX.X, op=ALU.add), [osq.b, zz.b], [zz.b])
            dve.op(lambda e: e.tensor_scalar(out=zz[:, 12:16], in0=zz[:, 8:12], scalar1=1.0 / 128, scalar2=EPS, op0=ALU.mult, op1=ALU.add), [zz.b], [zz.b])
            pool.op(lambda e: e.tensor_tensor(out=zz[:, 12:16], in0=zz[:, 12:16], in1=L.neghalf[:, 0:4], op=ALU.pow), [zz.b, L.neghalf.b], [zz.b])
            for j in range(4):
                dve.op(lambda e: e.scalar_tensor_tensor(out=mtk[:, j, :], in0=oo[:, j, :], scalar=zz[:, 12 + j:13 + j], in1=subln[:],
                                                        op0=ALU.mult, op1=ALU.mult), [oo.b, zz.b, subln.b], [mtk.b])

            def late(mtk=mtk, h=h, s=s):
                for j in range(4):
                    pe.op(lambda e: e.matmul(bX[:, j * 128:(j + 1) * 128], lhsT=mtk[:, j, :], rhs=ident_b[:], start=True, stop=True),
                          [mtk.b, ident_b.b], [bX_b], inc=(j == 3))
                mst = mst_r.next()
                dve.op(lambda e: e.tensor_copy(out=mst[:], in_=bX[:, :]), [], [mst.b, bX_b])
                sp.dma(L.mixT[(4 + h) * 128:(5 + h) * 128, s * 512:(s + 1) * 512], mst[:], [mst.b], [], mst.d)

            epi_late.append([20, late])

        Bsched = {}
        if with_B:
            RB = RetentionB(L, ph, Ring([fw.banks[i] for i in (4, 5, 6, 7)]))
            per = len(steps) // 32
            for c in range(32):
                Bsched.setdefault(c * per, []).append(lambda c=c: RB.s1(c))
                Bsched.setdefault(c * per + per // 6, []).append(lambda c=c: RB.s2(c))
                Bsched.setdefault(c * per + (5 * per) // 6, []).append(lambda c=c: RB.s3(c))
        LOOK = 2
        pendP = [emit_qk(steps[i]) for i in range(LOOK)]
        for k, step in enumerate(steps):
            if k % 16 == 8:
                L.issue_wcasts(1)
            for fn in Bsched.get(k, ()):
                fn()
            for item in list(epi_late):
                item[0] -= 1
                if item[0] <= 0:
                    epi_late.remove(item)
                    item[1]()
            if k + LOOK < len(steps):
                pendP.append(emit_qk(steps[k + LOOK]))
            curP = pendP.pop(0)
            emit_pv(step, curP)
            h, s, m, kb = step
            if kb == 4 * s + 3:
                emit_epilogue(h, s, m)
        for item in epi_late:
            item[1]()
        L.issue_wcasts()
        fw.barrier()


def L_lt(lt):
    return lt[:, 4:5]


def PHASE_DE(L, phases):
    L = NS(L)
    fw, I = L.fw, L.I
    pe, act, dve, pool, sp = fw.pe, fw.act, fw.dve, fw.pool, fw.sp
    ident_b, ident_f, epst, rmsnorm = L.ident_b, L.ident_f, L.epst, L.rmsnorm
    U32 = mybir.dt.uint32
    NT = 256
    with ExitStack() as outer:
        if "D" in phases:
            with ExitStack() as ph:
                wout = T(fw, ph, "wout", [128, 8, 1024], BF16, dma=True)
                wpq = T(fw, ph, "wpq", [128, 8, 2048], BF16, dma=True)
                keys = T(fw, ph, "keys", [128, 16, 128], BF16, dma=True)
                if "C" in phases:
                    sp.dma(wout[:], L.wout_bf.rearrange("(c p) f -> p c f", p=128), [], [wout.b], wout.d)
                    sp.dma(wpq[:], L.wpq_bf.rearrange("(c p) f -> p c f", p=128), [], [wpq.b], wpq.d)
                    sp.dma(keys[:], L.keys_bf[:, :, :], [], [keys.b], keys.d)
                else:
                    for c in range(8):
                        pool.dma(wout[:, c, :], I["w_out"][c * 128:(c + 1) * 128, :], [], [wout.b], wout.d)
                        pool.dma(wpq[:, c, :], I["w_pq"][c * 128:(c + 1) * 128, :], [], [wpq.b], wpq.d)
                    pool.dma(keys[:], I["keysT"][:, :, :], [], [keys.b], keys.d)
                g_ffn = T(fw, ph, "g_ffn", [128, 8], F32, dma=True)
                sp.dma(g_ffn[:], I["g_ffn"][:, :], [], [g_ffn.b], g_ffn.d)
                io16 = T(fw, ph, "io16", [128, 8, 16, 16], F32, dma=True)
                sp.dma(io16[:], I["iota16r"].rearrange("p (h k a) -> p h k a", h=8, k=16), [], [io16.b], io16.d)
                xr = Ring([T(fw, ph, "xd%d" % i, [128, 8, NT], F32, dma=True) for i in range(2)])
                mr = Ring([T(fw, ph, "md%d" % i, [128, 8, NT], BF16, dma=True) for i in range(2)])
                x1r = Ring([T(fw, ph, "x1d%d" % i, [128, 8, NT], F32, dma=True) for i in range(2)])
                h2r = Ring([T(fw, ph, "h2d%d" % i, [128, 8, NT], BF16, dma=True) for i in range(2)])
                qTr = Ring([T(fw, ph, "qT%d" % i, [128, 16, NT], BF16) for i in range(2)])
                scr_ = Ring([T(fw, ph, "sc%d" % i, [128, 16, 128], F32) for i in range(2)])
                sc2 = T(fw, ph, "sc2", [128, 16, 128], F32, parts=16)
                mx = T(fw, ph, "mx", [128, 16, 16], F32, parts=16)
                idx = T(fw, ph, "idx", [128, 16, 16], U32, parts=16)
                idxf = T(fw, ph, "idxf", [128, 16, 16], F32)
                cand = T(fw, ph, "cand", [128, 8, 112], F32)
                cand2 = T(fw, ph, "cand2", [128, 8, 112], F32, parts=8)
                m2 = T(fw, ph, "m2", [128, 8, 16], F32, parts=8)
                pos = T(fw, ph, "pos", [128, 8, 16], U32, parts=8)
                pa = T(fw, ph, "pa", [128, 4, 8, 16], U32)
                paf = T(fw, ph, "paf", [128, 4, 8, 16], F32)
                abf = T(fw, ph, "abf", [128, 4, 8, 16], F32)
                oh = T(fw, ph, "oh", [128, 8, 16, 16], F32)
                IJG = T(fw, ph, "IJG", [128, 3, 128], F32, parts=3)
                gz = T(fw, ph, "gz", [128, 16], F32)
                lstg = Ring([T(fw, ph, "lstg%d" % i, [128, 3, 128], BF16, dma=True) for i in range(2)])
                xT_v = I["xT"].rearrange("(c p) n -> p c n", p=128)
                mT_v = L.mixT.rearrange("(c p) n -> p c n", p=128)
                x1_v = L.x1T.rearrange("(c p) n -> p c n", p=128)
                h2_v = L.h2T_d.rearrange("(c p) n -> p c n", p=128)

                pst = {}

                def load_p(t):
                    ts = slice(t * NT, (t + 1) * NT)
                    xt, mt = xr.next(), mr.next()
                    sp.dma(xt[:], xT_v[:, :, ts], [], [xt.b], xt.d)
                    sp.dma(mt[:], mT_v[:, :, ts], [], [mt.b], mt.d)
                    pst[t] = dict(xt=xt, mt=mt, x1=x1r.next(), h2=h2r.next(), qT=qTr.next())

                def p1(t):
                    d = pst[t]
                    ts = slice(t * NT, (t + 1) * NT)
                    xt, mt, x1 = d["xt"], d["mt"], d["x1"]
                    for fc in range(8):
                        bk, bk_b = fw.bank()
                        for mc in range(8):
                            pe.op(lambda e: e.matmul(bk[:, 0:NT], lhsT=wout[:, mc, fc * 128:(fc + 1) * 128], rhs=mt[:, mc, :], start=(mc == 0), stop=(mc == 7)),
                                  [wout.b, mt.b], [bk_b], inc=(mc == 7))
                        dve.op(lambda e: e.tensor_tensor(out=x1[:, fc, :], in0=bk[:, 0:NT], in1=xt[:, fc, :], op=ALU.add), [xt.b], [x1.b, bk_b])
                    sp.dma(x1_v[:, :, ts], x1[:], [x1.b], [], x1.d)
                    L.rms_a(x1, NT)

                def p2(t):
                    d = pst.pop(t)
                    ts = slice(t * NT, (t + 1) * NT)
                    x1, h2, qT = d["x1"], d["h2"], d["qT"]
                    L.rms_b(x1, g_ffn, h2, NT)
                    sp.dma(h2_v[:, :, ts], h2[:], [h2.b], [], h2.d)
                    for hp in range(16):
                        bk, bk_b = fw.bank()
                        for c in range(8):
                            pe.op(lambda e: e.matmul(bk[:, 0:NT], lhsT=wpq[:, c, hp * 128:(hp + 1) * 128], rhs=h2[:, c, :], start=(c == 0), stop=(c == 7)),
                                  [wpq.b, h2.b], [bk_b], inc=(c == 7))
                        act.op(lambda e: e.activation(out=qT[:, hp, :], in_=bk[:, 0:NT], func=AF.Copy), [], [qT.b, bk_b])
                    return qT

                def stage_s(qT, sub):
                    nsl = slice(sub * 128, (sub + 1) * 128)
                    sc = scr_.next()
                    for q4 in range(4):
                        bk, bk_b = fw.bank()
                        for r in range(4):
                            hp = q4 * 4 + r
                            pe.op(lambda e: e.matmul(bk[:, r * 128:(r + 1) * 128], lhsT=qT[:, hp, nsl], rhs=keys[:, hp, :], start=True, stop=True),
                                  [qT.b, keys.b], [bk_b], inc=(r == 3))
                        act.op(lambda e: e.activation(out=sc[:, q4 * 4:(q4 + 1) * 4, :], in_=bk[:, :].rearrange("p (r k) -> p r k", k=128), func=AF.Copy),
                               [], [sc.b, bk_b])
                    return sc

                def k_a(sc):
                    for g in range(16):
                        dve.op(lambda e: e.max(out=mx[:, g, 0:8], in_=sc[:, g, :]), [sc.b], [mx.bs[g]])
                    for g in range(16):
                        dve.op(lambda e: e.max_index(out=idx[:, g, 0:8], in_max=mx[:, g, 0:8], in_values=sc[:, g, :]), [sc.b, mx.bs[g]], [idx.bs[g]])
                    for g in range(16):
                        dve.op(lambda e: e.match_replace(out=sc2[:, g, :], in_to_replace=mx[:, g, 0:8], in_values=sc[:, g, :], imm_value=-1e30),
                               [sc.b, mx.bs[g]], [sc2.bs[g]])
                    for g in range(16):
                        dve.op(lambda e: e.max(out=mx[:, g, 8:16], in_=sc2[:, g, :]), [sc2.bs[g]], [mx.bs[g]])
                    for g in range(16):
                        dve.op(lambda e: e.max_index(out=idx[:, g, 8:16], in_max=mx[:, g, 8:16], in_values=sc2[:, g, :]), [sc2.bs[g], mx.bs[g]], [idx.bs[g]])
                    dve.op(lambda e: e.tensor_copy(out=idxf[:], in_=idx[:]), idx.bs, [idxf.b])

                def k_b(t, sub):
                    ncol = slice(t * NT + sub * 128, t * NT + (sub + 1) * 128)
                    mxv = mx[:].rearrange("p (h two) k -> p h two k", two=2)
                    idv = idxf[:].rearrange("p (h two) k -> p h two k", two=2)
                    c1 = cand[:, :, 0:64].rearrange("p h (a b) -> p h a b", b=16)
                    dve.op(lambda e: e.tensor_tensor(out=c1, in0=mxv[:, :, 0, 0:4].unsqueeze(3).to_broadcast([128, 8, 4, 16]),
                                                     in1=mxv[:, :, 1, :].unsqueeze(2).to_broadcast([128, 8, 4, 16]), op=ALU.add), mx.bs, [cand.b])
                    c2 = cand[:, :, 64:112].rearrange("p h (a b) -> p h a b", b=4)
                    dve.op(lambda e: e.tensor_tensor(out=c2, in0=mxv[:, :, 0, 4:16].unsqueeze(3).to_broadcast([128, 8, 12, 4]),
                                                     in1=mxv[:, :, 1, 0:4].unsqueeze(2).to_broadcast([128, 8, 12, 4]), op=ALU.add), mx.bs, [cand.b])
                    for h in range(8):
                        dve.op(lambda e: e.max(out=m2[:, h, 0:8], in_=cand[:, h, :]), [cand.b], [m2.bs[h]])
                    for h in range(8):
                        dve.op(lambda e: e.max_index(out=pos[:, h, 0:8], in_max=m2[:, h, 0:8], in_values=cand[:, h, :]), [cand.b, m2.bs[h]], [pos.bs[h]])
                    for h in range(8):
                        dve.op(lambda e: e.match_replace(out=cand2[:, h, :], in_to_replace=m2[:, h, 0:8], in_values=cand[:, h, :], imm_value=-1e30),
                               [cand.b, m2.bs[h]], [cand2.bs[h]])
                    for h in range(8):
                        dve.op(lambda e: e.max(out=m2[:, h, 8:16], in_=cand2[:, h, :]), [cand2.bs[h]], [m2.bs[h]])
                    for h in range(8):
                        dve.op(lambda e: e.max_index(out=pos[:, h, 8:16], in_max=m2[:, h, 8:16], in_values=cand2[:, h, :]), [cand2.bs[h], m2.bs[h]], [pos.bs[h]])
                    dve.op(lambda e: e.tensor_single_scalar(out=pa[:, 0], in_=pos[:], scalar=4, op=ALU.logical_shift_right), pos.bs, [pa.b])
                    dve.op(lambda e: e.tensor_single_scalar(out=pa[:, 1], in_=pos[:], scalar=15, op=ALU.bitwise_and), pos.bs + [pa.b], [pa.b])
                    dve.op(lambda e: e.tensor_single_scalar(out=pa[:, 2], in_=pos[:], scalar=2, op=ALU.logical_shift_right), pos.bs + [pa.b], [pa.b])
                    dve.op(lambda e: e.tensor_single_scalar(out=pa[:, 3], in_=pos[:], scalar=3, op=ALU.bitwise_and), pos.bs + [pa.b], [pa.b])
                    dve.op(lambda e: e.tensor_copy(out=paf[:], in_=pa[:]), [pa.b], [paf.b])
                    dve.op(lambda e: e.tensor_single_scalar(out=abf[:, 2], in_=paf[:, 0], scalar=4.0, op=ALU.is_ge), [paf.b], [abf.b])
                    dve.op(lambda e: e.scalar_tensor_tensor(out=abf[:, 0], in0=paf[:, 2], scalar=-12.0, in1=paf[:, 0], op0=ALU.add, op1=ALU.subtract),
                           [paf.b, abf.b], [abf.b])
                    dve.op(lambda e: e.tensor_tensor(out=abf[:, 1], in0=paf[:, 3], in1=paf[:, 1], op=ALU.subtract), [paf.b, abf.b], [abf.b])
                    dve.op(lambda e: e.tensor_tensor(out=abf[:, 0:2], in0=abf[:, 0:2], in1=abf[:, 2:3].to_broadcast([128, 2, 8, 16]), op=ALU.mult),
                           [abf.b], [abf.b])
                    dve.op(lambda e: e.tensor_tensor(out=abf[:, 0:2], in0=abf[:, 0:2], in1=paf[:, 0:2], op=ALU.add), [abf.b, paf.b], [abf.b])
                    for w in range(2):
                        dve.op(lambda e: e.tensor_tensor(out=oh[:], in0=io16[:], in1=abf[:, w].unsqueeze(3).to_broadcast([128, 8, 16, 16]), op=ALU.is_equal),
                               [io16.b, abf.b], [oh.b])
                        dve.op(lambda e: e.tensor_tensor(out=oh[:], in0=oh[:], in1=idv[:, :, w, :].unsqueeze(2).to_broadcast([128, 8, 16, 16]), op=ALU.mult),
                               [oh.b, idxf.b], [oh.b])
                        dve.op(lambda e: e.tensor_reduce(out=IJG[:, w, :].rearrange("p (h k) -> p h k", k=16), in_=oh[:], axis=AX.X, op=ALU.add),
                               [oh.b], [IJG.bs[w]])
                    g3 = IJG[:, 2, :].rearrange("p (h k) -> p h k", k=16)
                    gb = IJG.bs[2]
                    dve.op(lambda e: e.tensor_tensor(out=g3, in0=m2[:], in1=m2[:, :, 0:1].to_broadcast([128, 8, 16]), op=ALU.subtract), m2.bs, [gb])
                    act.op(lambda e: e.activation(out=IJG[:, 2, :], in_=IJG[:, 2, :], func=AF.Exp), [gb], [gb])
                    dve.op(lambda e: e.tensor_reduce(out=gz[:, 0:8], in_=g3, axis=AX.X, op=ALU.add), [gb], [gz.b])
                    dve.op(lambda e: e.reciprocal(out=gz[:, 8:16], in_=gz[:, 0:8]), [gz.b], [gz.b])
                    dve.op(lambda e: e.tensor_tensor(out=g3, in0=g3, in1=gz[:, 8:16].unsqueeze(2).to_broadcast([128, 8, 16]), op=ALU.mult), [gb, gz.b], [gb])
                    bk, bk_b = fw.bank()
                    for a in range(3):
                        pe.op(lambda e: e.matmul(bk[:, a * 128:(a + 1) * 128], lhsT=IJG[:, a, :], rhs=ident_f[:], start=True, stop=True),
                              [IJG.bs[a], ident_f.b], [bk_b], inc=(a == 2))
                    lg = lstg.next()
                    act.op(lambda e: e.activation(out=lg[:], in_=bk[:, 0:384].rearrange("p (a n) -> p a n", n=128), func=AF.Copy),
                           [], [lg.b, bk_b])
                    sp.dma(L.LSTd[:, :, ncol], lg[:], [lg.b], [], lg.d)

                NTL = S // NT
                load_p(0)
                p1(0)
                qn = p2(0)
                for t in range(NTL):
                    qc = qn
                    if t + 1 < NTL:
                        load_p(t + 1)
                    s0 = stage_s(qc, 0)
                    s1 = stage_s(qc, 1)
                    k_a(s0)
                    if t + 1 < NTL:
                        p1(t + 1)
                    k_b(t, 0)
                    k_a(s1)
                    if t + 1 < NTL:
                        qn = p2(t + 1)
                    k_b(t, 1)
                fw.barrier()
        if "E" in phases:
            with ExitStack() as ph:
                g_fin = T(fw, ph, "g_fin", [128, 8], F32, dma=True)
                sp.dma(g_fin[:], I["g_fin"][:, :], [], [g_fin.b], g_fin.d)
                iof = T(fw, ph, "iof", [128, 128], F32, dma=True)
                sp.dma(iof[:], I["iota"][:, :], [], [iof.b], iof.d)
                NG = 8
                NTILE = S // NT
                io3 = T(fw, ph, "io3", [128, NG, 128], BF16)
                for r in range(NG):
                    dve.op(lambda e: e.tensor_copy(out=io3[:, r, :], in_=iof[:]), [iof.b], [io3.b])
                H = [T(fw, ph, "GTh%d" % i, [128, NT, 64], BF16) for i in range(2)]
                Ar = Ring([T(fw, ph, "A%d" % i, [128, NG, 64], BF16) for i in range(3)])
                Br = Ring([T(fw, ph, "B%d" % i, [128, NG, 128], BF16) for i in range(3)])
                lstr = Ring([T(fw, ph, "lst%d" % i, [128, 3, NT], BF16, dma=True) for i in range(3)])
                wdr = Ring([T(fw, ph, "wd%d" % i, [128, 8, 512], BF16, dma=True) for i in range(2)])
                wur = Ring([T(fw, ph, "wu%d" % i, [128, 4, 1024], BF16, dma=True) for i in range(3)])
                h2r = Ring([T(fw, ph, "h2e%d" % i, [128, 8, NT], BF16, dma=True) for i in range(2)])
                x1r = Ring([T(fw, ph, "x1e%d" % i, [128, 8, NT], F32, dma=True) for i in range(2)])
                ost = T(fw, ph, "ost", [128, 8, NT], F32, dma=True)
                LAG = 2
                gar = Ring([T(fw, ph, "ga%d" % i, [128, NT], F32) for i in range(3)])
                awr = Ring([T(fw, ph, "aw%d" % i, [128, NT], BF16) for i in range(LAG + 3)])
                x1_v = L.x1T.rearrange("(c p) n -> p c n", p=128)
                h2_v = L.h2T_d.rearrange("(c p) n -> p c n", p=128)
                oT_v = L.outT.rearrange("(c p) n -> p c n", p=128)
                Ob = [fw.banks[i] for i in range(4)]
                Ab = Ring([fw.banks[i] for i in (4, 5)])
                Gb = Ring([fw.banks[i] for i in (6, 7)])

                lists = {}

                def load_lists(t_):
                    lt_ = lstr.next()
                    sp.dma(lt_[:], L.LSTd[:, :, t_ * NT:(t_ + 1) * NT], [], [lt_.b], lt_.d)
                    lists[t_] = lt_

                units = [(0, 0, g) for g in range(NT // NG)]
                for t_ in range(NTILE):
                    units += [(t_, 1, g) for g in range(NT // NG)]
                    if t_ + 1 < NTILE:
                        units += [(t_ + 1, 0, g) for g in range(NT // NG)]
                ust = {"next1": 0, "next2": 0, "ops": {}}

                def g_stage1(u):
                    t_, half, grp = units[u]
                    LT = lists[t_]
                    n0 = grp * NG
                    A, B = Ar.next(), Br.next()
                    dve.op(lambda e: e.tensor_tensor(out=A[:], in0=io3[:, :, half * 64:(half + 1) * 64],
                                                     in1=LT[:, 0, n0:n0 + NG].unsqueeze(2).to_broadcast([128, NG, 64]), op=ALU.is_equal),
                           [io3.b, LT.b], [A.b])
                    pool.op(lambda e: e.tensor_tensor(out=A[:], in0=A[:], in1=LT[:, 2, n0:n0 + NG].unsqueeze(2).to_broadcast([128, NG, 64]), op=ALU.mult),
                            [A.b, LT.b], [A.b])
                    dve.op(lambda e: e.tensor_tensor(out=B[:], in0=io3[:], in1=LT[:, 1, n0:n0 + NG].unsqueeze(2).to_broadcast([128, NG, 128]), op=ALU.is_equal),
                           [io3.b, LT.b], [B.b])
                    ust["ops"][u] = (A, B)

                def g_stage2(u):
                    t_, half, grp = units[u]
                    A, B = ust["ops"].pop(u)
                    bk, bk_b = Gb.next()
                    for r in range(NG):
                        pe.op(lambda e: e.matmul(bk[:, r * 64:(r + 1) * 64], lhsT=B[:, r, :], rhs=A[:, r, :], start=True, stop=True),
                              [A.b, B.b], [bk_b], inc=(r == NG - 1))
                    nl = grp * NG
                    G = H[half]
                    act.op(lambda e: e.activation(out=G[:, nl:nl + NG, :], in_=bk[:, :].rearrange("j (n i) -> j n i", i=64), func=AF.Copy),
                           [], [G.b, bk_b])

                def g_step():
                    if ust["next2"] >= len(units):
                        return
                    while ust["next1"] <= min(ust["next2"] + 2, len(units) - 1) and units[ust["next1"]][0] in lists:
                        g_stage1(ust["next1"])
                        ust["next1"] += 1
                    g_stage2(ust["next2"])
                    ust["next2"] += 1

                load_lists(0)
                for _ in range(NT // NG):
                    g_step()

                tiles = {}

                def load_tile(t_):
                    h2_, x1_ = h2r.next(), x1r.next()
                    ts_ = slice(t_ * NT, (t_ + 1) * NT)
                    sp.dma(h2_[:], h2_v[:, :, ts_], [], [h2_.b], h2_.d)
                    sp.dma(x1_[:], x1_v[:, :, ts_], [], [x1_.b], x1_.d)
                    tiles[t_] = (h2_, x1_)

                wts = {}

                def load_w(gi):
                    ig = gi % 32
                    wd_, wu_ = wdr.next(), wur.next()
                    sp.dma(wd_[:], L.wdT_bf[ig], [], [wd_.b], wd_.d)
                    sp.dma(wu_[:], L.wup_bf[ig], [], [wu_.b], wu_.d)
                    wts[gi] = (wd_, wu_)

                deferred = [None]
                load_tile(0)
                load_w(0)
                for t in range(NTILE):
                    ts = slice(t * NT, (t + 1) * NT)
                    h2, x1 = tiles.pop(t)
                    if t + 1 < NTILE:
                        load_lists(t + 1)
                        if deferred[0] is None:
                            load_tile(t + 1)
                    for j in range(4):
                        ob, ob_b = Ob[j]
                        dve.op(lambda e: e.memset(ob[:, :], 0.0), [], [ob_b])
                    pend = []

                    def emit_up(item):
                        p_aw, p_wu, p_ii = item
                        for dc in range(8):
                            ob, ob_b = Ob[dc // 2]
                            pe.op(lambda e: e.matmul(ob[:, (dc % 2) * NT:(dc % 2 + 1) * NT], lhsT=p_wu[:, p_ii, dc * 128:(dc + 1) * 128], rhs=p_aw[:],
                                                     start=False, stop=False, skip_group_check=True), [p_wu.b, p_aw.b], [ob_b], inc=(dc == 7))

                    for i in range(128):
                        if i == 6 and deferred[0] is not None:
                            deferred[0]()
                            deferred[0] = None
                            if t + 1 < NTILE:
                                load_tile(t + 1)
                        ii = i % 4
                        gi = t * 32 + i // 4
                        if ii == 0 and gi + 1 < NTILE * 32:
                            load_w(gi + 1)
                        wd, wu = wts[gi]
                        if i % 2 == 0:
                            g_step()
                        ab, ab_b = Ab.next()
                        for c in range(8):
                            pe.op(lambda e: e.matmul(ab[:, 0:NT], lhsT=wd[:, c, ii * 128:(ii + 1) * 128], rhs=h2[:, c, :], start=(c == 0), stop=(c == 7)),
                                  [wd.b, h2.b], [ab_b], inc=(c == 7))
                        ga, aw = gar.next(), awr.next()
                        act.op(lambda e: e.activation(out=ga[:], in_=ab[:, 0:NT], func=AF.Gelu), [], [ga.b, ab_b])
                        G = H[i // 64]
                        dve.op(lambda e: e.tensor_tensor(out=aw[:], in0=ga[:], in1=G[:, :, i % 64], op=ALU.mult), [ga.b, G.b], [aw.b])
                        pend.append((aw, wu, ii))
                        if len(pend) > LAG:
                            emit_up(pend.pop(0))
                        if ii == 3:
                            wts.pop(gi - 1, None)
                    while pend:
                        emit_up(pend.pop(0))
                    for dc in range(8):
                        ob, ob_b = Ob[dc // 2]
                        dve.op(lambda e: e.tensor_tensor(out=x1[:, dc, :], in0=ob[:, (dc % 2) * NT:(dc % 2 + 1) * NT], in1=x1[:, dc, :], op=ALU.add),
                               [x1.b], [x1.b, ob_b])

                    def fin(x1=x1, ts=ts):
                        L.rms_a(x1, NT, bank=Ab.next())
                        L.rms_b(x1, g_fin, ost, NT)
                        sp.dma(oT_v[:, :, ts], ost[:], [ost.b], [], ost.d)

                    deferred[0] = fin
                deferred[0]()
                fw.barrier()


def kernel(**inputs):
    inp = {k: np.asarray(v) for k, v in inputs.items()}
    shared = _host_prep(inp)
    nc = build_program()
    x = np.asarray(inp["x"], np.float32)
    in_maps = []
    for c in range(8):
        m = dict(shared)
        m["xT"] = np.ascontiguousarray(x[c].T)
        in_maps.append(m)
    res = run_bass_kernel_spmd(nc, in_maps, core_ids=list(range(8)))
    out = np.stack([np.ascontiguousarray(np.asarray(res.results[c]["outT"]).T) for c in range(8)], axis=0)
    return out.astype(np.float32)
```

```python
import math
import os
from contextlib import ExitStack

import numpy as np
import concourse.bass as bass
import concourse.mybir as mybir
from concourse.bass_utils import run_bass_kernel_spmd

F32 = mybir.dt.float32
BF16 = mybir.dt.bfloat16
AF = mybir.ActivationFunctionType
ALU = mybir.AluOpType
AX = mybir.AxisListType

S = 4096
D = 1024
NC8 = 8
EPS = 1e-6
LAMBDA_INIT = 0.8 - 0.6 * math.exp(-0.3 * 0)
NEG = -30000.0


class TB:
    __slots__ = ("name", "w", "r")

    def __init__(self, name):
        self.name = name
        self.w = {}
        self.r = {}


class DSem:
    __slots__ = ("sem", "total")

    def __init__(self, sem):
        self.sem = sem
        self.total = 0


class Eng:
    def __init__(self, eng, sem, name, is_pe=False):
        self.eng = eng
        self.sem = sem
        self.name = name
        self.is_pe = is_pe
        self.cnt = 0
        self.seen = {}
        self.pend_r = []
        self.pend_w = []

    def _wait(self, evs):
        need = {}
        for so, v in evs:
            if isinstance(so, DSem):
                key, val = so.sem, so.total
            else:
                key, val = so.sem, v
            if need.get(key, 0) < val:
                need[key] = val
        for key, val in need.items():
            if self.seen.get(key, 0) < val:
                self.eng.wait_ge(key, val)
                self.seen[key] = val

    def _deps(self, reads, writes, own_dsem=None):
        evs = []
        for b in reads:
            for so, v in b.w.items():
                if so is self and self.is_pe:
                    continue
                evs.append((so, v))
        for b in writes:
            for so, v in b.w.items():
                if so is own_dsem or (so is self and self.is_pe):
                    continue
                evs.append((so, v))
            for so, v in b.r.items():
                if so is self and self.is_pe:
                    continue
                evs.append((so, v))
        return evs

    def op(self, fn, reads=(), writes=(), inc=True):
        self._wait(self._deps(reads, writes))
        ins = fn(self.eng)
        self.pend_r.extend(reads)
        self.pend_w.extend(writes)
        if inc:
            self.cnt += 1
            ins.then_inc(self.sem, 1)
            for b in self.pend_r:
                b.r[self] = self.cnt
            for b in self.pend_w:
                b.w = {self: self.cnt}
                b.r = {}
            self.pend_r = []
            self.pend_w = []
        return ins

    def dma(self, out, in_, reads, writes, dsem):
        assert not self.pend_r and not self.pend_w
        self._wait(self._deps(reads, writes, own_dsem=dsem))
        ins = self.eng.dma_start(out=out, in_=in_)
        dsem.total += 16
        ins.then_inc(dsem.sem, 16)
        for b in reads:
            b.r[dsem] = dsem.total
        for b in writes:
            b.w = {dsem: dsem.total}
            b.r = {}
        return ins


class FW:
    def __init__(self, nc, es):
        self.nc = nc
        self.es = es
        mk = lambda n: es.enter_context(nc.semaphore(n))
        self.pe = Eng(nc.tensor, mk("s_pe"), "pe", is_pe=True)
        self.act = Eng(nc.scalar, mk("s_act"), "act")
        self.dve = Eng(nc.vector, mk("s_dve"), "dve")
        self.pool = Eng(nc.gpsimd, mk("s_pool"), "pool")
        self.sp = Eng(nc.sync, mk("s_sp"), "sp")
        self.engs = [self.pe, self.act, self.dve, self.pool, self.sp]
        self.dsems = []
        self.nsem = 0
        self.banks = []
        self.bank_i = 0

    def dsem(self, name=None):
        self.nsem += 1
        d = DSem(self.es.enter_context(self.nc.semaphore(name or ("d%d" % self.nsem))))
        self.dsems.append(d)
        return d

    def barrier(self):
        for e in self.engs:
            evs = [(o, o.cnt) for o in self.engs if o is not e and o.cnt > 0]
            evs += [(d, d.total) for d in self.dsems if d.total > 0]
            e._wait(evs)

    def sb(self, stack, name, shape, dtype):
        t = stack.enter_context(self.nc.sbuf_tensor("sb_" + name, list(shape), dtype))
        return t, TB(name)

    def init_psum(self, stack):
        for i in range(8):
            t = stack.enter_context(self.nc.psum_tensor("bank%d" % i, [128, 512], F32))
            self.banks.append((t, TB("bank%d" % i)))

    def bank(self):
        b = self.banks[self.bank_i % 8]
        self.bank_i += 1
        return b


class T:
    def __init__(self, fw, stack, name, shape, dtype, dma=False, parts=0):
        self.t, self.b = fw.sb(stack, name, shape, dtype)
        self.d = fw.dsem("ds_" + name) if dma else None
        self.bs = [TB("%s.%d" % (name, i)) for i in range(parts)]

    def __getitem__(self, k):
        return self.t[k]


class Ring:
    def __init__(self, tiles):
        self.tiles = tiles
        self.i = 0

    def next(self):
        t = self.tiles[self.i % len(self.tiles)]
        self.i += 1
        return t


def _t5_bucket_np(n):
    n = np.maximum(n, 0)
    max_exact = 16
    nf = np.maximum(n, 1).astype(np.float32) / np.float32(max_exact)
    large = max_exact + (np.log(nf).astype(np.float32) / np.float32(math.log(128 / max_exact)) * np.float32(32 - max_exact)).astype(np.int32)
    large = np.minimum(large, 31)
    return np.where(n < max_exact, n, large)


def _const_tables():
    f32 = np.float32
    pos = np.arange(S, dtype=f32)
    freqs = (np.float32(10000.0) ** (-np.arange(32, dtype=f32) / np.float32(32))).astype(f32)
    ang = (pos[:, None] * freqs[None, :]).astype(f32)
    cos, sin = np.cos(ang).astype(f32), np.sin(ang).astype(f32)
    d = np.arange(128) % 64
    j = d % 32
    sign = np.where(d < 32, -1.0, 1.0).astype(f32)
    cosT2 = np.ascontiguousarray(cos[:, j].T)
    sinT2 = np.ascontiguousarray((sin[:, j] * sign[None, :]).T)
    gam = 1.0 - 2.0 ** (-5.0 - np.arange(8, dtype=np.float64))
    idx = np.arange(128, dtype=np.float64)
    dist = idx[None, :] - idx[:, None]
    decT = np.where(dist >= 0, gam[:, None, None] ** np.maximum(dist, 0.0)[None], 0.0) * 0.125
    decT = np.ascontiguousarray(decT.transpose(1, 0, 2).reshape(128, 1024)).astype(f32)
    kd = gam[None, :] ** (127.0 - idx)[:, None] * 0.125
    kdec = np.ascontiguousarray(np.repeat(kd, 64, axis=1)).astype(f32)
    qd = gam[:, None] ** (idx + 1.0)[None, :]
    qdec = np.zeros((128, 4, 512), f32)
    for p in range(128):
        for pair in range(4):
            h = 2 * pair + p // 64
            qdec[p, pair, :] = np.tile(qd[h], 4)
    cdec = np.ascontiguousarray(np.repeat((gam ** 128.0)[None, :], 64, axis=1).repeat(64, axis=0)).astype(f32)
    ident = np.eye(128, dtype=f32)
    iota = np.ascontiguousarray(np.broadcast_to(np.arange(128, dtype=f32)[None, :], (128, 128)))
    iota16r = np.ascontiguousarray(np.broadcast_to((np.arange(2048) % 16).astype(f32)[None, :], (128, 2048)))
    return dict(cosT2=cosT2, sinT2=sinT2, decT=decT, kdec=kdec, qdec=qdec, cdec=cdec, ident=ident, iota=iota, iota16r=iota16r)


def _host_prep(inp):
    f32 = np.float32
    w_in = np.asarray(inp["w_in"][0], f32)

    def swap_cols(w):
        return np.ascontiguousarray(w.reshape(1024, 8, 2, 32)[:, :, ::-1, :]).reshape(1024, 512)

    w_in_ext = np.ascontiguousarray(np.concatenate([w_in, swap_cols(w_in[:, 0:512]), swap_cols(w_in[:, 512:1024])], axis=1))
    rel_bias = np.asarray(inp["rel_bias"], f32)
    k = np.arange(128)[:, None]
    q = np.arange(128)[None, :]
    b0 = _t5_bucket_np(q - k)
    b1 = _t5_bucket_np(128 + q - k)
    biasT = np.zeros((128, 4, 2, 128), f32)
    for h in range(4):
        biasT[:, h, 0, :] = np.where(q >= k, rel_bias[b0, h], f32(NEG))
        biasT[:, h, 1, :] = rel_bias[b1, h]
    shared = dict(
        w_in_ext=w_in_ext,
        w_out=np.ascontiguousarray(inp["w_out"][0], dtype=f32),
        w_pq=np.ascontiguousarray(inp["peer_query"][0], dtype=f32),
        keysT=np.ascontiguousarray(np.asarray(inp["peer_keys"][0], f32).reshape(16, 128, 128).transpose(2, 0, 1)),
        w_downT=np.ascontiguousarray(np.asarray(inp["peer_down"][0], f32).T),
        w_up=np.ascontiguousarray(inp["peer_up"][0], dtype=f32),
        g_mix=np.ascontiguousarray(np.asarray(inp["norm_mix"][0], f32).reshape(8, 128).T),
        g_ffn=np.ascontiguousarray(np.asarray(inp["norm_ffn"][0], f32).reshape(8, 128).T),
        g_fin=np.ascontiguousarray(np.asarray(inp["norm_final"], f32).reshape(8, 128).T),
        retgn_b=np.ascontiguousarray(np.broadcast_to(np.asarray(inp["ret_gn"][0], f32)[None, :], (128, 512))),
        subln_b=np.ascontiguousarray(np.broadcast_to(np.asarray(inp["diff_subln"][0], f32)[None, :], (128, 128))),
        lamvec=np.ascontiguousarray(np.broadcast_to(np.stack([np.asarray(inp[n][0], f32) for n in
                                    ("diff_lambda_q1", "diff_lambda_k1", "diff_lambda_q2", "diff_lambda_k2")])[None], (128, 4, 64))),
        cbias=np.ascontiguousarray(np.broadcast_to(rel_bias[31][None, :], (128, 4))),
        biasT=biasT,
    )
    shared.update(_const_tables())
    return shared


IN_SPECS = [
    ("xT", [1024, S]), ("w_in_ext", [1024, 4608]), ("w_out", [1024, 1024]), ("w_pq", [1024, 2048]),
    ("keysT", [128, 16, 128]), ("w_downT", [1024, 16384]), ("w_up", [16384, 1024]),
    ("g_mix", [128, 8]), ("g_ffn", [128, 8]), ("g_fin", [128, 8]), ("retgn_b", [128, 512]), ("subln_b", [128, 128]),
    ("lamvec", [128, 4, 64]), ("cbias", [128, 4]), ("biasT", [128, 4, 2, 128]),
    ("cosT2", [128, S]), ("sinT2", [128, S]), ("decT", [128, 1024]), ("kdec", [128, 512]), ("qdec", [128, 4, 512]),
    ("cdec", [64, 512]), ("ident", [128, 128]), ("iota", [128, 128]), ("iota16r", [128, 2048]),
]


def build_program(dbg=False, phases="ABCDE"):
    nc = bass.Bass("TRN2", target_bir_lowering=False)
    I = {}
    for name, shape in IN_SPECS:
        I[name] = nc.dram_tensor(name, list(shape), F32, kind="ExternalInput").ap()
    outT = nc.dram_tensor("outT", [1024, S], F32, kind="ExternalOutput").ap()
    SK = "ExternalOutput" if dbg else "Internal"
    scr = lambda name, shape, dtype: nc.dram_tensor(name, list(shape), dtype, kind=SK).ap()
    QT_ret = scr("QT_ret", [512, S], BF16)
    QdT_ret = scr("QdT_ret", [512, S], BF16)
    KT_ret = scr("KT_ret", [512, S], BF16)
    QT_dif = scr("QT_dif", [512, S], BF16)
    KT_dif = scr("KT_dif", [512, S], BF16)
    V_ret = scr("V_ret", [S, 512], BF16)
    SG = scr("SG", [S, 512], F32)
    V_dif = scr("V_dif", [S, 512], BF16)
    mixT = scr("mixT", [1024, S], BF16)
    x1T = scr("x1T", [1024, S], F32)
    h2T_d = scr("h2T_d", [1024, S], BF16)
    LSTd = scr("LSTd", [128, 3, S], BF16)
    wdT_bf = nc.dram_tensor("wdT_bf", [32, 128, 8, 512], BF16, kind="Internal").ap()
    wup_bf = nc.dram_tensor("wup_bf", [32, 128, 4, 1024], BF16, kind="Internal").ap()
    wout_bf = nc.dram_tensor("wout_bf", [1024, 1024], BF16, kind="Internal").ap()
    wpq_bf = nc.dram_tensor("wpq_bf", [1024, 2048], BF16, kind="Internal").ap()
    keys_bf = nc.dram_tensor("keys_bf", [128, 16, 128], BF16, kind="Internal").ap()

    with ExitStack() as es:
        fw = FW(nc, es)
        fw.init_psum(es)
        pe, act, dve, pool, sp = fw.pe, fw.act, fw.dve, fw.pool, fw.sp
        out_dsems = []

        cst = ExitStack()
        es.enter_context(cst)
        ones_f = T(fw, cst, "ones_f", [128, 128], F32)
        epst = T(fw, cst, "epst", [128, 1], F32)
        ident_f = T(fw, cst, "ident_f", [128, 128], F32, dma=True)
        ident_b = T(fw, cst, "ident_b", [128, 128], BF16)
        sqr = Ring([T(fw, cst, "sq%d" % i, [128, 512], F32) for i in range(2)])
        sd = T(fw, cst, "sd", [128, 512], F32)
        dve.op(lambda e: e.memset(ones_f[:], 1.0), [], [ones_f.b])
        neghalf = T(fw, cst, "neghalf", [128, 16], F32)
        dve.op(lambda e: e.memset(neghalf[:], -0.5), [], [neghalf.b])
        dve.op(lambda e: e.memset(epst[:], EPS), [], [epst.b])
        sp.dma(ident_f[:], I["ident"][:, :], [], [ident_f.b], ident_f.d)
        dve.op(lambda e: e.tensor_copy(out=ident_b[:], in_=ident_f[:]), [ident_f.b], [ident_b.b])

        wcast = fw.dsem("wcast")
        wd_b = TB("wdT_bf")
        wu_b = TB("wup_bf")

        wdv = I["w_downT"].rearrange("(c p) e -> p c e", p=128)
        wuv = I["w_up"].rearrange("(i j) d -> j i d", j=128)
        wcast_todo = []
        for ig in range(32):
            wcast_todo.append(lambda ig=ig: pool.dma(wdT_bf[ig], wdv[:, :, ig * 512:(ig + 1) * 512], [], [wd_b], wcast))
            wcast_todo.append(lambda ig=ig: pool.dma(wup_bf[ig], wuv[:, ig * 4:(ig + 1) * 4, :], [], [wu_b], wcast))

        wsm_b = TB("wsmall_bf")
        for r in range(2):
            wcast_todo.append(lambda r=r: pool.dma(wout_bf[r * 512:(r + 1) * 512, :], I["w_out"][r * 512:(r + 1) * 512, :], [], [wsm_b], wcast))
        for r in range(4):
            wcast_todo.append(lambda r=r: pool.dma(wpq_bf[r * 256:(r + 1) * 256, :], I["w_pq"][r * 256:(r + 1) * 256, :], [], [wsm_b], wcast))
        wcast_todo.append(lambda: pool.dma(keys_bf[:, :, :], I["keysT"][:, :, :], [], [wsm_b], wcast))

        def issue_wcasts(n=None):
            k = len(wcast_todo) if n is None else min(n, len(wcast_todo))
            for _ in range(k):
                wcast_todo.pop(0)()

        def rms_a(xt, N, bank=None):
            bk, bk_b = bank if bank is not None else fw.bank()
            for c in range(8):
                sq = sqr.next()
                act.op(lambda e: e.activation(out=sq[:, 0:N], in_=xt[:, c, 0:N], func=AF.Square), [xt.b], [sq.b])
                pe.op(lambda e: e.matmul(bk[:, 0:N], lhsT=ones_f[:], rhs=sq[:, 0:N], start=(c == 0), stop=(c == 7)),
                      [ones_f.b, sq.b], [bk_b])
            act.op(lambda e: e.activation(out=sd[:, 0:N], in_=bk[:, 0:N], func=AF.Sqrt, bias=epst[:, 0:1], scale=1.0 / D),
                   [epst.b], [sd.b, bk_b])

        def rms_b(xt, g, out, N):
            dve.op(lambda e: e.reciprocal(out=sd[:, 0:N], in_=sd[:, 0:N]), [sd.b], [sd.b])
            for c in range(8):
                dve.op(lambda e: e.scalar_tensor_tensor(out=out[:, c, 0:N], in0=xt[:, c, 0:N], scalar=g[:, c:c + 1], in1=sd[:, 0:N],
                                                        op0=ALU.mult, op1=ALU.mult), [xt.b, g.b, sd.b], [out.b])

        def rmsnorm(xt, g, out, N, act_sq=True):
            rms_a(xt, N)
            rms_b(xt, g, out, N)

        if "A" in phases:
            with ExitStack() as ph:
                w_in = T(fw, ph, "w_in", [128, 8, 4608], BF16, dma=True)
                for c in range(8):
                    pool.dma(w_in[:, c, :], I["w_in_ext"][c * 128:(c + 1) * 128, :], [], [w_in.b], w_in.d)
                g_mix = T(fw, ph, "g_mix", [128, 8], F32, dma=True)
                sp.dma(g_mix[:], I["g_mix"][:, :], [], [g_mix.b], g_mix.d)
                qdec = T(fw, ph, "qdec", [128, 4, 512], F32, dma=True)
                sp.dma(qdec[:], I["qdec"][:, :, :], [], [qdec.b], qdec.d)
                xr = Ring([T(fw, ph, "xa%d" % i, [128, 8, 512], F32, dma=True) for i in range(2)])
                cr = Ring([T(fw, ph, "cs%d" % i, [128, 2, 512], F32, dma=True) for i in range(2)])
                hTr = Ring([T(fw, ph, "hT%d" % i, [128, 8, 512], BF16) for i in range(2)])
                t1r = Ring([T(fw, ph, "t1_%d" % i, [128, 512], F32) for i in range(3)])
                t2r = Ring([T(fw, ph, "t2_%d" % i, [128, 512], F32) for i in range(3)])
                sbf = Ring([T(fw, ph, "sbf%d" % i, [128, 512], BF16, dma=True) for i in range(8)])
                sf3 = Ring([T(fw, ph, "sf3_%d" % i, [128, 512], F32, dma=True) for i in range(3)])
                xT_v = I["xT"].rearrange("(c p) n -> p c n", p=128)

                def store(eng_q, dst, st):
                    eng_q.dma(dst, st[:], [st.b], [], st.d)

                loaded = {}

                def load_x(t_):
                    ts_ = slice(t_ * 512, (t_ + 1) * 512)
                    xt = xr.next()
                    sp.dma(xt[:], xT_v[:, :, ts_], [], [xt.b], xt.d)
                    cs_ = cr.next()
                    sp.dma(cs_[:, 0, :], I["cosT2"][:, ts_], [], [cs_.b], cs_.d)
                    sp.dma(cs_[:, 1, :], I["sinT2"][:, ts_], [], [cs_.b], cs_.d)
                    loaded[t_] = (xt, cs_)

                def prep(t_):
                    xt, cs_ = loaded.pop(t_)
                    hT_ = hTr.next()
                    rmsnorm(xt, g_mix, hT_, 512)
                    return hT_, cs_

                load_x(0)
                nxt_prep = prep(0)
                for t in range(8):
                    ts = slice(t * 512, (t + 1) * 512)
                    hT, cs = nxt_prep
                    if t + 1 < 8:
                        load_x(t + 1)

                    def fm_group(col0):
                        bk, bk_b = fw.bank()
                        for c in range(8):
                            pe.op(lambda e: e.matmul(bk[:, :], lhsT=w_in[:, c, col0:col0 + 128], rhs=hT[:, c, :], start=(c == 0), stop=(c == 7)),
                                  [w_in.b, hT.b], [bk_b], inc=(c == 7))
                        return bk, bk_b

                    for kind, c0, c0s, dst in (("q", 0, 3584, QT_ret), ("k", 512, 4096, KT_ret)):
                        for j in range(4):
                            bp, bp_b = fm_group(c0 + j * 128)
                            bs, bs_b = fm_group(c0s + j * 128)
                            t1 = t1r.next()
                            t2 = t2r.next()
                            dve.op(lambda e: e.tensor_tensor(out=t1[:], in0=bp[:, :], in1=cs[:, 0, :], op=ALU.mult), [cs.b], [t1.b, bp_b])
                            dve.op(lambda e: e.tensor_tensor(out=t2[:], in0=bs[:, :], in1=cs[:, 1, :], op=ALU.mult), [cs.b], [t2.b, bs_b])
                            st = sbf.next()
                            if kind == "q":
                                dve.op(lambda e: e.tensor_tensor(out=t1[:], in0=t1[:], in1=t2[:], op=ALU.add), [t1.b, t2.b], [t1.b])
                                act.op(lambda e: e.activation(out=st[:], in_=t1[:], func=AF.Copy), [t1.b], [st.b])
                                store(sp, dst[j * 128:(j + 1) * 128, ts], st)
                                st2 = sbf.next()
                                pool.op(lambda e: e.tensor_tensor(out=st2[:], in0=t1[:], in1=qdec[:, j, :], op=ALU.mult), [t1.b, qdec.b], [st2.b])
                                store(sp, QdT_ret[j * 128:(j + 1) * 128, ts], st2)
                            else:
                                dve.op(lambda e: e.tensor_tensor(out=st[:], in0=t1[:], in1=t2[:], op=ALU.add), [t1.b, t2.b], [st.b])
                                store(sp, dst[j * 128:(j + 1) * 128, ts], st)
                    for c0, dst, scl in ((2048, QT_dif, 0.125), (2560, KT_dif, 1.0)):
                        for j in range(4):
                            bk, bk_b = fm_group(c0 + j * 128)
                            st = sbf.next()
                            act.op(lambda e: e.activation(out=st[:], in_=bk[:, :], func=AF.Copy, scale=scl), [], [st.b, bk_b])
                            store(sp, dst[j * 128:(j + 1) * 128, ts], st)
                    if t + 1 < 8:
                        nxt_prep = prep(t + 1)
                    for sub in range(4):
                        rows = slice(t * 512 + sub * 128, t * 512 + (sub + 1) * 128)
                        for gi, col0 in enumerate((1024, 1536, 3072)):
                            bk, bk_b = fw.bank()
                            for c in range(8):
                                pe.op(lambda e: e.matmul(bk[:, :], lhsT=hT[:, c, sub * 128:(sub + 1) * 128], rhs=w_in[:, c, col0:col0 + 512],
                                                         start=(c == 0), stop=(c == 7)), [w_in.b, hT.b], [bk_b], inc=(c == 7))
                            if gi == 0:
                                st = sbf.next()
                                act.op(lambda e: e.activation(out=st[:], in_=bk[:, :], func=AF.Copy), [], [st.b, bk_b])
                                store(sp, V_ret[rows, :], st)
                            elif gi == 1:
                                st = sf3.next()
                                act.op(lambda e: e.activation(out=st[:], in_=bk[:, :], func=AF.Silu), [], [st.b, bk_b])
                                store(sp, SG[rows, :], st)
                            else:
                                st = sbf.next()
                                dve.op(lambda e: e.tensor_copy(out=st[:], in_=bk[:, :]), [], [st.b, bk_b])
                                store(sp, V_dif[rows, :], st)
                fw.barrier()
        if "C" not in phases:
            issue_wcasts()

        if "B" in phases and "C" in phases:
            PHASE_C(locals(), with_B=True)
        else:
            PHASE_B(locals()) if "B" in phases else None
            PHASE_C(locals()) if "C" in phases else None
        PHASE_DE(locals(), phases) if ("D" in phases or "E" in phases) else None

        fw.barrier()
    return nc


class NS:
    def __init__(self, d):
        self.__dict__.update(d)


class RetentionB:
    def __init__(self, L, ph, bank_ring):
        self.L = L
        fw, I = L.fw, L.I
        self.fw = fw
        sp = fw.sp
        self.banks = bank_ring
        self.decT = T(fw, ph, "decT", [128, 1024], F32, dma=True)
        sp.dma(self.decT[:], I["decT"][:, :], [], [self.decT.b], self.decT.d)
        self.kdec = T(fw, ph, "kdec", [128, 512], F32, dma=True)
        sp.dma(self.kdec[:], I["kdec"][:, :], [], [self.kdec.b], self.kdec.d)
        self.cdec = T(fw, ph, "cdec", [64, 512], F32, dma=True)
        sp.dma(self.cdec[:], I["cdec"][:, :], [], [self.cdec.b], self.cdec.d)
        self.retgn = T(fw, ph, "retgn", [128, 512], F32, dma=True)
        sp.dma(self.retgn[:], I["retgn_b"][:, :], [], [self.retgn.b], self.retgn.d)
        self.Qr = Ring([T(fw, ph, "Qg%d" % i, [64, 8, 512], BF16, dma=True) for i in range(2)])
        self.Qdr = Ring([T(fw, ph, "Qdg%d" % i, [64, 8, 512], BF16, dma=True) for i in range(2)])
        self.Kr = Ring([T(fw, ph, "Kg%d" % i, [64, 8, 512], BF16, dma=True) for i in range(2)])
        self.Vr = Ring([T(fw, ph, "Vg%d" % i, [128, 4, 512], BF16, dma=True) for i in range(2)])
        self.SGr = Ring([T(fw, ph, "SGg%d" % i, [128, 4, 512], F32, dma=True) for i in range(2)])
        self.sTm_r = Ring([T(fw, ph, "sTm%d" % i, [128, 1024], BF16) for i in range(2)])
        self.Kp_r = Ring([T(fw, ph, "Kp%d" % i, [128, 512], BF16) for i in range(2)])
        self.R32 = T(fw, ph, "R32", [64, 512], F32)
        self.Rb_r = Ring([T(fw, ph, "Rb%d" % i, [64, 512], BF16) for i in range(2)])
        self.ro = T(fw, ph, "ro", [128, 512], F32)
        self.sq = T(fw, ph, "rsq", [128, 512], F32)
        self.y = T(fw, ph, "ry", [128, 512], F32)
        self.st8 = T(fw, ph, "st8", [128, 32], F32)
        self.mtok = T(fw, ph, "mtok", [128, 512], BF16)
        self.mst_r = Ring([T(fw, ph, "mst%d" % i, [128, 4, 512], BF16, dma=True) for i in range(2)])
        fw.dve.op(lambda e: e.memset(self.R32[:], 0.0), [], [self.R32.b])
        self.Rb_prev = None
        self.groups = {}
        self.st = {}
        self.mst = None

    def load_group(self, tg):
        L, sp = self.L, self.fw.sp
        ts = slice(tg * 512, (tg + 1) * 512)
        Qg, Qdg, Kg, Vg, SGg = self.Qr.next(), self.Qdr.next(), self.Kr.next(), self.Vr.next(), self.SGr.next()
        sp.dma(Qg[:], L.QT_ret.rearrange("(h d) n -> d h n", d=64)[:, :, ts], [], [Qg.b], Qg.d)
        sp.dma(Qdg[:], L.QdT_ret.rearrange("(h d) n -> d h n", d=64)[:, :, ts], [], [Qdg.b], Qdg.d)
        sp.dma(Kg[:], L.KT_ret.rearrange("(h d) n -> d h n", d=64)[:, :, ts], [], [Kg.b], Kg.d)
        sp.dma(Vg[:], L.V_ret[ts, :].rearrange("(c p) f -> p c f", p=128), [], [Vg.b], Vg.d)
        sp.dma(SGg[:], L.SG[ts, :].rearrange("(c p) f -> p c f", p=128), [], [SGg.b], SGg.d)
        self.groups[tg] = (Qg, Qdg, Kg, Vg, SGg)

    def s1(self, c):
        fw = self.fw
        pe, dve = fw.pe, fw.dve
        tg, cc = divmod(c, 4)
        if cc == 0:
            if tg == 0:
                self.load_group(0)
            if tg + 1 < 8:
                self.load_group(tg + 1)
        Qg, Qdg, Kg, Vg, SGg = self.groups[tg]
        lr = slice(cc * 128, (cc + 1) * 128)
        ident_b = self.L.ident_b
        bA = [self.banks.next(), self.banks.next()]
        for h in range(8):
            bk, bk_b = bA[h // 4]
            pe.op(lambda e: e.matmul(bk[:, (h % 4) * 128:(h % 4 + 1) * 128], lhsT=Kg[:, h, lr], rhs=Qg[:, h, lr], start=True, stop=True),
                  [Kg.b, Qg.b], [bk_b], inc=(h % 4 == 3))
        sTm = self.sTm_r.next()
        for half in range(2):
            bk, bk_b = bA[half]
            dve.op(lambda e: e.tensor_tensor(out=sTm[:, half * 512:(half + 1) * 512], in0=bk[:, :], in1=self.decT[:, half * 512:(half + 1) * 512],
                                             op=ALU.mult), [self.decT.b], [sTm.b, bk_b])
        bT, bT_b = self.banks.next()
        for h in range(8):
            pe.op(lambda e: e.matmul(bT[:, h * 64:(h + 1) * 64], lhsT=Kg[:, h, lr], rhs=ident_b[0:64, 0:64], start=True, stop=True),
                  [Kg.b, ident_b.b], [bT_b], inc=(h == 7))
        Kp = self.Kp_r.next()
        dve.op(lambda e: e.tensor_tensor(out=Kp[:], in0=bT[:, :], in1=self.kdec[:], op=ALU.mult), [self.kdec.b], [Kp.b, bT_b])
        self.st[c] = (sTm, Kp)

    def s2(self, c):
        fw = self.fw
        pe, act, dve, pool = fw.pe, fw.act, fw.dve, fw.pool
        tg, cc = divmod(c, 4)
        Qg, Qdg, Kg, Vg, SGg = self.groups[tg]
        lr = slice(cc * 128, (cc + 1) * 128)
        sTm, Kp = self.st.pop(c)
        R32, ro, sq, y, st8, mtok, epst = self.R32, self.ro, self.sq, self.y, self.st8, self.mtok, self.L.epst
        Rb_prev = self.Rb_prev
        bO, bO_b = self.banks.next()
        for h in range(8):
            hs = slice(h * 64, (h + 1) * 64)
            pe.op(lambda e: e.matmul(bO[:, hs], lhsT=sTm[:, h * 128:(h + 1) * 128], rhs=Vg[:, cc, hs], start=True, stop=(c == 0)),
                  [sTm.b, Vg.b], [bO_b], inc=(c == 0 and h == 7))
            if c > 0:
                pe.op(lambda e: e.matmul(bO[:, hs], lhsT=Qdg[:, h, lr], rhs=Rb_prev[:, hs], start=False, stop=True),
                      [Qdg.b, Rb_prev.b], [bO_b], inc=(h == 7))
        if c < 31:
            bKV, bKV_b = self.banks.next()
            for h in range(8):
                hs = slice(h * 64, (h + 1) * 64)
                pe.op(lambda e: e.matmul(bKV[0:64, hs], lhsT=Kp[:, hs], rhs=Vg[:, cc, hs], start=True, stop=True),
                      [Kp.b, Vg.b], [bKV_b], inc=(h == 7))
            dve.op(lambda e: e.tensor_tensor(out=R32[:], in0=R32[:], in1=self.cdec[:], op=ALU.mult), [R32.b, self.cdec.b], [R32.b])
            dve.op(lambda e: e.tensor_tensor(out=R32[:], in0=R32[:], in1=bKV[0:64, :], op=ALU.add), [R32.b], [R32.b, bKV_b])
            Rb = self.Rb_r.next()
            pool.op(lambda e: e.tensor_copy(out=Rb[:], in_=R32[:]), [R32.b], [Rb.b])
            self.Rb_prev = Rb
        hview = lambda ap: ap.rearrange("p (h e) -> p h e", e=64)
        dve.op(lambda e: e.tensor_copy(out=ro[:], in_=bO[:, :]), [], [ro.b, bO_b])
        dve.op(lambda e: e.tensor_tensor(out=sq[:], in0=ro[:], in1=ro[:], op=ALU.mult), [ro.b], [sq.b])
        dve.op(lambda e: e.tensor_reduce(out=st8[:, 0:8], in_=hview(ro[:]), axis=AX.X, op=ALU.add), [ro.b], [st8.b])
        dve.op(lambda e: e.tensor_reduce(out=st8[:, 8:16], in_=hview(sq[:]), axis=AX.X, op=ALU.add), [sq.b, st8.b], [st8.b])
        dve.op(lambda e: e.tensor_scalar(out=st8[:, 16:24], in0=st8[:, 0:8], scalar1=1.0 / 64, scalar2=None, op0=ALU.mult), [st8.b], [st8.b])
        dve.op(lambda e: e.tensor_tensor(out=st8[:, 24:32], in0=st8[:, 16:24], in1=st8[:, 16:24], op=ALU.mult), [st8.b], [st8.b])
        dve.op(lambda e: e.scalar_tensor_tensor(out=st8[:, 8:16], in0=st8[:, 8:16], scalar=1.0 / 64, in1=st8[:, 24:32],
                                                op0=ALU.mult, op1=ALU.subtract), [st8.b], [st8.b])
        dve.op(lambda e: e.tensor_scalar(out=st8[:, 8:16], in0=st8[:, 8:16], scalar1=EPS, scalar2=None, op0=ALU.add), [st8.b], [st8.b])
        neghalf = self.L.neghalf
        pool.op(lambda e: e.tensor_tensor(out=st8[:, 8:16], in0=st8[:, 8:16], in1=neghalf[:, 0:8], op=ALU.pow), [st8.b, neghalf.b], [st8.b])
        mean_b = st8[:, 16:24].unsqueeze(2).to_broadcast([128, 8, 64])
        rstd_b = st8[:, 8:16].unsqueeze(2).to_broadcast([128, 8, 64])
        dve.op(lambda e: e.tensor_tensor(out=hview(y[:]), in0=hview(ro[:]), in1=mean_b, op=ALU.subtract), [ro.b, st8.b], [y.b])
        dve.op(lambda e: e.tensor_tensor(out=hview(y[:]), in0=hview(y[:]), in1=rstd_b, op=ALU.mult), [y.b, st8.b], [y.b])
        dve.op(lambda e: e.tensor_tensor(out=y[:], in0=y[:], in1=SGg[:, cc, :], op=ALU.mult), [y.b, SGg.b], [y.b])
        dve.op(lambda e: e.tensor_tensor(out=mtok[:], in0=y[:], in1=self.retgn[:], op=ALU.mult), [y.b, self.retgn.b], [mtok.b])

    def s3(self, c):
        fw = self.fw
        pe, dve, sp = fw.pe, fw.dve, fw.sp
        tg, cc = divmod(c, 4)
        lr = slice(cc * 128, (cc + 1) * 128)
        ident_b, mtok = self.L.ident_b, self.mtok
        if cc == 0:
            self.mst = self.mst_r.next()
        mst = self.mst
        bX, bX_b = self.banks.next()
        for fc in range(4):
            pe.op(lambda e: e.matmul(bX[:, fc * 128:(fc + 1) * 128], lhsT=mtok[:, fc * 128:(fc + 1) * 128], rhs=ident_b[:], start=True, stop=True),
                  [mtok.b, ident_b.b], [bX_b], inc=(fc == 3))
        dve.op(lambda e: e.tensor_copy(out=mst[:, :, lr], in_=bX[:, :].rearrange("p (f l) -> p f l", l=128)), [], [mst.b, bX_b])
        if cc == 3:
            ts = slice(tg * 512, (tg + 1) * 512)
            sp.dma(self.L.mixT[0:512, ts].rearrange("(f p) n -> p f n", p=128), mst[:], [mst.b], [], mst.d)


def PHASE_B(L):
    L = NS(L)
    fw = L.fw

    class _AllBanks:
        def next(self):
            return fw.bank()

    with ExitStack() as ph:
        B = RetentionB(L, ph, _AllBanks())
        for c in range(32):
            B.s1(c)
            B.s2(c)
            B.s3(c)
        fw.barrier()


def PHASE_C(L, with_B=False):
    L = NS(L)
    fw, I = L.fw, L.I
    pe, act, dve, pool, sp = fw.pe, fw.act, fw.dve, fw.pool, fw.sp
    ident_b, ident_f, epst = L.ident_b, L.ident_f, L.epst
    with ExitStack() as ph:
        biasT = T(fw, ph, "biasT", [128, 4, 2, 128], F32, dma=True)
        sp.dma(biasT[:], I["biasT"][:, :, :, :], [], [biasT.b], biasT.d)
        cb = T(fw, ph, "cb", [128, 4], F32, dma=True)
        sp.dma(cb[:], I["cbias"][:, :], [], [cb.b], cb.d)
        subln = T(fw, ph, "subln", [128, 128], F32, dma=True)
        sp.dma(subln[:], I["subln_b"][:, :], [], [subln.b], subln.d)
        lamv = T(fw, ph, "lamv", [128, 4, 64], F32, dma=True)
        sp.dma(lamv[:], I["lamvec"][:, :, :], [], [lamv.b], lamv.d)
        bhl = T(fw, ph, "bhl", [128, 2, 4, 2, 128], BF16)
        Vp = T(fw, ph, "Vp", [128, 32, 4, 129], BF16)
        lt = T(fw, ph, "lt", [128, 8], F32)
        zero1 = T(fw, ph, "zero1", [128, 1], F32)
        dve.op(lambda e: e.memset(zero1[:], 0.0), [], [zero1.b])
        prod = T(fw, ph, "lprod", [128, 2, 64], F32)
        dve.op(lambda e: e.tensor_tensor(out=prod[:, 0, :], in0=lamv[:, 0, :], in1=lamv[:, 1, :], op=ALU.mult), [lamv.b], [prod.b])
        dve.op(lambda e: e.tensor_tensor(out=prod[:, 1, :], in0=lamv[:, 2, :], in1=lamv[:, 3, :], op=ALU.mult), [lamv.b, prod.b], [prod.b])
        dve.op(lambda e: e.tensor_reduce(out=lt[:, 0:2], in_=prod[:], axis=AX.X, op=ALU.add), [prod.b], [lt.b])
        act.op(lambda e: e.activation(out=lt[:, 2:4], in_=lt[:, 0:2], func=AF.Exp), [lt.b], [lt.b])
        dve.op(lambda e: e.tensor_tensor(out=lt[:, 4:5], in0=lt[:, 3:4], in1=lt[:, 2:3], op=ALU.subtract), [lt.b], [lt.b])
        dve.op(lambda e: e.tensor_scalar(out=lt[:, 4:5], in0=lt[:, 4:5], scalar1=-LAMBDA_INIT, scalar2=None, op0=ALU.add), [lt.b], [lt.b])
        dve.op(lambda e: e.tensor_scalar(out=subln[:], in0=subln[:], scalar1=1.0 - LAMBDA_INIT, scalar2=None, op0=ALU.mult), [subln.b], [subln.b])
        QTr = Ring([T(fw, ph, "QTh%d" % i, [128, S], BF16, dma=True) for i in range(2)])
        KTr = Ring([T(fw, ph, "KTh%d" % i, [128, S], BF16, dma=True) for i in range(2)])
        heads = {}

        def load_head(h):
            QTh, KTh = QTr.next(), KTr.next()
            sp.dma(QTh[:], L.QT_dif[h * 128:(h + 1) * 128, :], [], [QTh.b], QTh.d)
            sp.dma(KTh[:], L.KT_dif[h * 128:(h + 1) * 128, :], [], [KTh.b], KTh.d)
            heads[h] = (QTh, KTh)

        load_head(0)
        with ExitStack() as tmp:
            btmp = T(fw, tmp, "btmp", [128, 4, 2, 128], F32)
            dve.op(lambda e: e.tensor_copy(out=bhl[:, 0], in_=biasT[:]), [biasT.b], [bhl.b])
            dve.op(lambda e: e.tensor_copy(out=btmp[:], in_=bhl[:, 0]), [bhl.b], [btmp.b])
            dve.op(lambda e: e.tensor_tensor(out=btmp[:], in0=biasT[:], in1=btmp[:], op=ALU.subtract), [biasT.b, btmp.b], [btmp.b])
            dve.op(lambda e: e.tensor_copy(out=bhl[:, 1], in_=btmp[:]), [btmp.b, bhl.b], [bhl.b])
            Vall = T(fw, tmp, "Vall", [128, 32, 512], BF16, dma=True)
            for g in range(4):
                sp.dma(Vall[:, g * 8:(g + 1) * 8, :], L.V_dif[g * 1024:(g + 1) * 1024, :].rearrange("(c p) f -> p c f", p=128), [], [Vall.b], Vall.d)
            pool.op(lambda e: e.memset(Vp[:, :, :, 128:129], 1.0), [], [Vp.b])
            for g in range(4):
                src = Vall[:, g * 8:(g + 1) * 8, :].rearrange("p c (h e) -> p c h e", e=128)
                if g % 2 == 0:
                    dve.op(lambda e: e.tensor_copy(out=Vp[:, g * 8:(g + 1) * 8, :, 0:128], in_=src), [Vall.b], [Vp.b])
                else:
                    act.op(lambda e: e.activation(out=Vp[:, g * 8:(g + 1) * 8, :, 0:128], in_=src, func=AF.Copy), [Vall.b], [Vp.b])
            fw.barrier()
        Pr = Ring([T(fw, ph, "P%d" % i, [128, 512], BF16) for i in range(3)])
        o1 = T(fw, ph, "o1", [128, 4, 128], F32)
        oo = T(fw, ph, "oo", [128, 4, 128], F32)
        osq = T(fw, ph, "osq", [128, 4, 128], F32)
        zz = T(fw, ph, "zz", [128, 16], F32)
        mtk_r = Ring([T(fw, ph, "mtk%d" % i, [128, 4, 128], BF16) for i in range(2)])
        oraw = T(fw, ph, "oraw", [128, 4, 132], F32, parts=4)
        epi_late = []
        mst_r = Ring([T(fw, ph, "cmst%d" % i, [128, 512], BF16, dma=True) for i in range(2)])
        Ob = [fw.banks[i] for i in range(4)]
        Sb = Ring([fw.banks[i] for i in (4, 5, 6)])
        bX, bX_b = fw.banks[7]
        steps = [(h, s, m, kb) for h in range(4) for s in range(8) for m in range(2) for kb in range(4 * s + 4)]

        def emit_qk(step):
            h, s, m, kb = step
            if h not in heads:
                load_head(h)
            if s == 5 and m == 0 and kb == 0 and h + 1 < 4 and (h + 1) not in heads:
                load_head(h + 1)
            QTh, KTh = heads[h]
            pr = slice(m * 64, (m + 1) * 64)
            qb_lo = max(4 * s, kb)
            off = (qb_lo - 4 * s) * 128
            near = []
            if kb >= 4 * s:
                near.append((kb, 0))
            if 4 * s <= kb + 1 <= 4 * s + 3:
                near.append((kb + 1, 1))
            sbk, sbk_b = Sb.next()
            pe.op(lambda e: e.matmul(sbk[:, off:512], lhsT=KTh[pr, kb * 128:(kb + 1) * 128], rhs=QTh[pr, qb_lo * 128:(4 * s + 4) * 128],
                                     start=True, stop=(len(near) == 0)), [KTh.b, QTh.b], [sbk_b], inc=(len(near) == 0))
            for ni, (qb, kind) in enumerate(near):
                o_ = (qb - 4 * s) * 128
                for hl in range(2):
                    last = (ni == len(near) - 1) and hl == 1
                    pe.op(lambda e: e.matmul(sbk[:, o_:o_ + 128], lhsT=ident_b[:], rhs=bhl[:, hl, h, kind, :], start=False, stop=last),
                          [ident_b.b, bhl.b], [sbk_b], inc=last)
            far_lo = max(4 * s, kb + 2)
            P = Pr.next()
            if far_lo <= 4 * s + 3:
                fo = (far_lo - 4 * s) * 128
                act.op(lambda e: e.activation(out=P[:, fo:512], in_=sbk[:, fo:512], func=AF.Exp, bias=cb[:, h:h + 1], scale=1.0),
                       [cb.b], [P.b, sbk_b])
            if near:
                n0 = (near[0][0] - 4 * s) * 128
                n1 = (near[-1][0] - 4 * s + 1) * 128
                act.op(lambda e: e.activation(out=P[:, n0:n1], in_=sbk[:, n0:n1], func=AF.Exp, bias=zero1[:, 0:1], scale=1.0),
                       [zero1.b], [P.b, sbk_b])
            return P

        def emit_pv(step, P):
            h, s, m, kb = step
            qb_lo = max(4 * s, kb)
            for qb in range(qb_lo, 4 * s + 4):
                j = qb - 4 * s
                ob, ob_b = Ob[j]
                pe.op(lambda e: e.matmul(ob[:, 0:129], lhsT=P[:, j * 128:(j + 1) * 128], rhs=Vp[:, kb, h, :], start=(kb == 0), stop=(kb == qb)),
                      [P.b, Vp.b], [ob_b], inc=True)

        def emit_epilogue(h, s, m):
            for j in range(4):
                ob, ob_b = Ob[j]
                act.op(lambda e: e.activation(out=oraw[:, j, 0:129], in_=ob[:, 0:129], func=AF.Copy), [], [oraw.bs[j], ob_b])
            for j in range(4):
                rb = oraw.bs[j]
                if m == 0:
                    dve.op(lambda e: e.reciprocal(out=zz[:, j:j + 1], in_=oraw[:, j, 128:129]), [rb], [zz.b])
                    dve.op(lambda e: e.tensor_scalar(out=o1[:, j, :], in0=oraw[:, j, 0:128], scalar1=zz[:, j:j + 1], scalar2=None, op0=ALU.mult),
                           [rb, zz.b], [o1.b])
                else:
                    dve.op(lambda e: e.reciprocal(out=zz[:, j:j + 1], in_=oraw[:, j, 128:129]), [rb], [zz.b])
                    dve.op(lambda e: e.tensor_tensor(out=zz[:, 4 + j:5 + j], in0=zz[:, j:j + 1], in1=L_lt(lt), op=ALU.mult), [zz.b, lt.b], [zz.b])
                    dve.op(lambda e: e.scalar_tensor_tensor(out=oo[:, j, :], in0=oraw[:, j, 0:128], scalar=zz[:, 4 + j:5 + j], in1=o1[:, j, :],
                                                            op0=ALU.mult, op1=ALU.add), [rb, zz.b, o1.b], [oo.b])
            if m == 0:
                return
            dve.op(lambda e: e.tensor_tensor(out=osq[:], in0=oo[:], in1=oo[:], op=ALU.mult), [oo.b], [osq.b])
            mtk = mtk_r.next()
            dve.op(lambda e: e.tensor_reduce(out=zz[:, 8:12], in_=osq[:], axis=AX.X, op=ALU.add), [osq.b, zz.b], [zz.b])
            dve.op(lambda e: e.tensor_scalar(out=zz[:, 12:16], in0=zz[:, 8:12], scalar1=1.0 / 128, scalar2=EPS, op0=ALU.mult, op1=ALU.add), [zz.b], [zz.b])
            pool.op(lambda e: e.tensor_tensor(out=zz[:, 12:16], in0=zz[:, 12:16], in1=L.neghalf[:, 0:4], op=ALU.pow), [zz.b, L.neghalf.b], [zz.b])
            for j in range(4):
                dve.op(lambda e: e.scalar_tensor_tensor(out=mtk[:, j, :], in0=oo[:, j, :], scalar=zz[:, 12 + j:13 + j], in1=subln[:],
                                                        op0=ALU.mult, op1=ALU.mult), [oo.b, zz.b, subln.b], [mtk.b])

            def late(mtk=mtk, h=h, s=s):
                for j in range(4):
                    pe.op(lambda e: e.matmul(bX[:, j * 128:(j + 1) * 128], lhsT=mtk[:, j, :], rhs=ident_b[:], start=True, stop=True),
                          [mtk.b, ident_b.b], [bX_b], inc=(j == 3))
                mst = mst_r.next()
                dve.op(lambda e: e.tensor_copy(out=mst[:], in_=bX[:, :]), [], [mst.b, bX_b])
                sp.dma(L.mixT[(4 + h) * 128:(5 + h) * 128, s * 512:(s + 1) * 512], mst[:], [mst.b], [], mst.d)

            epi_late.append([20, late])

        Bsched = {}
        if with_B:
            RB = RetentionB(L, ph, Ring([fw.banks[i] for i in (4, 5, 6, 7)]))
            per = len(steps) // 32
            for c in range(32):
                Bsched.setdefault(c * per, []).append(lambda c=c: RB.s1(c))
                Bsched.setdefault(c * per + per // 6, []).append(lambda c=c: RB.s2(c))
                Bsched.setdefault(c * per + (5 * per) // 6, []).append(lambda c=c: RB.s3(c))
        LOOK = 2
        pendP = [emit_qk(steps[i]) for i in range(LOOK)]
        for k, step in enumerate(steps):
            if k % 16 == 8:
                L.issue_wcasts(1)
            for fn in Bsched.get(k, ()):
                fn()
            for item in list(epi_late):
                item[0] -= 1
                if item[0] <= 0:
                    epi_late.remove(item)
                    item[1]()
            if k + LOOK < len(steps):
                pendP.append(emit_qk(steps[k + LOOK]))
            curP = pendP.pop(0)
            emit_pv(step, curP)
            h, s, m, kb = step
            if kb == 4 * s + 3:
                emit_epilogue(h, s, m)
        for item in epi_late:
            item[1]()
        L.issue_wcasts()
        fw.barrier()


def L_lt(lt):
    return lt[:, 4:5]


def PHASE_DE(L, phases):
    L = NS(L)
    fw, I = L.fw, L.I
    pe, act, dve, pool, sp = fw.pe, fw.act, fw.dve, fw.pool, fw.sp
    ident_b, ident_f, epst, rmsnorm = L.ident_b, L.ident_f, L.epst, L.rmsnorm
    U32 = mybir.dt.uint32
    NT = 256
    with ExitStack() as outer:
        if "D" in phases:
            with ExitStack() as ph:
                wout = T(fw, ph, "wout", [128, 8, 1024], BF16, dma=True)
                wpq = T(fw, ph, "wpq", [128, 8, 2048], BF16, dma=True)
                keys = T(fw, ph, "keys", [128, 16, 128], BF16, dma=True)
                if "C" in phases:
                    sp.dma(wout[:], L.wout_bf.rearrange("(c p) f -> p c f", p=128), [], [wout.b], wout.d)
                    sp.dma(wpq[:], L.wpq_bf.rearrange("(c p) f -> p c f", p=128), [], [wpq.b], wpq.d)
                    sp.dma(keys[:], L.keys_bf[:, :, :], [], [keys.b], keys.d)
                else:
                    for c in range(8):
                        pool.dma(wout[:, c, :], I["w_out"][c * 128:(c + 1) * 128, :], [], [wout.b], wout.d)
                        pool.dma(wpq[:, c, :], I["w_pq"][c * 128:(c + 1) * 128, :], [], [wpq.b], wpq.d)
                    pool.dma(keys[:], I["keysT"][:, :, :], [], [keys.b], keys.d)
                g_ffn = T(fw, ph, "g_ffn", [128, 8], F32, dma=True)
                sp.dma(g_ffn[:], I["g_ffn"][:, :], [], [g_ffn.b], g_ffn.d)
                io16 = T(fw, ph, "io16", [128, 8, 16, 16], F32, dma=True)
                sp.dma(io16[:], I["iota16r"].rearrange("p (h k a) -> p h k a", h=8, k=16), [], [io16.b], io16.d)
                xr = Ring([T(fw, ph, "xd%d" % i, [128, 8, NT], F32, dma=True) for i in range(2)])
                mr = Ring([T(fw, ph, "md%d" % i, [128, 8, NT], BF16, dma=True) for i in range(2)])
                x1r = Ring([T(fw, ph, "x1d%d" % i, [128, 8, NT], F32, dma=True) for i in range(2)])
                h2r = Ring([T(fw, ph, "h2d%d" % i, [128, 8, NT], BF16, dma=True) for i in range(2)])
                qTr = Ring([T(fw, ph, "qT%d" % i, [128, 16, NT], BF16) for i in range(2)])
                scr_ = Ring([T(fw, ph, "sc%d" % i, [128, 16, 128], F32) for i in range(2)])
                sc2 = T(fw, ph, "sc2", [128, 16, 128], F32, parts=16)
                mx = T(fw, ph, "mx", [128, 16, 16], F32, parts=16)
                idx = T(fw, ph, "idx", [128, 16, 16], U32, parts=16)
                idxf = T(fw, ph, "idxf", [128, 16, 16], F32)
                cand = T(fw, ph, "cand", [128, 8, 112], F32)
                cand2 = T(fw, ph, "cand2", [128, 8, 112], F32, parts=8)
                m2 = T(fw, ph, "m2", [128, 8, 16], F32, parts=8)
                pos = T(fw, ph, "pos", [128, 8, 16], U32, parts=8)
                pa = T(fw, ph, "pa", [128, 4, 8, 16], U32)
                paf = T(fw, ph, "paf", [128, 4, 8, 16], F32)
                abf = T(fw, ph, "abf", [128, 4, 8, 16], F32)
                oh = T(fw, ph, "oh", [128, 8, 16, 16], F32)
                IJG = T(fw, ph, "IJG", [128, 3, 128], F32, parts=3)
                gz = T(fw, ph, "gz", [128, 16], F32)
                lstg = Ring([T(fw, ph, "lstg%d" % i, [128, 3, 128], BF16, dma=True) for i in range(2)])
                xT_v = I["xT"].rearrange("(c p) n -> p c n", p=128)
                mT_v = L.mixT.rearrange("(c p) n -> p c n", p=128)
                x1_v = L.x1T.rearrange("(c p) n -> p c n", p=128)
                h2_v = L.h2T_d.rearrange("(c p) n -> p c n", p=128)

                pst = {}

                def load_p(t):
                    ts = slice(t * NT, (t + 1) * NT)
                    xt, mt = xr.next(), mr.next()
                    sp.dma(xt[:], xT_v[:, :, ts], [], [xt.b], xt.d)
                    sp.dma(mt[:], mT_v[:, :, ts], [], [mt.b], mt.d)
                    pst[t] = dict(xt=xt, mt=mt, x1=x1r.next(), h2=h2r.next(), qT=qTr.next())

                def p1(t):
                    d = pst[t]
                    ts = slice(t * NT, (t + 1) * NT)
                    xt, mt, x1 = d["xt"], d["mt"], d["x1"]
                    for fc in range(8):
                        bk, bk_b = fw.bank()
                        for mc in range(8):
                            pe.op(lambda e: e.matmul(bk[:, 0:NT], lhsT=wout[:, mc, fc * 128:(fc + 1) * 128], rhs=mt[:, mc, :], start=(mc == 0), stop=(mc == 7)),
                                  [wout.b, mt.b], [bk_b], inc=(mc == 7))
                        dve.op(lambda e: e.tensor_tensor(out=x1[:, fc, :], in0=bk[:, 0:NT], in1=xt[:, fc, :], op=ALU.add), [xt.b], [x1.b, bk_b])
                    sp.dma(x1_v[:, :, ts], x1[:], [x1.b], [], x1.d)
                    L.rms_a(x1, NT)

                def p2(t):
                    d = pst.pop(t)
                    ts = slice(t * NT, (t + 1) * NT)
                    x1, h2, qT = d["x1"], d["h2"], d["qT"]
                    L.rms_b(x1, g_ffn, h2, NT)
                    sp.dma(h2_v[:, :, ts], h2[:], [h2.b], [], h2.d)
                    for hp in range(16):
                        bk, bk_b = fw.bank()
                        for c in range(8):
                            pe.op(lambda e: e.matmul(bk[:, 0:NT], lhsT=wpq[:, c, hp * 128:(hp + 1) * 128], rhs=h2[:, c, :], start=(c == 0), stop=(c == 7)),
                                  [wpq.b, h2.b], [bk_b], inc=(c == 7))
                        act.op(lambda e: e.activation(out=qT[:, hp, :], in_=bk[:, 0:NT], func=AF.Copy), [], [qT.b, bk_b])
                    return qT

                def stage_s(qT, sub):
                    nsl = slice(sub * 128, (sub + 1) * 128)
                    sc = scr_.next()
                    for q4 in range(4):
                        bk, bk_b = fw.bank()
                        for r in range(4):
                            hp = q4 * 4 + r
                            pe.op(lambda e: e.matmul(bk[:, r * 128:(r + 1) * 128], lhsT=qT[:, hp, nsl], rhs=keys[:, hp, :], start=True, stop=True),
                                  [qT.b, keys.b], [bk_b], inc=(r == 3))
                        act.op(lambda e: e.activation(out=sc[:, q4 * 4:(q4 + 1) * 4, :], in_=bk[:, :].rearrange("p (r k) -> p r k", k=128), func=AF.Copy),
                               [], [sc.b, bk_b])
                    return sc

                def k_a(sc):
                    for g in range(16):
                        dve.op(lambda e: e.max(out=mx[:, g, 0:8], in_=sc[:, g, :]), [sc.b], [mx.bs[g]])
                    for g in range(16):
                        dve.op(lambda e: e.max_index(out=idx[:, g, 0:8], in_max=mx[:, g, 0:8], in_values=sc[:, g, :]), [sc.b, mx.bs[g]], [idx.bs[g]])
                    for g in range(16):
                        dve.op(lambda e: e.match_replace(out=sc2[:, g, :], in_to_replace=mx[:, g, 0:8], in_values=sc[:, g, :], imm_value=-1e30),
                               [sc.b, mx.bs[g]], [sc2.bs[g]])
                    for g in range(16):
                        dve.op(lambda e: e.max(out=mx[:, g, 8:16], in_=sc2[:, g, :]), [sc2.bs[g]], [mx.bs[g]])
                    for g in range(16):
                        dve.op(lambda e: e.max_index(out=idx[:, g, 8:16], in_max=mx[:, g, 8:16], in_values=sc2[:, g, :]), [sc2.bs[g], mx.bs[g]], [idx.bs[g]])
                    dve.op(lambda e: e.tensor_copy(out=idxf[:], in_=idx[:]), idx.bs, [idxf.b])

                def k_b(t, sub):
                    ncol = slice(t * NT + sub * 128, t * NT + (sub + 1) * 128)
                    mxv = mx[:].rearrange("p (h two) k -> p h two k", two=2)
                    idv = idxf[:].rearrange("p (h two) k -> p h two k", two=2)
                    c1 = cand[:, :, 0:64].rearrange("p h (a b) -> p h a b", b=16)
                    dve.op(lambda e: e.tensor_tensor(out=c1, in0=mxv[:, :, 0, 0:4].unsqueeze(3).to_broadcast([128, 8, 4, 16]),
                                                     in1=mxv[:, :, 1, :].unsqueeze(2).to_broadcast([128, 8, 4, 16]), op=ALU.add), mx.bs, [cand.b])
                    c2 = cand[:, :, 64:112].rearrange("p h (a b) -> p h a b", b=4)
                    dve.op(lambda e: e.tensor_tensor(out=c2, in0=mxv[:, :, 0, 4:16].unsqueeze(3).to_broadcast([128, 8, 12, 4]),
                                                     in1=mxv[:, :, 1, 0:4].unsqueeze(2).to_broadcast([128, 8, 12, 4]), op=ALU.add), mx.bs, [cand.b])
                    for h in range(8):
                        dve.op(lambda e: e.max(out=m2[:, h, 0:8], in_=cand[:, h, :]), [cand.b], [m2.bs[h]])
                    for h in range(8):
                        dve.op(lambda e: e.max_index(out=pos[:, h, 0:8], in_max=m2[:, h, 0:8], in_values=cand[:, h, :]), [cand.b, m2.bs[h]], [pos.bs[h]])
                    for h in range(8):
                        dve.op(lambda e: e.match_replace(out=cand2[:, h, :], in_to_replace=m2[:, h, 0:8], in_values=cand[:, h, :], imm_value=-1e30),
                               [cand.b, m2.bs[h]], [cand2.bs[h]])
                    for h in range(8):
                        dve.op(lambda e: e.max(out=m2[:, h, 8:16], in_=cand2[:, h, :]), [cand2.bs[h]], [m2.bs[h]])
                    for h in range(8):
                        dve.op(lambda e: e.max_index(out=pos[:, h, 8:16], in_max=m2[:, h, 8:16], in_values=cand2[:, h, :]), [cand2.bs[h], m2.bs[h]], [pos.bs[h]])
                    dve.op(lambda e: e.tensor_single_scalar(out=pa[:, 0], in_=pos[:], scalar=4, op=ALU.logical_shift_right), pos.bs, [pa.b])
                    dve.op(lambda e: e.tensor_single_scalar(out=pa[:, 1], in_=pos[:], scalar=15, op=ALU.bitwise_and), pos.bs + [pa.b], [pa.b])
                    dve.op(lambda e: e.tensor_single_scalar(out=pa[:, 2], in_=pos[:], scalar=2, op=ALU.logical_shift_right), pos.bs + [pa.b], [pa.b])
                    dve.op(lambda e: e.tensor_single_scalar(out=pa[:, 3], in_=pos[:], scalar=3, op=ALU.bitwise_and), pos.bs + [pa.b], [pa.b])
                    dve.op(lambda e: e.tensor_copy(out=paf[:], in_=pa[:]), [pa.b], [paf.b])
                    dve.op(lambda e: e.tensor_single_scalar(out=abf[:, 2], in_=paf[:, 0], scalar=4.0, op=ALU.is_ge), [paf.b], [abf.b])
                    dve.op(lambda e: e.scalar_tensor_tensor(out=abf[:, 0], in0=paf[:, 2], scalar=-12.0, in1=paf[:, 0], op0=ALU.add, op1=ALU.subtract),
                           [paf.b, abf.b], [abf.b])
                    dve.op(lambda e: e.tensor_tensor(out=abf[:, 1], in0=paf[:, 3], in1=paf[:, 1], op=ALU.subtract), [paf.b, abf.b], [abf.b])
                    dve.op(lambda e: e.tensor_tensor(out=abf[:, 0:2], in0=abf[:, 0:2], in1=abf[:, 2:3].to_broadcast([128, 2, 8, 16]), op=ALU.mult),
                           [abf.b], [abf.b])
                    dve.op(lambda e: e.tensor_tensor(out=abf[:, 0:2], in0=abf[:, 0:2], in1=paf[:, 0:2], op=ALU.add), [abf.b, paf.b], [abf.b])
                    for w in range(2):
                        dve.op(lambda e: e.tensor_tensor(out=oh[:], in0=io16[:], in1=abf[:, w].unsqueeze(3).to_broadcast([128, 8, 16, 16]), op=ALU.is_equal),
                               [io16.b, abf.b], [oh.b])
                        dve.op(lambda e: e.tensor_tensor(out=oh[:], in0=oh[:], in1=idv[:, :, w, :].unsqueeze(2).to_broadcast([128, 8, 16, 16]), op=ALU.mult),
                               [oh.b, idxf.b], [oh.b])
                        dve.op(lambda e: e.tensor_reduce(out=IJG[:, w, :].rearrange("p (h k) -> p h k", k=16), in_=oh[:], axis=AX.X, op=ALU.add),
                               [oh.b], [IJG.bs[w]])
                    g3 = IJG[:, 2, :].rearrange("p (h k) -> p h k", k=16)
                    gb = IJG.bs[2]
                    dve.op(lambda e: e.tensor_tensor(out=g3, in0=m2[:], in1=m2[:, :, 0:1].to_broadcast([128, 8, 16]), op=ALU.subtract), m2.bs, [gb])
                    act.op(lambda e: e.activation(out=IJG[:, 2, :], in_=IJG[:, 2, :], func=AF.Exp), [gb], [gb])
                    dve.op(lambda e: e.tensor_reduce(out=gz[:, 0:8], in_=g3, axis=AX.X, op=ALU.add), [gb], [gz.b])
                    dve.op(lambda e: e.reciprocal(out=gz[:, 8:16], in_=gz[:, 0:8]), [gz.b], [gz.b])
                    dve.op(lambda e: e.tensor_tensor(out=g3, in0=g3, in1=gz[:, 8:16].unsqueeze(2).to_broadcast([128, 8, 16]), op=ALU.mult), [gb, gz.b], [gb])
                    bk, bk_b = fw.bank()
                    for a in range(3):
                        pe.op(lambda e: e.matmul(bk[:, a * 128:(a + 1) * 128], lhsT=IJG[:, a, :], rhs=ident_f[:], start=True, stop=True),
                              [IJG.bs[a], ident_f.b], [bk_b], inc=(a == 2))
                    lg = lstg.next()
                    act.op(lambda e: e.activation(out=lg[:], in_=bk[:, 0:384].rearrange("p (a n) -> p a n", n=128), func=AF.Copy),
                           [], [lg.b, bk_b])
                    sp.dma(L.LSTd[:, :, ncol], lg[:], [lg.b], [], lg.d)

                NTL = S // NT
                load_p(0)
                p1(0)
                qn = p2(0)
                for t in range(NTL):
                    qc = qn
                    if t + 1 < NTL:
                        load_p(t + 1)
                    s0 = stage_s(qc, 0)
                    s1 = stage_s(qc, 1)
                    k_a(s0)
                    if t + 1 < NTL:
                        p1(t + 1)
                    k_b(t, 0)
                    k_a(s1)
                    if t + 1 < NTL:
                        qn = p2(t + 1)
                    k_b(t, 1)
                fw.barrier()
        if "E" in phases:
            with ExitStack() as ph:
                g_fin = T(fw, ph, "g_fin", [128, 8], F32, dma=True)
                sp.dma(g_fin[:], I["g_fin"][:, :], [], [g_fin.b], g_fin.d)
                iof = T(fw, ph, "iof", [128, 128], F32, dma=True)
                sp.dma(iof[:], I["iota"][:, :], [], [iof.b], iof.d)
                NG = 8
                NTILE = S // NT
                io3 = T(fw, ph, "io3", [128, NG, 128], BF16)
                for r in range(NG):
                    dve.op(lambda e: e.tensor_copy(out=io3[:, r, :], in_=iof[:]), [iof.b], [io3.b])
                H = [T(fw, ph, "GTh%d" % i, [128, NT, 64], BF16) for i in range(2)]
                Ar = Ring([T(fw, ph, "A%d" % i, [128, NG, 64], BF16) for i in range(3)])
                Br = Ring([T(fw, ph, "B%d" % i, [128, NG, 128], BF16) for i in range(3)])
                lstr = Ring([T(fw, ph, "lst%d" % i, [128, 3, NT], BF16, dma=True) for i in range(3)])
                wdr = Ring([T(fw, ph, "wd%d" % i, [128, 8, 512], BF16, dma=True) for i in range(2)])
                wur = Ring([T(fw, ph, "wu%d" % i, [128, 4, 1024], BF16, dma=True) for i in range(3)])
                h2r = Ring([T(fw, ph, "h2e%d" % i, [128, 8, NT], BF16, dma=True) for i in range(2)])
                x1r = Ring([T(fw, ph, "x1e%d" % i, [128, 8, NT], F32, dma=True) for i in range(2)])
                ost = T(fw, ph, "ost", [128, 8, NT], F32, dma=True)
                LAG = 2
                gar = Ring([T(fw, ph, "ga%d" % i, [128, NT], F32) for i in range(3)])
                awr = Ring([T(fw, ph, "aw%d" % i, [128, NT], BF16) for i in range(LAG + 3)])
                x1_v = L.x1T.rearrange("(c p) n -> p c n", p=128)
                h2_v = L.h2T_d.rearrange("(c p) n -> p c n", p=128)
                oT_v = L.outT.rearrange("(c p) n -> p c n", p=128)
                Ob = [fw.banks[i] for i in range(4)]
                Ab = Ring([fw.banks[i] for i in (4, 5)])
                Gb = Ring([fw.banks[i] for i in (6, 7)])

                lists = {}

                def load_lists(t_):
                    lt_ = lstr.next()
                    sp.dma(lt_[:], L.LSTd[:, :, t_ * NT:(t_ + 1) * NT], [], [lt_.b], lt_.d)
                    lists[t_] = lt_

                units = [(0, 0, g) for g in range(NT // NG)]
                for t_ in range(NTILE):
                    units += [(t_, 1, g) for g in range(NT // NG)]
                    if t_ + 1 < NTILE:
                        units += [(t_ + 1, 0, g) for g in range(NT // NG)]
                ust = {"next1": 0, "next2": 0, "ops": {}}

                def g_stage1(u):
                    t_, half, grp = units[u]
                    LT = lists[t_]
                    n0 = grp * NG
                    A, B = Ar.next(), Br.next()
                    dve.op(lambda e: e.tensor_tensor(out=A[:], in0=io3[:, :, half * 64:(half + 1) * 64],
                                                     in1=LT[:, 0, n0:n0 + NG].unsqueeze(2).to_broadcast([128, NG, 64]), op=ALU.is_equal),
                           [io3.b, LT.b], [A.b])
                    pool.op(lambda e: e.tensor_tensor(out=A[:], in0=A[:], in1=LT[:, 2, n0:n0 + NG].unsqueeze(2).to_broadcast([128, NG, 64]), op=ALU.mult),
                            [A.b, LT.b], [A.b])
                    dve.op(lambda e: e.tensor_tensor(out=B[:], in0=io3[:], in1=LT[:, 1, n0:n0 + NG].unsqueeze(2).to_broadcast([128, NG, 128]), op=ALU.is_equal),
                           [io3.b, LT.b], [B.b])
                    ust["ops"][u] = (A, B)

                def g_stage2(u):
                    t_, half, grp = units[u]
                    A, B = ust["ops"].pop(u)
                    bk, bk_b = Gb.next()
                    for r in range(NG):
                        pe.op(lambda e: e.matmul(bk[:, r * 64:(r + 1) * 64], lhsT=B[:, r, :], rhs=A[:, r, :], start=True, stop=True),
                              [A.b, B.b], [bk_b], inc=(r == NG - 1))
                    nl = grp * NG
                    G = H[half]
                    act.op(lambda e: e.activation(out=G[:, nl:nl + NG, :], in_=bk[:, :].rearrange("j (n i) -> j n i", i=64), func=AF.Copy),
                           [], [G.b, bk_b])

                def g_step():
                    if ust["next2"] >= len(units):
                        return
                    while ust["next1"] <= min(ust["next2"] + 1, len(units) - 1) and units[ust["next1"]][0] in lists:
                        g_stage1(ust["next1"])
                        ust["next1"] += 1
                    g_stage2(ust["next2"])
                    ust["next2"] += 1

                load_lists(0)
                for _ in range(NT // NG):
                    g_step()

                tiles = {}

                def load_tile(t_):
                    h2_, x1_ = h2r.next(), x1r.next()
                    ts_ = slice(t_ * NT, (t_ + 1) * NT)
                    sp.dma(h2_[:], h2_v[:, :, ts_], [], [h2_.b], h2_.d)
                    sp.dma(x1_[:], x1_v[:, :, ts_], [], [x1_.b], x1_.d)
                    tiles[t_] = (h2_, x1_)

                wts = {}

                def load_w(gi):
                    ig = gi % 32
                    wd_, wu_ = wdr.next(), wur.next()
                    sp.dma(wd_[:], L.wdT_bf[ig], [], [wd_.b], wd_.d)
                    sp.dma(wu_[:], L.wup_bf[ig], [], [wu_.b], wu_.d)
                    wts[gi] = (wd_, wu_)

                deferred = [None]
                load_tile(0)
                load_w(0)
                for t in range(NTILE):
                    ts = slice(t * NT, (t + 1) * NT)
                    h2, x1 = tiles.pop(t)
                    if t + 1 < NTILE:
                        load_lists(t + 1)
                        if deferred[0] is None:
                            load_tile(t + 1)
                    if t == 0:
                        for j in range(4):
                            ob, ob_b = Ob[j]
                            dve.op(lambda e: e.memset(ob[:, :], 0.0), [], [ob_b])
                    pend = []

                    def emit_up(item):
                        p_aw, p_wu, p_ii = item
                        for dc in range(8):
                            ob, ob_b = Ob[dc // 2]
                            pe.op(lambda e: e.matmul(ob[:, (dc % 2) * NT:(dc % 2 + 1) * NT], lhsT=p_wu[:, p_ii, dc * 128:(dc + 1) * 128], rhs=p_aw[:],
                                                     start=False, stop=False, skip_group_check=True), [p_wu.b, p_aw.b], [ob_b], inc=(dc == 7))

                    for i in range(128):
                        if i == 6 and deferred[0] is not None:
                            deferred[0]()
                            deferred[0] = None
                            if t + 1 < NTILE:
                                load_tile(t + 1)
                        ii = i % 4
                        gi = t * 32 + i // 4
                        if ii == 0 and gi + 1 < NTILE * 32:
                            load_w(gi + 1)
                        wd, wu = wts[gi]
                        if i % 2 == 0:
                            g_step()
                        ab, ab_b = Ab.next()
                        for c in range(8):
                            pe.op(lambda e: e.matmul(ab[:, 0:NT], lhsT=wd[:, c, ii * 128:(ii + 1) * 128], rhs=h2[:, c, :], start=(c == 0), stop=(c == 7)),
                                  [wd.b, h2.b], [ab_b], inc=(c == 7))
                        ga, aw = gar.next(), awr.next()
                        act.op(lambda e: e.activation(out=ga[:], in_=ab[:, 0:NT], func=AF.Gelu), [], [ga.b, ab_b])
                        G = H[i // 64]
                        dve.op(lambda e: e.tensor_tensor(out=aw[:], in0=ga[:], in1=G[:, :, i % 64], op=ALU.mult), [ga.b, G.b], [aw.b])
                        pend.append((aw, wu, ii))
                        if len(pend) > LAG:
                            emit_up(pend.pop(0))
                        if ii == 3:
                            wts.pop(gi - 1, None)
                    while pend:
                        emit_up(pend.pop(0))
                    for dc in range(8):
                        ob, ob_b = Ob[dc // 2]
                        dve.op(lambda e: e.tensor_tensor(out=x1[:, dc, :], in0=ob[:, (dc % 2) * NT:(dc % 2 + 1) * NT], in1=x1[:, dc, :], op=ALU.add),
                               [x1.b], [x1.b, ob_b])
                        if dc % 2 == 1 and t + 1 < NTILE:
                            dve.op(lambda e: e.memset(ob[:, :], 0.0), [], [ob_b])

                    def fin(x1=x1, ts=ts):
                        L.rms_a(x1, NT, bank=Ab.next())
                        L.rms_b(x1, g_fin, ost, NT)
                        sp.dma(oT_v[:, :, ts], ost[:], [ost.b], [], ost.d)

                    deferred[0] = fin
                deferred[0]()
                fw.barrier()


def kernel(**inputs):
    inp = {k: np.asarray(v) for k, v in inputs.items()}
    shared = _host_prep(inp)
    nc = build_program()
    x = np.asarray(inp["x"], np.float32)
    in_maps = []
    for c in range(8):
        m = dict(shared)
        m["xT"] = np.ascontiguousarray(x[c].T)
        in_maps.append(m)
    res = run_bass_kernel_spmd(nc, in_maps, core_ids=list(range(8)))
    out = np.stack([np.ascontiguousarray(np.asarray(res.results[c]["outT"]).T) for c in range(8)], axis=0)
    return out.astype(np.float32)
```
